# Optimizing a Trainium2 kernel written in Bass

```python
import math
import jax
import jax.numpy as jnp
from jax import lax
import numpy as np

D_MODEL = 1024
BATCH = 4
SEQ = 4096
DEPTH = 2

GRID_W = 64
CTX_LEN = 256

A_HEADS = 8
A_HEAD_DIM = 64
A_W = A_HEADS * A_HEAD_DIM
A_DECAY_RANK = 64
A_ICL_RANK = 64
A_GATE_RANK = 128
A_IN = 3 * A_W + 2 * A_DECAY_RANK + 2 * A_ICL_RANK + A_GATE_RANK
A_DECAY_SCALE = math.exp(-0.5)
GN_EPS_RWKV = 64e-5
B_W = 512
B_BLOCKS = 8
B_BLOCK = B_W // B_BLOCKS
B_CONV = 4
B_C = 8.0
B_IN = 2 * B_W
C_HEADS = 4
C_HEAD_DIM = 128
C_W = C_HEADS * C_HEAD_DIM
C_IN = 4 * C_W
ROPE_BASE = 10000.0
D_HEADS = 4
D_HEAD_DIM = 128
D_W = D_HEADS * D_HEAD_DIM
D_IN = 4 * D_W + 4 * D_HEADS
CHUNK = 128
EVEN_IN = A_IN + B_IN
ODD_IN = C_IN + D_IN
MIX_W = A_W + B_W
N_EXPERTS = 32
TOP_K = 4
D_FF = 1024
SWIGLU_ALPHA = 1.702
SWIGLU_LIMIT = 7.0
DN_ALPHA = (2 * DEPTH) ** 0.25
DN_BETA = (8 * DEPTH) ** -0.25
LN_EPS = 1e-5
N_MOD = 6
N_EVEN = (DEPTH + 1) // 2
N_ODD = DEPTH // 2

kernel_name = 'hybrid_rwkv7_rglru_retention_mlstm_moe_dit'


def _flip(t):
    return t[:, ::-1]


def _ident(t):
    return t


def _heads(t, n):
    return t.reshape(t.shape[:-1] + (n, t.shape[-1] // n))


def layer_norm(x, g, b):
    xf = x.astype(jnp.float32)
    mu = jnp.mean(xf, -1, keepdims=True)
    var = jnp.mean(jnp.square(xf - mu), -1, keepdims=True)
    return ((xf - mu) * lax.rsqrt(var + LN_EPS) * g + b).astype(x.dtype)


def group_norm(x, g, b, n_groups, eps):
    xf = _heads(x.astype(jnp.float32), n_groups)
    mu = jnp.mean(xf, -1, keepdims=True)
    var = jnp.mean(jnp.square(xf - mu), -1, keepdims=True)
    return ((xf - mu) * lax.rsqrt(var + eps)).reshape(x.shape) * g + b


def grid_qshift(z):
    bz, t, w = z.shape
    rows = t // GRID_W
    g = z.reshape(bz, rows, GRID_W, w // 4, 4)
    left = jnp.pad(g[:, :, :-1, :, 0], ((0, 0), (0, 0), (1, 0), (0, 0)))
    right = jnp.pad(g[:, :, 1:, :, 1], ((0, 0), (0, 0), (0, 1), (0, 0)))
    up = jnp.pad(g[:, :-1, :, :, 2], ((0, 0), (1, 0), (0, 0), (0, 0)))
    down = jnp.pad(g[:, 1:, :, :, 3], ((0, 0), (0, 1), (0, 0), (0, 0)))
    return jnp.stack([left, right, up, down], -1).reshape(bz, t, w)


def bidir_shift(z):
    bz, t, w = z.shape
    g = z.reshape(bz, t, w // 2, 2)
    prev = jnp.pad(g[:, :-1, :, 0], ((0, 0), (1, 0), (0, 0)))
    nxt = jnp.pad(g[:, 1:, :, 1], ((0, 0), (0, 1), (0, 0)))
    return jnp.stack([prev, nxt], -1).reshape(bz, t, w)


def axial_rope(t):
    n_tok, dh = t.shape[1], t.shape[-1]
    idx = jnp.arange(n_tok)
    row = (idx // GRID_W).astype(jnp.float32)
    col = (idx % GRID_W).astype(jnp.float32)
    n_freq = dh // 4
    inv = ROPE_BASE ** (-jnp.arange(n_freq, dtype=jnp.float32) / n_freq)
    ang = jnp.concatenate([row[:, None] * inv, col[:, None] * inv], -1)[None, :, None, :]
    cos, sin = jnp.cos(ang), jnp.sin(ang)
    t1, t2 = t[..., : dh // 2], t[..., dh // 2:]
    return jnp.concatenate([t1 * cos - t2 * sin, t1 * sin + t2 * cos], -1)


def centred_depthwise_conv(u, w, bias):
    y = lax.conv_general_dilated(
        u, w[:, None, :].astype(u.dtype), window_strides=(1,),
        padding=[(B_CONV // 2, B_CONV - 1 - B_CONV // 2)],
        dimension_numbers=('NWC', 'WIO', 'NWC'), feature_group_count=u.shape[-1])
    return y + bias


def _lin_comb(left, right):
    al, bl = left
    ar, br = right
    return al * ar, ar * bl + br


def linear_recurrence(a, b, h0):
    b = b.at[:, 0].add(a[:, 0] * h0)
    _, h = lax.associative_scan(_lin_comb, (a, b), axis=1)
    return h, h[:, -1]


def rwkv7_scan(s0, r, w, k, v, kk, a):
    xs = tuple(jnp.moveaxis(t, 1, 0) for t in (r, w, k, v, kk, a))

    def step(s, inp):
        r_t, w_t, k_t, v_t, kk_t, a_t = inp
        s_kk = jnp.einsum('bhvk,bhk->bhv', s, kk_t)
        s = (s * w_t[:, :, None, :] - s_kk[..., None] * (kk_t * a_t)[:, :, None, :]
             + v_t[..., None] * k_t[:, :, None, :])
        return s, jnp.einsum('bhvk,bhk->bhv', s, r_t)

    s_end, out = lax.scan(step, s0, xs)
    return jnp.moveaxis(out, 0, 1), s_end


def rwkv7_group(pc, pl, mu, w0, w_b, a0, a_b, g_b, k_k, k_a, r_k, gn_g, gn_b):
    zc = pc + (bidir_shift(pc) - pc) * mu
    zl = pl + (grid_qshift(pl) - pl) * mu

    def prep(z):
        z = z.astype(jnp.float32)
        r, k, v = (z[..., j * A_W:(j + 1) * A_W] for j in range(3))
        o = 3 * A_W
        wd = z[..., o:o + 2 * A_DECAY_RANK].reshape(z.shape[:-1] + (2, A_DECAY_RANK))
        o += 2 * A_DECAY_RANK
        ad = z[..., o:o + 2 * A_ICL_RANK].reshape(z.shape[:-1] + (2, A_ICL_RANK))
        o += 2 * A_ICL_RANK
        g = jax.nn.sigmoid(z[..., o:o + A_GATE_RANK]) @ g_b
        kk = _heads(k * k_k, A_HEADS)
        kk = kk / jnp.maximum(jnp.sqrt(jnp.sum(kk * kk, -1, keepdims=True)), 1e-12)
        dirs = []
        for d in range(2):
            w = jnp.exp(-A_DECAY_SCALE * jax.nn.sigmoid(w0[d] + jnp.tanh(wd[..., d, :]) @ w_b[d]))
            a = jax.nn.sigmoid(a0[d] + ad[..., d, :] @ a_b[d])
            kd = k * (1.0 + (a - 1.0) * k_a)
            dirs.append(tuple(_heads(t, A_HEADS) for t in (r, w, kd, v)) + (kk, _heads(a, A_HEADS)))
        return dirs, g

    def finish(wkv, dirs, g):
        r, v = dirs[0][0], dirs[0][3]
        bonus = sum(jnp.sum(r * dd[2] * r_k, -1, keepdims=True) for dd in dirs) * v
        y = group_norm(wkv.reshape(g.shape), gn_g, gn_b, A_HEADS, GN_EPS_RWKV) + bonus.reshape(g.shape)
        return y * g

    dirs_c, g_c = prep(zc)
    dirs_l, g_l = prep(zl)
    s0 = jnp.zeros((pc.shape[0], A_HEADS, A_HEAD_DIM, A_HEAD_DIM), jnp.float32)
    wkv_c = 0.0
    wkv_l = 0.0
    for d, fl in enumerate((_ident, _flip)):
        out_c, s_c = rwkv7_scan(s0, *(fl(t) for t in dirs_c[d]))
        out_l, _ = rwkv7_scan(s_c, *(fl(t) for t in dirs_l[d]))
        wkv_c = wkv_c + fl(out_c)
        wkv_l = wkv_l + fl(out_l)
    return finish(wkv_c, dirs_c, g_c).astype(pc.dtype), finish(wkv_l, dirs_l, g_l).astype(pl.dtype)


def rglru_group(pc, pl, conv_w, conv_b, gate_w, gate_b, lam):
    def prep(p):
        u = centred_depthwise_conv(p[..., :B_W], conv_w, conv_b).astype(jnp.float32)
        gate = jax.nn.gelu(p[..., B_W:].astype(jnp.float32))
        pre = jnp.einsum('btgi,dsgio->btdsgo', _heads(u, B_BLOCKS), gate_w)
        pre = pre.reshape(u.shape[:2] + (2, 2, B_W)) + gate_b
        rec = jax.nn.sigmoid(pre[..., 0, :])
        inp = jax.nn.sigmoid(pre[..., 1, :])
        log_a = -B_C * rec * jax.nn.softplus(-lam)
        a = jnp.exp(log_a)
        bx = jnp.sqrt(-jnp.expm1(2.0 * log_a)) * inp * u[..., None, :]
        return a, bx, gate

    a_c, b_c, g_c = prep(pc)
    a_l, b_l, g_l = prep(pl)
    h0 = jnp.zeros((pc.shape[0], B_W), jnp.float32)
    h_c = 0.0
    h_l = 0.0
    for d, fl in enumerate((_ident, _flip)):
        hc_d, h_end = linear_recurrence(fl(a_c[..., d, :]), fl(b_c[..., d, :]), h0)
        hl_d, _ = linear_recurrence(fl(a_l[..., d, :]), fl(b_l[..., d, :]), h_end)
        h_c = h_c + fl(hc_d)
        h_l = h_l + fl(hl_d)
    return (h_c * g_c).astype(pc.dtype), (h_l * g_l).astype(pl.dtype)


def retention_chunked(q, k, v, log_gamma, r0):
    bq, n_tok, nh, dk = q.shape
    dv = v.shape[-1]
    n = n_tok // CHUNK
    q, k = (t.reshape(bq, n, CHUNK, nh, dk) for t in (q, k))
    v = v.reshape(bq, n, CHUNK, nh, dv)
    pos = jnp.arange(CHUNK, dtype=jnp.float32)
    lg = log_gamma.astype(jnp.float32)[:, None]
    diff = pos[:, None] - pos[None, :]
    decay = jnp.where(diff >= 0, jnp.exp(lg[:, :, None] * jnp.maximum(diff, 0.0)), 0.0)
    scores = jnp.einsum('bnihd,bnjhd->bnhij', q, k) * decay
    intra = jnp.einsum('bnhij,bnjhe->bnihe', scores, v)
    zeta = jnp.exp(lg * (CHUNK - 1.0 - pos))
    kv = jnp.einsum('bnjhd,hj,bnjhe->bnhde', k, zeta, v)
    chunk_decay = jnp.exp(lg[:, 0] * CHUNK)[:, None, None]

    def step(r, kv_i):
        return r * chunk_decay + kv_i, r

    r_last, r_prev = lax.scan(step, r0, jnp.moveaxis(kv, 1, 0))
    r_prev = jnp.moveaxis(r_prev, 0, 1)
    xi = jnp.exp(lg * (pos + 1.0)).T
    inter = jnp.einsum('bnihd,bnhde->bnihe', q, r_prev) * xi[:, :, None]
    return (intra + inter).reshape(bq, n_tok, nh, dv), r_last


def retention_group(pc, pl, log_gamma, gn_g, gn_b):
    def prep(p, rotate):
        p = p.astype(jnp.float32)
        q, k, v = (_heads(p[..., j * C_W:(j + 1) * C_W], C_HEADS) for j in range(3))
        k = k * C_HEAD_DIM ** -0.5
        if rotate:
            q, k = axial_rope(q), axial_rope(k)
        return q, k, v, p[..., 3 * C_W:]

    qc, kc, vc, gc = prep(pc, False)
    ql, kl, vl, gl = prep(pl, True)
    r0 = jnp.zeros((pc.shape[0], C_HEADS, C_HEAD_DIM, C_HEAD_DIM), jnp.float32)
    o_c = 0.0
    o_l = 0.0
    for d, fl in enumerate((_ident, _flip)):
        oc_d, r_c = retention_chunked(fl(qc), fl(kc), fl(vc), log_gamma[d], r0)
        ol_d, _ = retention_chunked(fl(ql), fl(kl), fl(vl), log_gamma[d], r_c)
        o_c = o_c + fl(oc_d)
        o_l = o_l + fl(ol_d)
    y_c = group_norm(o_c.reshape(gc.shape), gn_g, gn_b, C_HEADS, LN_EPS) * jax.nn.silu(gc)
    y_l = group_norm(o_l.reshape(gl.shape), gn_g, gn_b, C_HEADS, LN_EPS) * jax.nn.silu(gl)
    return y_c.astype(pc.dtype), y_l.astype(pl.dtype)


def mlstm_chunked(q, k, v, ig, lf, state):
    bq, n_tok, nh, dh = q.shape
    n = n_tok // CHUNK
    q, k, v = (t.reshape(bq, n, CHUNK, nh, dh) for t in (q, k, v))
    ig, lf = (t.reshape(bq, n, CHUNK, nh) for t in (ig, lf))
    b = jnp.cumsum(lf, axis=2)
    tri = jnp.tril(jnp.ones((CHUNK, CHUNK), bool))[None, None, :, :, None]
    d_log = jnp.where(tri, b[:, :, :, None, :] - b[:, :, None, :, :] + ig[:, :, None, :, :], -jnp.inf)
    b_end = b[:, :, -1]
    w_end = b_end[:, :, None, :] - b + ig
    m_end = jnp.max(w_end, axis=2)
    e_end = jnp.exp(w_end - m_end[:, :, None, :])
    c_chunk = jnp.einsum('bnjh,bnjhd,bnjhe->bnhed', e_end, k, v)
    n_chunk = jnp.einsum('bnjh,bnjhd->bnhd', e_end, k)

    def step(carry, inp):
        c_st, n_st, m_st = carry
        c_i, n_i, m_i, b_i = inp
        m_new = jnp.maximum(b_i + m_st, m_i)
        s_old = jnp.exp(b_i + m_st - m_new)
        s_new = jnp.exp(m_i - m_new)
        new = (c_st * s_old[..., None, None] + c_i * s_new[..., None, None],
               n_st * s_old[..., None] + n_i * s_new[..., None], m_new)
        return new, carry

    xs = tuple(jnp.moveaxis(t, 1, 0) for t in (c_chunk, n_chunk, m_end, b_end))
    final, prev = lax.scan(step, state, xs)
    c_prev, n_prev, m_prev = (jnp.moveaxis(t, 0, 1) for t in prev)
    g_inter = b + m_prev[:, :, None, :]
    m_t = jnp.maximum(g_inter, jnp.max(d_log, axis=3))
    s_intra = jnp.einsum('bnihd,bnjhd->bnijh', q, k) * jnp.exp(d_log - m_t[:, :, :, None, :])
    s_inter = jnp.exp(g_inter - m_t)
    num = (jnp.einsum('bnijh,bnjhe->bnihe', s_intra, v)
           + jnp.einsum('bnihd,bnhed->bnihe', q, c_prev) * s_inter[..., None])
    den = jnp.sum(s_intra, axis=3) + jnp.einsum('bnihd,bnhd->bnih', q, n_prev) * s_inter
    den = jnp.maximum(jnp.abs(den), jnp.exp(-m_t))
    h = num / den[..., None]
    return h.reshape(bq, n_tok, nh, dh), final


def mlstm_group(pc, pl, ibias, fbias, gn_g, gn_b):
    def prep(p):
        p = p.astype(jnp.float32)
        q, k, v = (_heads(p[..., j * D_W:(j + 1) * D_W], D_HEADS) for j in range(3))
        gt = p[..., 4 * D_W:].reshape(p.shape[:2] + (2, 2, D_HEADS))
        ig = gt[..., 0, :, :] + ibias
        lf = jax.nn.log_sigmoid(gt[..., 1, :, :] + fbias)
        return q, k * D_HEAD_DIM ** -0.5, v, p[..., 3 * D_W:4 * D_W], ig, lf

    qc, kc, vc, oc, igc, lfc = prep(pc)
    ql, kl, vl, ol, igl, lfl = prep(pl)
    bz = pc.shape[0]
    s0 = (jnp.zeros((bz, D_HEADS, D_HEAD_DIM, D_HEAD_DIM), jnp.float32),
          jnp.zeros((bz, D_HEADS, D_HEAD_DIM), jnp.float32),
          jnp.full((bz, D_HEADS), -jnp.inf, jnp.float32))
    h_c = 0.0
    h_l = 0.0
    for d, fl in enumerate((_ident, _flip)):
        hc_d, s_c = mlstm_chunked(fl(qc), fl(kc), fl(vc), fl(igc[..., d, :]), fl(lfc[..., d, :]), s0)
        hl_d, _ = mlstm_chunked(fl(ql), fl(kl), fl(vl), fl(igl[..., d, :]), fl(lfl[..., d, :]), s_c)
        h_c = h_c + fl(hc_d)
        h_l = h_l + fl(hl_d)
    y_c = group_norm(h_c.reshape(oc.shape), gn_g, gn_b, D_HEADS, LN_EPS) * jax.nn.sigmoid(oc)
    y_l = group_norm(h_l.reshape(ol.shape), gn_g, gn_b, D_HEADS, LN_EPS) * jax.nn.sigmoid(ol)
    return y_c.astype(pc.dtype), y_l.astype(pl.dtype)


def moe(h, router_w, router_b, w1, b1, w2, b2):
    logits = (h @ router_w + router_b).astype(jnp.float32)
    top_val, top_idx = lax.top_k(logits, TOP_K)
    top_w = jax.nn.softmax(top_val, axis=-1)
    gates = jnp.sum(jax.nn.one_hot(top_idx, N_EXPERTS, dtype=jnp.float32) * top_w[..., None], axis=1)
    gates = gates.astype(h.dtype)
    y = jnp.zeros_like(h)
    for e in range(N_EXPERTS):
        u = h @ w1[e] + b1[e]
        glu = jnp.minimum(u[:, :D_FF], SWIGLU_LIMIT)
        lin = jnp.clip(u[:, D_FF:], -SWIGLU_LIMIT, SWIGLU_LIMIT)
        act = (lin + 1.0) * glu * jax.nn.sigmoid(SWIGLU_ALPHA * glu)
        y = y + gates[:, e:e + 1] * (act @ w2[e] + b2[e])
    return y


def setup_inputs(seed: int = 0) -> dict:
    key = jax.random.key(seed)
    ks = iter(jax.random.split(key, 64))

    def nrm(shape, scale):
        return jax.random.normal(next(ks), shape, jnp.float32) * scale

    lam_u = jax.random.uniform(next(ks), (N_EVEN, 2, B_W), jnp.float32, 0.9, 0.999)
    lam_base = lam_u ** (1.0 / B_C)
    gammas = jnp.log(1.0 - 2.0 ** (-5.0 - jnp.arange(C_HEADS, dtype=jnp.float32)))
    return {
        'x': nrm((BATCH, SEQ, D_MODEL), 1.0),
        'c': nrm((BATCH, D_MODEL), 1.0),
        'ctx': nrm((BATCH, CTX_LEN, D_MODEL), 1.0),
        'c_ctx': nrm((D_MODEL,), 1.0),
        'w_mod': nrm((DEPTH, D_MODEL, N_MOD * D_MODEL), D_MODEL ** -0.5),
        'b_mod': nrm((DEPTH, N_MOD * D_MODEL), 0.02),
        'ln_g': 1.0 + nrm((DEPTH, 2, D_MODEL), 0.02),
        'ln_b': nrm((DEPTH, 2, D_MODEL), 0.02),
        'even_w_in': nrm((N_EVEN, D_MODEL, EVEN_IN), D_MODEL ** -0.5),
        'even_w_out': nrm((N_EVEN, MIX_W, D_MODEL), MIX_W ** -0.5 * DN_BETA),
        'a_mu': jax.random.uniform(next(ks), (N_EVEN, A_IN), jnp.float32),
        'a_w0': jnp.linspace(-5.0, 1.0, A_W) + nrm((N_EVEN, 2, A_W), 0.1),
        'a_wB': nrm((N_EVEN, 2, A_DECAY_RANK, A_W), 0.1),
        'a_a0': nrm((N_EVEN, 2, A_W), 0.1),
        'a_aB': nrm((N_EVEN, 2, A_ICL_RANK, A_W), 0.5 * A_ICL_RANK ** -0.5),
        'a_gB': nrm((N_EVEN, A_GATE_RANK, A_W), A_GATE_RANK ** -0.5),
        'a_kk': 0.85 + nrm((N_EVEN, A_W), 0.05),
        'a_ka': 1.0 + nrm((N_EVEN, A_W), 0.05),
        'a_rk': nrm((N_EVEN, A_HEADS, A_HEAD_DIM), 0.1),
        'a_gn_g': 1.0 + nrm((N_EVEN, A_W), 0.02),
        'a_gn_b': nrm((N_EVEN, A_W), 0.02),
        'b_conv_w': nrm((N_EVEN, B_CONV, B_W), 0.5),
        'b_conv_b': nrm((N_EVEN, B_W), 0.02),
        'b_gate_w': nrm((N_EVEN, 2, 2, B_BLOCKS, B_BLOCK, B_BLOCK), B_BLOCK ** -0.5),
        'b_gate_b': nrm((N_EVEN, 2, 2, B_W), 0.02),
        'b_lam': jnp.log(lam_base) - jnp.log1p(-lam_base),
        'odd_w_in': nrm((N_ODD, D_MODEL, ODD_IN), D_MODEL ** -0.5),
        'odd_w_out': nrm((N_ODD, MIX_W, D_MODEL), MIX_W ** -0.5 * DN_BETA),
        'c_log_gamma': gammas * (1.0 + nrm((N_ODD, 2, C_HEADS), 0.05)),
        'c_gn_g': 1.0 + nrm((N_ODD, C_W), 0.02),
        'c_gn_b': nrm((N_ODD, C_W), 0.02),
        'd_ibias': nrm((N_ODD, 2, D_HEADS), 0.1),
        'd_fbias': jnp.linspace(3.0, 6.0, D_HEADS) + nrm((N_ODD, 2, D_HEADS), 0.1),
        'd_gn_g': 1.0 + nrm((N_ODD, D_W), 0.02),
        'd_gn_b': nrm((N_ODD, D_W), 0.02),
        'router_w': nrm((DEPTH, D_MODEL, N_EXPERTS), D_MODEL ** -0.5),
        'router_b': nrm((DEPTH, N_EXPERTS), 0.01),
        'exp_w1': nrm((DEPTH, N_EXPERTS, D_MODEL, 2 * D_FF), D_MODEL ** -0.5),
        'exp_b1': nrm((DEPTH, N_EXPERTS, 2 * D_FF), 0.02),
        'exp_w2': nrm((DEPTH, N_EXPERTS, D_FF, D_MODEL), D_FF ** -0.5 * DN_BETA),
        'exp_b2': nrm((DEPTH, N_EXPERTS, D_MODEL), 0.02),
    }


def reference(x, c, ctx, c_ctx, w_mod, b_mod, ln_g, ln_b, even_w_in, even_w_out, a_mu, a_w0, a_wB,
              a_a0, a_aB, a_gB, a_kk, a_ka, a_rk, a_gn_g, a_gn_b, b_conv_w, b_conv_b, b_gate_w,
              b_gate_b, b_lam, odd_w_in, odd_w_out, c_log_gamma, c_gn_g, c_gn_b, d_ibias, d_fbias,
              d_gn_g, d_gn_b, router_w, router_b, exp_w1, exp_b1, exp_w2, exp_b2):
    xl, xc = x, ctx
    bz = x.shape[0]
    for layer in range(DEPTH):
        last = layer == DEPTH - 1
        mod_l = (jax.nn.silu(c) @ w_mod[layer] + b_mod[layer]).reshape(bz, N_MOD, 1, D_MODEL)
        mod_c = (jax.nn.silu(c_ctx) @ w_mod[layer] + b_mod[layer]).reshape(N_MOD, 1, D_MODEL)
        hl = xl * (1.0 + mod_l[:, 1]) + mod_l[:, 0]
        hc = xc * (1.0 + mod_c[1]) + mod_c[0]
        i = layer // 2
        if layer % 2 == 0:
            pc, pl = hc @ even_w_in[i], hl @ even_w_in[i]
            ya_c, ya_l = rwkv7_group(pc[..., :A_IN], pl[..., :A_IN], a_mu[i], a_w0[i], a_wB[i], a_a0[i],
                                     a_aB[i], a_gB[i], a_kk[i], a_ka[i], a_rk[i], a_gn_g[i], a_gn_b[i])
            yb_c, yb_l = rglru_group(pc[..., A_IN:], pl[..., A_IN:], b_conv_w[i], b_conv_b[i],
                                     b_gate_w[i], b_gate_b[i], b_lam[i])
            w_out = even_w_out[i]
        else:
            pc, pl = hc @ odd_w_in[i], hl @ odd_w_in[i]
            ya_c, ya_l = retention_group(pc[..., :C_IN], pl[..., :C_IN], c_log_gamma[i], c_gn_g[i], c_gn_b[i])
            yb_c, yb_l = mlstm_group(pc[..., C_IN:], pl[..., C_IN:], d_ibias[i], d_fbias[i], d_gn_g[i], d_gn_b[i])
            w_out = odd_w_out[i]
        y_l = jnp.concatenate([ya_l, yb_l], -1) @ w_out
        xl = layer_norm(DN_ALPHA * xl + mod_l[:, 2] * y_l, ln_g[layer, 0], ln_b[layer, 0])
        moe_args = (router_w[layer], router_b[layer], exp_w1[layer], exp_b1[layer], exp_w2[layer], exp_b2[layer])
        hl = xl * (1.0 + mod_l[:, 4]) + mod_l[:, 3]
        if last:
            ffn_l = moe(hl.reshape(-1, D_MODEL), *moe_args).reshape(hl.shape)
        else:
            y_c = jnp.concatenate([ya_c, yb_c], -1) @ w_out
            xc = layer_norm(DN_ALPHA * xc + mod_c[2] * y_c, ln_g[layer, 0], ln_b[layer, 0])
            hc = xc * (1.0 + mod_c[4]) + mod_c[3]
            n_ctx = hc.shape[0] * hc.shape[1]
            ffn = moe(jnp.concatenate([hc.reshape(-1, D_MODEL), hl.reshape(-1, D_MODEL)], 0), *moe_args)
            ffn_c = ffn[:n_ctx].reshape(hc.shape)
            ffn_l = ffn[n_ctx:].reshape(hl.shape)
            xc = layer_norm(DN_ALPHA * xc + mod_c[5] * ffn_c, ln_g[layer, 1], ln_b[layer, 1])
        xl = layer_norm(DN_ALPHA * xl + mod_l[:, 5] * ffn_l, ln_g[layer, 1], ln_b[layer, 1])
    return xl
```

```python
import numpy as np
import concourse.bass as bass
import concourse.mybir as mybir
from concourse.bass_utils import run_bass_kernel_spmd

F32 = mybir.dt.float32
BF16 = mybir.dt.bfloat16
AF = mybir.ActivationFunctionType
ALU = mybir.AluOpType
AX = mybir.AxisListType


class Buf:
    def __init__(self, ap, name):
        self.ap = ap
        self.name = name
        self.w = None
        self.r = []
        self.excl = False

    def __getitem__(self, idx):
        return V(self, self.ap[idx])

    def v(self, ap=None):
        return V(self, self.ap if ap is None else ap)


class V:
    def __init__(self, buf, ap):
        self.buf = buf
        self.ap = ap

    def __getitem__(self, idx):
        return V(self.buf, self.ap[idx])

    def re(self, pat, **kw):
        return V(self.buf, self.ap.rearrange(pat, **kw))

    def bcast(self, shape):
        return V(self.buf, self.ap.to_broadcast(list(shape)))


class KB:
    ENG = ("tensor", "vector", "scalar", "gpsimd", "sync")

    def __init__(self, n_dma_sems=24, same_engine_sync=True):
        self.nc = bass.Bass("TRN2", target_bir_lowering=False)
        nc = self.nc
        self.e = {n: getattr(nc, n) for n in self.ENG}
        self.sem = {n: nc.alloc_semaphore(name=f"prog_{n}") for n in self.ENG}
        self.cnt = {n: 0 for n in self.ENG}
        self.seen = {n: {} for n in self.ENG}
        self.dsem = [nc.alloc_semaphore(name=f"dma_{i}") for i in range(n_dma_sems)]
        self.dcnt = [0] * n_dma_sems
        self.dnext = 0
        self.same_engine_sync = same_engine_sync
        self.out_tokens = []
        self.n_inst = 0

    def sb(self, name, shape, dtype=F32):
        self.uid = getattr(self, "uid", 0) + 1
        t = self.nc.alloc_sbuf_tensor(f"sb_{name}_p{self.uid}", list(shape), dtype)
        return Buf(t.ap(), name)

    def ps(self, name, shape, dtype=F32):
        t = self.nc.alloc_psum_tensor("psum_" + name, list(shape), dtype)
        b = Buf(t.ap(), name)
        b.excl = True
        return b

    def dram(self, name, shape, dtype=F32, kind="Internal"):
        t = self.nc.dram_tensor(name, list(shape), dtype, kind=kind)
        return Buf(t.ap(), name)

    def split(self, buf, views, name=None):
        return [Buf(v, f"{name or buf.name}_{i}") for i, v in enumerate(views)]

    def _wait(self, eng, tok):
        if tok is None:
            return
        kind = tok[0]
        if kind == "e":
            _, e2, c = tok
            if e2 == eng and (not self.same_engine_sync or eng == "tensor"):
                return
            key = ("e", e2)
            sem = self.sem[e2]
        else:
            _, i, c = tok
            key = ("d", i)
            sem = self.dsem[i]
        if self.seen[eng].get(key, 0) >= c:
            return
        self.e[eng].wait_ge(sem, c)
        self.seen[eng][key] = c

    def _deps(self, eng, reads, writes):
        for v in reads:
            self._wait(eng, v.buf.w)
        for v in writes:
            self._wait(eng, v.buf.w)
            for t in v.buf.r:
                self._wait(eng, t)

    def _mark(self, tok, reads, writes):
        for v in reads:
            b = v.buf
            b.r = [t for t in b.r if not (t[0] == tok[0] and t[1] == tok[1])] + [tok]
        for v in writes:
            v.buf.w = tok
            v.buf.r = []

    def op(self, eng, fn, reads, writes):
        ex = [v for v in reads if v.buf.excl]
        if ex:
            reads = [v for v in reads if not v.buf.excl]
            writes = list(writes) + ex
        self._deps(eng, reads, writes)
        inst = fn(self.e[eng])
        self.cnt[eng] += 1
        inst.then_inc(self.sem[eng], 1)
        self._mark(("e", eng, self.cnt[eng]), reads, writes)
        self.n_inst += 1
        return inst

    def dma(self, q, out, in_, **kw):
        i = self.dnext
        self.dnext = (self.dnext + 1) % len(self.dsem)
        if self.dcnt[i] > 0:
            self._wait(q, ("d", i, self.dcnt[i]))
        self._deps(q, [in_], [out])
        inst = self.e[q].dma_start(out=out.ap, in_=in_.ap, **kw)
        self.dcnt[i] += 16
        inst.then_inc(self.dsem[i], 16)
        tok = ("d", i, self.dcnt[i])
        self._mark(tok, [in_], [out])
        self.n_inst += 1
        return tok

    def dbg(self, name, v, shape, dtype=F32):
        if not getattr(self, "debug", False):
            return
        d = self.dram("dbg_" + name, list(shape), dtype, kind="ExternalOutput")
        self.dma("sync", d.v(), v)
        self.dbg_bufs = getattr(self, "dbg_bufs", []) + [d]

    def finish(self, bufs, eng="sync"):
        bufs = list(bufs) + getattr(self, "dbg_bufs", [])
        for b in bufs:
            self._wait(eng, b.w)

    def mm(self, out, lhsT, rhs, start=True, stop=True):
        return self.op("tensor", lambda e: e.matmul(out.ap, lhsT.ap, rhs.ap, start=start, stop=stop),
                       [lhsT, rhs] + ([] if start else [out]), [out])

    def tr(self, out, in_, ident):
        return self.op("tensor", lambda e: e.transpose(out.ap, in_.ap, ident.ap), [in_, ident], [out])

    def act(self, out, in_, func, bias=None, scale=None, eng="scalar", accum_out=None):
        kw = {}
        reads = [in_]
        writes = [out]
        if bias is not None:
            if isinstance(bias, V):
                kw["bias"] = bias.ap
                reads.append(bias)
            else:
                kw["bias"] = bias
        if scale is not None:
            if isinstance(scale, V):
                kw["scale"] = scale.ap
                reads.append(scale)
            else:
                kw["scale"] = scale
        if accum_out is not None:
            kw["accum_out"] = accum_out.ap
            writes.append(accum_out)
        return self.op(eng, lambda e: e.activation(out.ap, in_.ap, func, **kw), reads, writes)

    def tt(self, out, a, b, op, eng="vector"):
        return self.op(eng, lambda e: e.tensor_tensor(out.ap, a.ap, b.ap, op), [a, b], [out])

    def ts(self, out, a, s1, s2, op0, op1=None, eng="vector", accum_out=None):
        reads = [a]
        writes = [out]

        def g(s):
            if isinstance(s, V):
                reads.append(s)
                return s.ap
            return s
        s1a, s2a = g(s1), g(s2)
        kw = {}
        if op1 is not None:
            kw["op1"] = op1
        if accum_out is not None:
            kw["accum_out"] = accum_out.ap
            writes.append(accum_out)
        return self.op(eng, lambda e: e.tensor_scalar(out.ap, a.ap, s1a, s2a, op0, **kw), reads, writes)

    def stt(self, out, a, s, b, op0, op1, eng="vector"):
        reads = [a, b]
        if isinstance(s, V):
            reads.append(s)
            sa = s.ap
        else:
            sa = s
        return self.op(eng, lambda e: e.scalar_tensor_tensor(out.ap, a.ap, sa, b.ap, op0, op1), reads, [out])

    def copy(self, out, in_, eng="vector"):
        if eng == "scalar":
            return self.op(eng, lambda e: e.copy(out.ap, in_.ap), [in_], [out])
        return self.op(eng, lambda e: e.tensor_copy(out.ap, in_.ap), [in_], [out])

    def memset(self, out, val, eng="vector"):
        return self.op(eng, lambda e: e.memset(out.ap, val), [], [out])

    def reduce(self, out, in_, op, axis=AX.X, eng="vector"):
        return self.op(eng, lambda e: e.tensor_reduce(out.ap, in_.ap, axis, op), [in_], [out])

    def recip(self, out, in_, eng="vector"):
        return self.op(eng, lambda e: e.reciprocal(out.ap, in_.ap), [in_], [out])


import contextlib


class Scope:
    uid = 0

    def __init__(self, k):
        self.k = k
        self.stack = contextlib.ExitStack()

    def sb(self, name, shape, dtype=F32):
        Scope.uid += 1
        name = f"sc_{name}_u{Scope.uid}"
        t = self.stack.enter_context(self.k.nc.sbuf_tensor(name, list(shape), dtype))
        return Buf(t.ap(), name)

    def __enter__(self):
        return self

    def __exit__(self, *a):
        self.k.barrier()
        self.stack.close()
        return False


def _barrier(self):
    for eng in self.ENG:
        for e2 in self.ENG:
            if e2 != eng and self.cnt[e2] > 0:
                self._wait(eng, ("e", e2, self.cnt[e2]))
        for i, c in enumerate(self.dcnt):
            if c > 0:
                self._wait(eng, ("d", i, c))


KB.barrier = _barrier
KB.scope = lambda self: Scope(self)


class PSView:
    def __init__(self, b):
        self.b = b

    def v(self):
        return self.b[:, 0:128]

    def __getitem__(self, idx):
        return self.b[:, 0:128][idx]


D = 1024
NE = 32
DN_ALPHA = 4 ** 0.25
LN_EPS = 1e-5
SW_ALPHA = 1.702
SW_LIM = 7.0


def load_consts(k, cd):
    c = {}
    c["ident"] = k.sb("c_ident", [128, 128], F32)
    k.dma("sync", c["ident"].v(), cd["ident"].v())
    c["onesm"] = k.sb("c_onesm", [128, 128], F32)
    k.memset(c["onesm"].v(), 1.0 / D)
    c["eps"] = k.sb("c_eps", [128, 1], F32)
    k.memset(c["eps"].v(), LN_EPS)
    return c


def ln_block(k, c, s, N, gcol, bcol, out_fn, tmp, ps_mean, ps_ex2):
    sq, mean_sb, rstd, t = tmp["sq"], tmp["mean"], tmp["rstd"], tmp["t"]
    k.act(sq[:, :, :N], s[:, :, :N], AF.Square)
    for ci in range(8):
        k.mm(ps_mean[:, :N], c["onesm"].v(), s[:, ci, :N], start=(ci == 0), stop=(ci == 7))
    for ci in range(8):
        k.mm(ps_ex2[:, :N], c["onesm"].v(), sq[:, ci, :N], start=(ci == 0), stop=(ci == 7))
    k.copy(mean_sb[:, :N], ps_mean[:, :N], eng="scalar")
    k.tt(rstd[:, :N], mean_sb[:, :N], mean_sb[:, :N], ALU.mult)
    k.tt(rstd[:, :N], ps_ex2[:, :N], rstd[:, :N], ALU.subtract)
    k.act(rstd[:, :N], rstd[:, :N], AF.Sqrt, bias=c["eps"].v())
    k.recip(rstd[:, :N], rstd[:, :N])
    for ci in range(8):
        k.tt(t[:, :N], s[:, ci, :N], mean_sb[:, :N], ALU.subtract)
        k.tt(t[:, :N], t[:, :N], rstd[:, :N], ALU.mult)
        out_fn(ci, t[:, :N])


def phase_C(k, c, io, groups, NTG, PS):
    nc = k.nc
    psc = k.scope()
    psc.__enter__()
    modv = psc.sb("modv", [128, 48, 2], F32)
    sc1 = psc.sb("sc1", [128, 8, 2], F32)
    sc4 = psc.sb("sc4", [128, 8, 2], F32)
    lnp = psc.sb("lnp", [128, 32], F32)
    hT = psc.sb("hT", [128, 8, NTG], BF16)
    acc = psc.sb("acc", [128, 8, NTG], F32)
    gatesT = psc.sb("gatesT", [32, NTG], F32)
    b1s = psc.sb("b1s", [128, 32 * 16], F32)
    b2s = psc.sb("b2s", [128, 32 * 8], F32)
    rbs = psc.sb("rbs", [32, 1], F32)
    k.dma("sync", lnp.v(), io["lnp"].v())
    k.dma("sync", b1s.v(), io["b1"].v())
    k.dma("sync", b2s.v(), io["b2"].v())
    k.dma("sync", rbs.v(), io["rb"].v())

    if "modv" in io:
        k.copy(modv.v(), io["modv"].v(), eng="gpsimd")
    else:
        compute_mod(k, PS, io, list(range(6)), modv)
    k.ts(sc1.v(), modv[:, 8:16, :], 1.0, None, ALU.add)
    k.ts(sc4.v(), modv[:, 32:40, :], 1.0, None, ALU.add)

    def mv(m, ci, col):
        return modv[:, m * 8 + ci, col:col + 1]

    k.dbg("modv", modv.v(), [128, 48, 2])
    for blocks in groups:
        _group(k, c, io, blocks, PS, modv, sc4, lnp, hT, acc, gatesT, b1s, b2s, rbs, mv)
    psc.__exit__(None, None, None)


def _group(k, c, io, blocks, PS, modv, sc4, lnp, hT, acc, gatesT, b1s, b2s, rbs, mv):
    nc = k.nc
    g0 = blocks[0][0]
    k.memset(acc.v(), 0.0, eng="gpsimd")

    with k.scope() as sc:
        wo = sc.sb("wo", [128, 8, 1024], BF16)
        rw = sc.sb("rw", [128, 8, 32], F32)
        xb = sc.sb("xb", [128, 8, 512], F32)
        yb = sc.sb("yb", [128, 8, 512], BF16)
        s = sc.sb("s", [128, 8, 512], F32)
        x1 = sc.sb("x1", [128, 8, 512], F32)
        hf = sc.sb("hf", [128, 8, 512], F32)
        tmp = {"sq": sc.sb("sq", [128, 8, 512], F32), "mean": sc.sb("mean", [128, 512], F32),
               "rstd": sc.sb("rstd", [128, 512], F32), "t": sc.sb("t", [128, 512], F32)}
        t2 = sc.sb("t2", [128, 512], F32)
        lg = sc.sb("lg", [32, 512], F32)
        top8 = sc.sb("top8", [128, 8], F32)
        sm = sc.sb("sm", [128, 4], F32)
        ex = sc.sb("ex", [128, 32], F32)
        msk = sc.sb("msk", [128, 32], F32)
        k.dma("gpsimd", wo.v(), io["w_out"].v().re("(c p) n -> p c n", p=128))
        with nc.allow_non_contiguous_dma(reason="router w"):
            k.dma("sync", rw.v(), io["rw"].v().re("(c p) n -> p c n", p=128))
        for (st, N, col) in blocks:
            if "load_xy" in io:
                io["load_xy"](xb, yb, st, N, s, tmp["sq"])
            else:
                k.dma("sync", xb[:, :, :N], io["xT"].v()[:, st:st + N].re("(c p) t -> p c t", p=128))
                k.dma("gpsimd", yb[:, :, :N], io["yT"].v()[:, st:st + N].re("(c p) t -> p c t", p=128))
            for dc in range(8):
                ps = PS[dc % 2]
                for kc in range(8):
                    k.mm(ps[:, :N], wo[:, kc, dc * 128:(dc + 1) * 128], yb[:, kc, :N], start=(kc == 0), stop=(kc == 7))
                k.act(t2[:, :N], ps[:, :N], AF.Identity, scale=mv(2, dc, col))
                k.stt(s[:, dc, :N], xb[:, dc, :N], DN_ALPHA, t2[:, :N], ALU.mult, ALU.add)

            def o1(ci, tv, N=N, col=col):
                k.act(x1[:, ci, :N], tv, AF.Identity, scale=lnp[:, ci:ci + 1], bias=lnp[:, 16 + ci:17 + ci])
                k.act(hf[:, ci, :N], x1[:, ci, :N], AF.Identity, scale=sc4[:, ci, col:col + 1], bias=mv(3, ci, col))
                k.copy(hT[:, ci, st - g0:st - g0 + N], hf[:, ci, :N], eng="gpsimd")
            if st == 128:
                k.dbg("s", s.v(), [128, 8, 512])
            ln_block(k, c, s, N, None, None, o1, tmp, PS[2], PS[3])
            if st == 128:
                k.dbg("x1", x1.v(), [128, 8, 512])
                k.dbg("hf", hf.v(), [128, 8, 512])
                k.dbg("mean", tmp["mean"].v(), [128, 512])
                k.dbg("rstd", tmp["rstd"].v(), [128, 512])
            k.dma("sync", io["x1d"].v()[:, st:st + N].re("(c p) t -> p c t", p=128), x1[:, :, :N])
            for kc in range(8):
                k.mm(PS[4][0:32, :N], rw[:, kc, :], hf[:, kc, :N], start=(kc == 0), stop=(kc == 7))
            k.act(lg[:, :N], PS[4][0:32, :N], AF.Identity, bias=rbs.v())
            for j in range(N // 128):
                pt = PS[5 + (j % 2)]
                k.tr(pt[:, 0:32], lg[:, j * 128:(j + 1) * 128], c["ident"][0:32, 0:32])
                k.op("vector", lambda e: e.max(top8.ap, pt.ap[:, 0:32]), [pt.v()], [top8.v()])
                k.ts(sm[:, 0:1], top8[:, 0:1], -1.0, None, ALU.mult)
                k.act(ex.v(), pt[:, 0:32], AF.Exp, bias=sm[:, 0:1])
                k.ts(msk.v(), pt[:, 0:32], top8[:, 3:4], None, ALU.is_ge)
                k.tt(ex.v(), ex.v(), msk.v(), ALU.mult)
                k.reduce(sm[:, 1:2], ex.v(), ALU.add)
                k.recip(sm[:, 2:3], sm[:, 1:2])
                k.ts(ex.v(), ex.v(), sm[:, 2:3], None, ALU.mult)
                pg = PS[7]
                k.tr(pg[0:32, 0:128], ex.v(), c["ident"].v())
                k.copy(gatesT[:, st - g0 + j * 128: st - g0 + (j + 1) * 128], pg[0:32, 0:128], eng="scalar")

    if g0 == 0:
        k.dbg("gatesT", gatesT.v(), [32, gatesT.ap.shape[1]])
    with k.scope() as sc:
        NR = 3
        w1r = [sc.sb(f"w1r{i}", [128, 8, 2048], BF16) for i in range(2)]
        w2r = [sc.sb(f"w2r{i}", [128, 8, 1024], BF16) for i in range(2)]
        actT = [sc.sb(f"actT{i}", [128, 8, 512], BF16) for i in range(2)]
        g = [sc.sb(f"g{i}", [128, 512], F32) for i in range(2)]
        sg = [sc.sb(f"sg{i}", [128, 512], F32) for i in range(2)]
        l = [sc.sb(f"l{i}", [128, 512], F32) for i in range(2)]
        ty = [sc.sb(f"ty{i}", [128, 512], F32) for i in range(2)]
        it = 0
        for e in range(NE):
            w1 = w1r[e % 2]
            w2 = w2r[e % 2]
            if "w1b" in io:
                cb1 = io["w1b"][e // 4]
                cb2 = io["w2b"][e // 8]
                k.dma("sync", w1.v(), V(cb1, cb1.ap.rearrange("(e c p h) n -> e p c (h n)", e=4, p=128, h=2)[e % 4]))
                k.dma("sync", w2.v(), V(cb2, cb2.ap.rearrange("(e c p) n -> e p c n", e=8, p=128)[e % 8]))
            else:
                for h in range(2):
                    k.dma("gpsimd", w1[:, :, h * 1024:(h + 1) * 1024],
                          io["w1"].v()[e, :, h * 1024:(h + 1) * 1024].re("(c p) n -> p c n", p=128))
                k.dma("gpsimd", w2.v(), io["w2"].v()[e].re("(c p) n -> p c n", p=128))
            for bi, (st, N, col) in enumerate(blocks):
                a = actT[bi % 2]
                for fc in range(8):
                    pg_, pl_ = PS[(fc % 2) * 2], PS[(fc % 2) * 2 + 1]
                    for kc in range(8):
                        k.mm(pg_[:, :N], w1[:, kc, fc * 128:(fc + 1) * 128], hT[:, kc, st - g0:st - g0 + N], start=(kc == 0), stop=(kc == 7))
                    for kc in range(8):
                        k.mm(pl_[:, :N], w1[:, kc, 1024 + fc * 128:1024 + (fc + 1) * 128], hT[:, kc, st - g0:st - g0 + N], start=(kc == 0), stop=(kc == 7))
                    i2 = it % 2
                    it += 1
                    k.ts(g[i2][:, :N], pg_[:, :N], b1s[:, e * 16 + fc:e * 16 + fc + 1], SW_LIM, ALU.add, ALU.min)
                    k.act(sg[i2][:, :N], g[i2][:, :N], AF.Silu, scale=SW_ALPHA)
                    k.ts(l[i2][:, :N], pl_[:, :N], b1s[:, e * 16 + 8 + fc:e * 16 + 8 + fc + 1], SW_LIM, ALU.add, ALU.min)
                    k.ts(l[i2][:, :N], l[i2][:, :N], -SW_LIM, 1.0, ALU.max, ALU.add)
                    k.stt(a[:, fc, :N], l[i2][:, :N], 1.0 / SW_ALPHA, sg[i2][:, :N], ALU.mult, ALU.mult)
                pgb = PS[6]
                k.mm(pgb[:, :N], c["ident"][0:32, e:e + 1].bcast([32, 128]), gatesT[:, st - g0:st - g0 + N])
                for dc in range(8):
                    py = PS[4 + (dc % 2)]
                    for fc in range(8):
                        k.mm(py[:, :N], w2[:, fc, dc * 128:(dc + 1) * 128], a[:, fc, :N], start=(fc == 0), stop=(fc == 7))
                    t = ty[dc % 2]
                    k.act(t[:, :N], py[:, :N], AF.Identity, bias=b2s[:, e * 8 + dc:e * 8 + dc + 1])
                    k.tt(t[:, :N], t[:, :N], pgb[:, :N], ALU.mult)
                    k.tt(acc[:, dc, st - g0:st - g0 + N], acc[:, dc, st - g0:st - g0 + N], t[:, :N], ALU.add, eng="gpsimd")

    if g0 == 0:
        k.dbg("acc", acc.v(), list(acc.ap.shape))
    with k.scope() as sc:
        x1 = sc.sb("x1b", [128, 8, 512], F32)
        s = sc.sb("s2", [128, 8, 512], F32)
        xo = sc.sb("xo", [128, 8, 512], F32)
        tmp = {"sq": sc.sb("sq2", [128, 8, 512], F32), "mean": sc.sb("mean2", [128, 512], F32),
               "rstd": sc.sb("rstd2", [128, 512], F32), "t": sc.sb("tt2", [128, 512], F32)}
        t2 = sc.sb("t22", [128, 512], F32)
        for (st, N, col) in blocks:
            k.dma("sync", x1[:, :, :N], io["x1d"].v()[:, st:st + N].re("(c p) t -> p c t", p=128))
            for dc in range(8):
                k.act(t2[:, :N], acc[:, dc, st - g0:st - g0 + N], AF.Identity, scale=mv(5, dc, col))
                k.stt(s[:, dc, :N], x1[:, dc, :N], DN_ALPHA, t2[:, :N], ALU.mult, ALU.add)

            def o2(ci, tv, N=N):
                k.act(xo[:, ci, :N], tv, AF.Identity, scale=lnp[:, 8 + ci:9 + ci], bias=lnp[:, 24 + ci:25 + ci])
            ln_block(k, c, s, N, None, None, o2, tmp, PS[2], PS[3])
            k.dma("sync", io["out"].v()[:, st:st + N].re("(c p) t -> p c t", p=128), xo[:, :, :N])


def cast_weights(k, io, tag):
    w1f = io["w1"].ap.rearrange("e k (h n) -> (e k h) n", h=2)
    w2f = io["w2"].ap.rearrange("e k n -> (e k) n")
    w1b = k.dram(f"w1b_{tag}", [65536, 1024], BF16, kind="Internal")
    w2b = k.dram(f"w2b_{tag}", [32768, 1024], BF16, kind="Internal")
    io["w1b"] = k.split(w1b, [w1b.ap[i * 8192:(i + 1) * 8192, :] for i in range(8)], name=f"w1b_{tag}")
    io["w2b"] = k.split(w2b, [w2b.ap[i * 8192:(i + 1) * 8192, :] for i in range(4)], name=f"w2b_{tag}")
    for i in range(8):
        k.dma("gpsimd", io["w1b"][i].v(), V(io["w1"], w1f[i * 8192:(i + 1) * 8192, :]))
    for i in range(4):
        k.dma("gpsimd", io["w2b"][i].v(), V(io["w2"], w2f[i * 8192:(i + 1) * 8192, :]))


T_CTX = 256
T_LAT = 4096
T_ALL = T_CTX + T_LAT
A_BLOCKS = [(0, 256, 1)] + [(256 + 512 * i, 512, 0) for i in range(8)]


def compute_mod(k, PS, io, mlist, modv):
    with k.scope() as sc:
        scs = sc.sb("scs", [128, 8, 2], F32)
        bm = sc.sb("bm", [128, 48], F32)
        wm = [sc.sb(f"wm{i}", [128, 8, 512], F32) for i in range(4)]
        k.dma("sync", scs.v(), io["sc"].v().re("p (c j) -> p c j", j=2))
        k.dma("sync", bm.v(), io["b_mod"].v())
        k.act(scs.v(), scs.v(), AF.Silu)
        pieces = [(m, h) for m in mlist for h in range(2)]

        def load(i):
            m, h = pieces[i]
            c0 = m * 1024 + h * 512
            k.dma("sync", wm[i % 4].v(), io["w_mod"].v()[:, c0:c0 + 512].re("(c p) n -> p c n", p=128))
        for i in range(min(3, len(pieces))):
            load(i)
        for i, (m, h) in enumerate(pieces):
            if i + 3 < len(pieces):
                load(i + 3)
            w = wm[i % 4]
            for d4 in range(4):
                dc = h * 4 + d4
                ps = PS[dc % 2]
                for kc in range(8):
                    k.mm(ps[:, 0:2], w[:, kc, d4 * 128:(d4 + 1) * 128], scs[:, kc, :], start=(kc == 0), stop=(kc == 7))
                j = m * 8 + dc
                k.act(modv[:, j, :], ps[:, 0:2], AF.Identity, bias=bm[:, j:j + 1])


def in_proj(k, PS, io, NC, pT):
    nc = k.nc
    with k.scope() as sc:
        if "modv" in io:
            modv = io["modv"]
        else:
            modv = sc.sb("modvA", [128, 48, 2], F32)
            compute_mod(k, PS, io, [0, 1], modv)
        sc1 = sc.sb("sc1A", [128, 8, 2], F32)
        k.ts(sc1.v(), modv[:, 8:16, :], 1.0, None, ALU.add)
        hT = sc.sb("hTA", [128, 8, T_ALL], BF16)
        xb = [sc.sb(f"xbA{i}", [128, 8, 512], F32) for i in range(2)]
        for bi, (st, N, col) in enumerate(A_BLOCKS):
            x = xb[bi % 2]
            k.dma("sync", x[:, :, :N], io["xT"].v()[:, st:st + N].re("(c p) t -> p c t", p=128))
            for ci in range(8):
                k.act(hT[:, ci, st:st + N], x[:, ci, :N], AF.Identity, scale=sc1[:, ci, col:col + 1],
                      bias=modv[:, ci, col:col + 1], eng=("scalar" if ci % 2 == 0 else "scalar"))
        wr = [sc.sb(f"wA{i}", [128, 8, 128], BF16) for i in range(3)]
        ob = [sc.sb(f"obA{i}", [128, 512], F32) for i in range(4)]
        it = 0
        for c in range(NC):
            w = wr[c % 3]
            with nc.allow_non_contiguous_dma(reason="w_in col chunk"):
                k.dma("gpsimd", w.v(), io["w_in"].v()[:, c * 128:(c + 1) * 128].re("(c p) n -> p c n", p=128))
            for (st, N, col) in A_BLOCKS:
                ps = PS[it % 4]
                o = ob[it % 4]
                it += 1
                for kc in range(8):
                    k.mm(ps[:, :N], w[:, kc, :], hT[:, kc, st:st + N], start=(kc == 0), stop=(kc == 7))
                k.copy(o[:, :N], ps[:, :N], eng=("scalar" if it % 2 == 0 else "vector"))
                k.dma("sync", pT.v()[c * 128:(c + 1) * 128, st:st + N], o[:, :N])


def rglru(k, PS, io, pT, c_u0, c_g0, yT, y_row0):
    T = T_ALL
    SEG = [(0, T_CTX), (T_CTX, T_ALL)]
    with k.scope() as sc:
        cv = sc.sb("cv", [128, 10], F32)
        gb = sc.sb("gb", [128, 8], F32)
        lam = sc.sb("lam", [128, 4], F32)
        one = sc.sb("one", [128, 1], F32)
        k.memset(one.v(), 1.0)
        k.dma("sync", cv.v(), io["conv"].v())
        k.dma("sync", gb.v(), io["gb"].v())
        k.dma("sync", lam.v(), io["lam"].v())
        k.act(lam.v(), lam.v(), AF.Exp, scale=-1.0)
        k.act(lam.v(), lam.v(), AF.Ln, bias=one.v())
        k.ts(lam.v(), lam.v(), -8.0, None, ALU.mult)
        urs = [sc.sb(f"ur{i}", [128, T], F32) for i in range(2)]
        u = sc.sb("u", [128, T], F32)
        gts = [sc.sb(f"gt{i}", [128, T], F32) for i in range(2)]
        HT = T // 2

        def ldrows(dst, row0):
            for hf in range(2):
                k.dma("sync", dst[:, hf * HT:(hf + 1) * HT], pT.v()[row0:row0 + 128, hf * HT:(hf + 1) * HT])
        for cc_ in range(2):
            ldrows(urs[cc_], (c_u0 + cc_) * 128)
            ldrows(gts[cc_], (c_g0 + cc_) * 128)
        a = sc.sb("a", [128, T], F32)
        bx = sc.sb("bx", [128, T], F32)
        hs = sc.sb("hs", [128, T], F32)
        hb = sc.sb("hb", [128, T], F32)
        wbd = sc.sb("wbd", [128, 4, 128], F32)
        t1 = [sc.sb(f"t1_{i}", [128, 512], F32) for i in range(2)]
        t2 = [sc.sb(f"t2_{i}", [128, 512], F32) for i in range(2)]
        for cc in range(2):
            ur, gt = urs[cc], gts[cc]
            k.dma("sync", wbd.v(), io["gwbd"].v()[cc].re("d s p n -> p (d s) n"))
            w = lambda j: cv[:, cc * 5 + j:cc * 5 + j + 1]
            k.ts(u.v(), ur.v(), w(2), w(4), ALU.mult, ALU.add)
            for (s0, s1) in SEG:
                k.stt(u[:, s0 + 2:s1], ur[:, s0:s1 - 2], w(0), u[:, s0 + 2:s1], ALU.mult, ALU.add)
                k.stt(u[:, s0 + 1:s1], ur[:, s0:s1 - 1], w(1), u[:, s0 + 1:s1], ALU.mult, ALU.add)
                k.stt(u[:, s0:s1 - 1], ur[:, s0 + 1:s1], w(3), u[:, s0:s1 - 1], ALU.mult, ALU.add)
            k.act(ur.v(), gt.v(), AF.Square)
            k.ts(ur.v(), ur.v(), 0.044715, 1.0, ALU.mult, ALU.add, eng="gpsimd")
            k.tt(ur.v(), ur.v(), gt.v(), ALU.mult, eng="gpsimd")
            k.act(ur.v(), ur.v(), AF.Sigmoid, scale=1.5957691216057308)
            k.tt(gt.v(), gt.v(), ur.v(), ALU.mult, eng="gpsimd")
            for d in range(2):
                it = 0
                for (st, N, col) in A_BLOCKS:
                    pr, pi = PS[(it % 2) * 2], PS[(it % 2) * 2 + 1]
                    r_, i_ = t1[it % 2], t2[it % 2]
                    it += 1
                    k.mm(pr[:, :N], wbd[:, d * 2 + 0, :], u[:, st:st + N])
                    k.mm(pi[:, :N], wbd[:, d * 2 + 1, :], u[:, st:st + N])
                    k.act(r_[:, :N], pr[:, :N], AF.Sigmoid, bias=gb[:, cc * 4 + d * 2:cc * 4 + d * 2 + 1])
                    k.act(i_[:, :N], pi[:, :N], AF.Sigmoid, bias=gb[:, cc * 4 + d * 2 + 1:cc * 4 + d * 2 + 2])
                    k.act(a[:, st:st + N], r_[:, :N], AF.Exp, scale=lam[:, cc * 2 + d:cc * 2 + d + 1])
                    k.act(r_[:, :N], a[:, st:st + N], AF.Square)
                    k.ts(r_[:, :N], r_[:, :N], -1.0, 1.0, ALU.mult, ALU.add)
                    k.act(r_[:, :N], r_[:, :N], AF.Sqrt)
                    k.tt(i_[:, :N], i_[:, :N], r_[:, :N], ALU.mult)
                    k.tt(bx[:, st:st + N], i_[:, :N], u[:, st:st + N], ALU.mult)
                if d == 0:
                    k.op("vector", lambda e: e.tensor_tensor_scan(hs.ap, a.ap, bx.ap, 0.0, ALU.mult, ALU.add),
                         [a.v(), bx.v()], [hs.v()])
                else:
                    ra, rb_, rh = a.ap[:, 0:T_CTX][:, ::-1], bx.ap[:, 0:T_CTX][:, ::-1], hb.ap[:, 0:T_CTX][:, ::-1]
                    k.op("vector", lambda e: e.tensor_tensor_scan(rh, ra, rb_, 0.0, ALU.mult, ALU.add),
                         [a.v(), bx.v()], [hb.v()])
                    ra, rb_, rh = a.ap[:, T_CTX:T][:, ::-1], bx.ap[:, T_CTX:T][:, ::-1], hb.ap[:, T_CTX:T][:, ::-1]
                    k.op("vector", lambda e: e.tensor_tensor_scan(rh, ra, rb_, hb.ap[:, 0:1], ALU.mult, ALU.add),
                         [a.v(), bx.v(), hb.v()], [hb.v()])
            k.tt(hs.v(), hs.v(), hb.v(), ALU.add)
            k.tt(hs.v(), hs.v(), gt.v(), ALU.mult)
            k.dma("sync", yT.v()[y_row0 + cc * 128:y_row0 + (cc + 1) * 128, :], hs.v())


A_DECAY = -0.6065306597126334
GN_EPS_RWKV = 64e-5
CH = 64
RB = 256


def rwkv_prep(k, PS, c, io, pT, g, PRE, GBd):
    T = T_ALL
    with k.scope() as sc:
        rp = sc.sb("rp", [128, 16], F32)
        m42 = sc.sb("m42", [128, 6], F32)
        mus = sc.sb("mus", [128, 6, 7], F32)
        wBs = sc.sb("wBs", [128, 128], F32)
        aBs = sc.sb("aBs", [128, 128], F32)
        gBs = sc.sb("gBs", [128, 128], F32)
        k.dma("sync", rp.v(), io["rp"].v()[g])
        k.dma("sync", m42.v(), io["m42"].v())
        k.dma("sync", wBs.v(), io["wBs"].v()[g])
        k.dma("sync", aBs.v(), io["aBs"].v()[g])
        k.dma("sync", gBs.v(), io["gBs"].v()[g])
        for ty in range(6):
            k.ts(mus[:, ty, 0:1], rp[:, ty:ty + 1], -1.0, 1.0, ALU.mult, ALU.add)
            k.ts(mus[:, ty, 1:7], m42.v(), rp[:, ty:ty + 1], None, ALU.mult)
        pts = [sc.sb(f"ptmp{i}", [128, T], F32) for i in range(2)]
        z = [sc.sb(f"z{ty}", [128, T], F32) for ty in range(6)]
        chunk_ids = [g, 2 + g, 4 + g, 6, 7, 8]
        HT = T // 2

        def ldp(ty_):
            cid_ = chunk_ids[ty_]
            for hf in range(2):
                k.dma("sync", pts[ty_ % 2][:, hf * HT:(hf + 1) * HT], pT.v()[cid_ * 128:(cid_ + 1) * 128, hf * HT:(hf + 1) * HT])
        ldp(0)
        for ty in range(6):
            if ty + 1 < 6:
                ldp(ty + 1)
            pt = pts[ty % 2]
            zz = z[ty]
            eng = "vector"
            k.ts(zz.v(), pt.v(), mus[:, ty, 0:1], None, ALU.mult, eng=eng)
            pl = pt[:, T_CTX:T].re("p (r c) -> p r c", c=64)
            zl = zz[:, T_CTX:T].re("p (r c) -> p r c", c=64)
            k.stt(zl[:, :, 1:64], pl[:, :, 0:63], mus[:, ty, 1:2], zl[:, :, 1:64], ALU.mult, ALU.add)
            k.stt(zl[:, :, 0:63], pl[:, :, 1:64], mus[:, ty, 2:3], zl[:, :, 0:63], ALU.mult, ALU.add)
            k.stt(zl[:, 1:64, :], pl[:, 0:63, :], mus[:, ty, 3:4], zl[:, 1:64, :], ALU.mult, ALU.add)
            k.stt(zl[:, 0:63, :], pl[:, 1:64, :], mus[:, ty, 4:5], zl[:, 0:63, :], ALU.mult, ALU.add)
            k.stt(zz[:, 1:T_CTX], pt[:, 0:T_CTX - 1], mus[:, ty, 5:6], zz[:, 1:T_CTX], ALU.mult, ALU.add)
            k.stt(zz[:, 0:T_CTX - 1], pt[:, 1:T_CTX], mus[:, ty, 6:7], zz[:, 0:T_CTX - 1], ALU.mult, ALU.add)
        zr, zk, zv, zwd, zad, zg = z
        k.dma("sync", PRE.v()[0:128, :], zr.v())
        k.dma("sync", PRE.v()[128:256, :], zv.v())
        k.act(zwd.v(), zwd.v(), AF.Tanh)
        k.act(zg.v(), zg.v(), AF.Sigmoid)
        pt = pts[0]
        k.ts(pt.v(), zk.v(), rp[:, 10:11], None, ALU.mult)
        tb = [[sc.sb(f"tb{i}_{j}", [128, 512], F32) for j in range(2)] for i in range(10)]
        for bi, (st, N, col) in enumerate(A_BLOCKS):
            j2 = bi % 2
            t = [tb[i][j2] for i in range(10)]
            sl = slice(st, st + N)
            k.act(t[0][:, :N], pt[:, sl], AF.Square)
            k.mm(PS[0][:, :N], c["bones"].v(), t[0][:, :N])
            k.act(t[0][:, :N], PS[0][:, :N], AF.Sqrt)
            k.ts(t[0][:, :N], t[0][:, :N], 1e-12, None, ALU.max)
            k.recip(t[0][:, :N], t[0][:, :N])
            k.tt(t[0][:, :N], pt[:, sl], t[0][:, :N], ALU.mult)
            k.dma("sync", PRE.v()[256:384, sl], t[0][:, :N])
            k.mm(PS[1][:, :N], gBs.v(), zg[:, sl])
            k.copy(t[1][:, :N], PS[1][:, :N], eng="scalar")
            k.dma("sync", GBd.v()[0:128, sl], t[1][:, :N])
            for d in range(2):
                ds = slice(d * 64, (d + 1) * 64)
                pw, pa = PS[2 + d * 2], PS[3 + d * 2]
                k.mm(pw[:, :N], wBs[ds, :], zwd[ds, sl])
                k.mm(pa[:, :N], aBs[ds, :], zad[ds, sl])
                lw, av, kd, bb = t[2 + d * 3], t[8], t[3 + d * 3], t[4 + d * 3]
                k.act(lw[:, :N], pw[:, :N], AF.Sigmoid, bias=rp[:, 6 + d:7 + d])
                k.ts(lw[:, :N], lw[:, :N], A_DECAY, None, ALU.mult, eng="gpsimd")
                k.dma("sync", PRE.v()[(5 + 3 * d) * 128:(6 + 3 * d) * 128, sl], lw[:, :N])
                k.act(av[:, :N], pa[:, :N], AF.Sigmoid, bias=rp[:, 8 + d:9 + d])
                k.tt(bb[:, :N], t[0][:, :N], av[:, :N], ALU.mult)
                k.dma("sync", PRE.v()[(4 + 3 * d) * 128:(5 + 3 * d) * 128, sl], bb[:, :N])
                k.ts(kd[:, :N], av[:, :N], -1.0, rp[:, 11:12], ALU.add, ALU.mult)
                k.stt(kd[:, :N], kd[:, :N], 1.0, zk[:, sl], ALU.add, ALU.mult)
                k.dma("sync", PRE.v()[(3 + 3 * d) * 128:(4 + 3 * d) * 128, sl], kd[:, :N])
            k.tt(t[9][:, :N], t[3][:, :N], t[6][:, :N], ALU.add)
            k.stt(t[9][:, :N], zr[:, sl], rp[:, 12:13], t[9][:, :N], ALU.mult, ALU.mult)
            k.mm(PS[6][:, :N], c["bones"].v(), t[9][:, :N])
            k.tt(t[1][:, :N], PS[6][:, :N], zv[:, sl], ALU.mult)
            k.dma("sync", GBd.v()[128:256, sl], t[1][:, :N])


import os
USTEP = int(os.environ.get("USTEP", "5"))
NCH = int(os.environ.get("NCH", "4"))
NBK = int(os.environ.get("NBK", "100"))


class Chain:
    pass


def rwkv_scan(k, PSq, c, PREs, wkv):
    T = T_ALL
    NBLK = T // RB
    chains = []
    sc = k.scope()
    sc.__enter__()
    for g in range(2):
        for d in range(2):
            ch = Chain()
            ch.g, ch.d = g, d
            nm = f"c{g}{d}"
            ch.H = sc.sb(nm + "H", [128, 128], F32)
            k.memset(ch.H.v(), 0.0)
            ch.blk = [sc.sb(nm + f"blk{i}", [128, 6, RB], F32) for i in range(2)]
            ch.Lb = [sc.sb(nm + f"L{i}", [128, 4, RB], F32) for i in range(2)]
            names = ["kapT", "rT", "ktT", "btT", "khT", "bhT", "vT", "kh", "nbh", "V", "AkkT", "ArkT", "nArbT", "W", "U"]
            ch.t = {n: sc.sb(nm + n, [128, 128], F32) for n in names}
            for n in ["kapT", "rT", "ktT", "btT", "khT", "bhT", "vT"]:
                k.memset(ch.t[n].v(), 0.0, eng="gpsimd")
            ch.P = [sc.sb(nm + f"P{i}", [128, 128], F32) for i in range(2)]
            ch.PT = [sc.sb(nm + f"PT{i}", [128, 128], F32) for i in range(2)]
            ch.R = [sc.sb(nm + f"R{i}", [128, 128], F32) for i in range(2)]
            if d == 0:
                ch.order = list(range(NBLK))
            else:
                ch.order = [0] + list(range(NBLK - 1, 0, -1))
            chains.append(ch)
    psn = [0]

    def ps():
        p = PSq[psn[0] % len(PSq)]
        psn[0] += 1
        return p

    def load_block(ch, i):
        b = ch.order[i]
        buf = ch.blk[i % 2]
        rows = [0, 1, 2, 3 + 3 * ch.d, 4 + 3 * ch.d, 5 + 3 * ch.d]
        for j, r in enumerate(rows):
            k.dma("sync", buf[:, j, :], PREs[ch.g].v()[r * 128:(r + 1) * 128, b * RB:(b + 1) * RB])
        Lb = ch.Lb[i % 2]
        lw = buf[:, 5, :]
        if ch.d == 0:
            k.op("vector", lambda e: e.tensor_tensor_scan(Lb.ap[:, 0, :], c["rmask"].ap[:, 0:RB], lw.ap, 0.0, ALU.mult, ALU.add),
                 [lw, c["rmask"].v()], [Lb.v()])
        else:
            k.op("vector", lambda e: e.tensor_tensor_scan(Lb.ap[:, 0, :][:, ::-1], c["rmask"].ap[:, 0:RB], lw.ap[:, ::-1], 0.0, ALU.mult, ALU.add),
                 [lw, c["rmask"].v()], [Lb.v()])
        k.tt(Lb[:, 1, :], Lb[:, 0, :], lw, ALU.subtract, eng="gpsimd")
        k.act(Lb[:, 1, :], Lb[:, 1, :], AF.Exp)
        k.act(Lb[:, 2, :], Lb[:, 0, :], AF.Exp)
        k.act(Lb[:, 3, :], Lb[:, 0, :], AF.Exp, scale=-1.0)

    def unit(ch, i, j):
        d = ch.d
        buf = ch.blk[i % 2]
        Lb = ch.Lb[i % 2]
        cj = j if d == 0 else (RB // CH - 1 - j)
        c0 = cj * CH
        cs = slice(c0, c0 + CH)
        b = ch.order[i]
        tok0 = b * RB + c0
        last = c0 + CH - 1 if d == 0 else c0
        t = ch.t
        eLC = Lb[:, 2, last:last + 1]
        for h in range(2):
            ps_ = slice(h * 64, (h + 1) * 64)
            e1 = "vector" if h == 0 else "gpsimd"
            e2 = "gpsimd" if h == 0 else "vector"
            k.tt(t["kapT"][ps_, ps_], buf[ps_, 2, cs], Lb[ps_, 1, cs], ALU.mult, eng=e1)
            k.tt(t["rT"][ps_, ps_], buf[ps_, 0, cs], Lb[ps_, 2, cs], ALU.mult, eng=e2)
            k.tt(t["ktT"][ps_, ps_], buf[ps_, 3, cs], Lb[ps_, 3, cs], ALU.mult, eng=e1)
            k.tt(t["btT"][ps_, ps_], buf[ps_, 4, cs], Lb[ps_, 3, cs], ALU.mult, eng=e2)
            k.ts(t["khT"][ps_, ps_], t["ktT"][ps_, ps_], eLC[ps_, :], None, ALU.mult, eng=e1)
            k.ts(t["bhT"][ps_, ps_], t["btT"][ps_, ps_], eLC[ps_, :], None, ALU.mult, eng=e2)
            k.copy(t["vT"][ps_, ps_], buf[ps_, 1, cs], eng=e1)
        ident = c["ident"].v()
        if USTEP < 2:
            return
        p1, p2, p3 = ps(), ps(), ps()
        USUB = int(os.environ.get("USUB", "9"))
        k.tr(p1.v(), t["khT"].v(), ident)
        if USUB >= 2:
            k.tr(p2.v(), t["bhT"].v(), ident)
            k.tr(p3.v(), t["vT"].v(), ident)
        if USUB >= 3:
            k.copy(t["kh"].v(), p1.v(), eng="scalar")
        if USUB >= 4:
            k.act(t["nbh"].v(), p2.v(), AF.Identity, scale=-1.0)
            k.copy(t["V"].v(), p3.v(), eng="scalar")
        if USTEP < 3:
            return
        if d == 0:
            ms, msT, mi, nms, nmsT, nmi = c["MUs"], c["MLs"], c["MUi"], c["nMUs"], c["nMLs"], c["nMUi"]
        else:
            ms, msT, mi, nms, nmsT, nmi = c["MLs"], c["MUs"], c["MLi"], c["nMLs"], c["nMUs"], c["nMLi"]
        q1, q2, q3, q4, q5 = ps(), ps(), ps(), ps(), ps()
        k.mm(q1.v(), t["btT"].v(), t["kapT"].v())
        k.mm(q2.v(), t["kapT"].v(), t["btT"].v())
        k.mm(q3.v(), t["ktT"].v(), t["kapT"].v())
        k.mm(q4.v(), t["ktT"].v(), t["rT"].v())
        k.mm(q5.v(), t["btT"].v(), t["rT"].v())
        P, PT, R = ch.P, ch.PT, ch.R
        k.tt(P[0].v(), q1.v(), nms.v(), ALU.mult)
        k.tt(PT[0].v(), q2.v(), nmsT.v(), ALU.mult)
        k.tt(t["AkkT"].v(), q3.v(), ms.v(), ALU.mult)
        k.tt(t["ArkT"].v(), q4.v(), mi.v(), ALU.mult)
        k.tt(t["nArbT"].v(), q5.v(), nmi.v(), ALU.mult)
        k.tt(R[0].v(), P[0].v(), ident, ALU.add, eng="gpsimd")
        cur = 0
        if USTEP < 4:
            return
        for it in range(5):
            nx = 1 - cur
            a1, a2 = ps(), ps()
            k.mm(a1.v(), PT[cur].v(), P[cur].v())
            k.mm(a2.v(), P[cur].v(), PT[cur].v())
            k.copy(PT[nx].v(), a2.v(), eng="scalar")
            k.copy(P[nx].v(), a1.v(), eng="vector")
            a3 = ps()
            k.mm(a3.v(), PT[nx].v(), R[cur].v())
            k.tt(R[nx].v(), R[cur].v(), a3.v(), ALU.add)
            cur = nx
        Tt = R[cur]
        if USTEP < 5:
            return
        w_ps = ps()
        k.mm(w_ps.v(), t["kapT"].v(), ch.H.v(), start=True, stop=False)
        k.mm(w_ps.v(), t["AkkT"].v(), t["V"].v(), start=False, stop=True)
        k.copy(t["W"].v(), w_ps.v(), eng="scalar")
        u_ps = ps()
        k.mm(u_ps.v(), Tt.v(), t["W"].v())
        k.copy(t["U"].v(), u_ps.v(), eng="vector")
        o_ps = ps()
        k.mm(o_ps.v(), ch.H.v(), t["rT"].v(), start=True, stop=False)
        k.mm(o_ps.v(), t["V"].v(), t["ArkT"].v(), start=False, stop=False)
        k.mm(o_ps.v(), t["U"].v(), t["nArbT"].v(), start=False, stop=True)
        h_ps = ps()
        k.mm(h_ps.v(), t["kh"].v(), t["V"].v(), start=True, stop=False)
        k.mm(h_ps.v(), t["nbh"].v(), t["U"].v(), start=False, stop=True)
        wk = wkv[ch.g]
        for h in range(2):
            ps_ = slice(h * 64, (h + 1) * 64)
            k.tt(wk[ps_, tok0:tok0 + CH], wk[ps_, tok0:tok0 + CH], o_ps[ps_, ps_], ALU.add)
        k.stt(ch.H.v(), ch.H.v(), eLC, h_ps.v(), ALU.mult, ALU.add)

    NB = min(T // RB, NBK)
    chains = chains[:NCH]
    for ch in chains:
        load_block(ch, 0)
    for i in range(NB):
        for ch in chains:
            if i + 1 < NB:
                load_block(ch, i + 1)
        for j in range(RB // CH):
            for ch in chains:
                unit(ch, i, j)
    sc.__exit__(None, None, None)


def rwkv_finish(k, PS, c, io, g, GBd, wkv, yT, row0):
    T = T_ALL
    with k.scope() as sc:
        rp = sc.sb("rpf", [128, 16], F32)
        k.dma("sync", rp.v(), io["rp"].v()[g])
        eps = sc.sb("epsf", [128, 1], F32)
        k.memset(eps.v(), GN_EPS_RWKV)
        tb = [[sc.sb(f"tf{i}_{j}", [128, 512], F32) for j in range(2)] for i in range(5)]
        for bi, (st, N, col) in enumerate(A_BLOCKS):
            sl = slice(st, st + N)
            t = [tb[i][bi % 2] for i in range(5)]
            k.dma("sync", t[3][:, :N], GBd.v()[0:128, sl])
            k.dma("sync", t[4][:, :N], GBd.v()[128:256, sl])
            w = wkv[g]
            k.mm(PS[0][:, :N], c["bones64"].v(), w[:, sl])
            k.act(t[0][:, :N], w[:, sl], AF.Square)
            k.mm(PS[1][:, :N], c["bones64"].v(), t[0][:, :N])
            k.copy(t[1][:, :N], PS[0][:, :N], eng="scalar")
            k.tt(t[2][:, :N], t[1][:, :N], t[1][:, :N], ALU.mult)
            k.tt(t[2][:, :N], PS[1][:, :N], t[2][:, :N], ALU.subtract)
            k.act(t[2][:, :N], t[2][:, :N], AF.Sqrt, bias=eps.v())
            k.recip(t[2][:, :N], t[2][:, :N])
            k.tt(t[0][:, :N], w[:, sl], t[1][:, :N], ALU.subtract)
            k.tt(t[0][:, :N], t[0][:, :N], t[2][:, :N], ALU.mult)
            k.act(t[0][:, :N], t[0][:, :N], AF.Identity, scale=rp[:, 13:14], bias=rp[:, 14:15])
            k.tt(t[0][:, :N], t[0][:, :N], t[4][:, :N], ALU.add)
            k.tt(t[0][:, :N], t[0][:, :N], t[3][:, :N], ALU.mult)
            k.dma("sync", yT.v()[row0:row0 + 128, sl], t[0][:, :N])


def rwkv_scan2(k, PSb, c, PREs, wkv):
    T = T_ALL
    NBLK = T // RB
    UPB = RB // CH
    sc = k.scope()
    sc.__enter__()
    chains = []
    for g in range(2):
        for d in range(2):
            ch = Chain()
            ch.g, ch.d = g, d
            nm = f"s{g}{d}"
            ch.H = sc.sb(nm + "H", [128, 128], F32)
            k.memset(ch.H.v(), 0.0)
            ch.blk = [sc.sb(nm + f"blk{i}", [128, 6, RB], F32) for i in range(2)]
            ch.Lb = [sc.sb(nm + f"L{i}", [128, 4, RB], F32) for i in range(2)]
            ch.kapT = [sc.sb(nm + f"kapT{i}", [128, 128], F32) for i in range(2)]
            ch.rT = [sc.sb(nm + f"rT{i}", [128, 128], F32) for i in range(2)]
            ch.t = {n: sc.sb(nm + n, [128, 128], F32) for n in ["ktT", "btT", "khT", "nbhT", "vT", "W", "U"]}
            for tl in ch.kapT + ch.rT + [ch.t[n] for n in ["ktT", "btT", "khT", "nbhT", "vT"]]:
                k.memset(tl.v(), 0.0, eng="gpsimd")
            ch.TRS = [sc.sb(nm + f"TRS{i}", [128, 3, 128], F32) for i in range(2)]
            ch.AAA = [sc.sb(nm + f"AAA{i}", [128, 3, 128], F32) for i in range(2)]
            ch.PP = [sc.sb(nm + f"PP{i}", [128, 2, 128], F32) for i in range(2)]
            ch.R = [[sc.sb(nm + f"R{s_}{i}", [128, 128], F32) for i in range(2)] for s_ in range(2)]
            ch.order = list(range(NBLK)) if d == 0 else [0] + list(range(NBLK - 1, 0, -1))
            chains.append(ch)
    psn = [0]

    def ps():
        p = PSb[psn[0] % len(PSb)]
        psn[0] += 1
        return p

    def load_block(ch, i):
        b = ch.order[i]
        buf = ch.blk[i % 2]
        rows = [0, 1, 2, 3 + 3 * ch.d, 4 + 3 * ch.d, 5 + 3 * ch.d]
        for j, r in enumerate(rows):
            k.dma("sync", buf[:, j, :], PREs[ch.g].v()[r * 128:(r + 1) * 128, b * RB:(b + 1) * RB])
        Lb = ch.Lb[i % 2]
        lw = buf[:, 5, :]
        if ch.d == 0:
            k.op("vector", lambda e: e.tensor_tensor_scan(Lb.ap[:, 0, :], c["rmask"].ap[:, 0:RB], lw.ap, 0.0, ALU.mult, ALU.add),
                 [lw, c["rmask"].v()], [Lb.v()])
        else:
            k.op("vector", lambda e: e.tensor_tensor_scan(Lb.ap[:, 0, :][:, ::-1], c["rmask"].ap[:, 0:RB], lw.ap[:, ::-1], 0.0, ALU.mult, ALU.add),
                 [lw, c["rmask"].v()], [Lb.v()])
        k.tt(Lb[:, 1, :], Lb[:, 0, :], lw, ALU.subtract)
        k.act(Lb[:, 1, :], Lb[:, 1, :], AF.Exp)
        k.act(Lb[:, 2, :], Lb[:, 0, :], AF.Exp)
        k.act(Lb[:, 3, :], Lb[:, 0, :], AF.Exp, scale=-1.0)

    def geom(ch, n):
        i, j = n // UPB, n % UPB
        d = ch.d
        cj = j if d == 0 else (UPB - 1 - j)
        c0 = cj * CH
        last = c0 + CH - 1 if d == 0 else c0
        return i, slice(c0, c0 + CH), ch.order[i] * RB + c0, last

    ident = c["ident"].v()

    def A1(ch, n):
        i, cs, tok0, last = geom(ch, n)
        s_ = n % 2
        buf, Lb, t = ch.blk[i % 2], ch.Lb[i % 2], ch.t
        eLC = Lb[:, 2, last:last + 1]
        for h in range(2):
            p_ = slice(h * 64, (h + 1) * 64)
            e1 = "vector" if h == 0 else "gpsimd"
            e2 = "gpsimd" if h == 0 else "vector"
            k.tt(ch.kapT[s_][p_, p_], buf[p_, 2, cs], Lb[p_, 1, cs], ALU.mult, eng=e1)
            k.tt(ch.rT[s_][p_, p_], buf[p_, 0, cs], Lb[p_, 2, cs], ALU.mult, eng=e2)
            k.tt(t["ktT"][p_, p_], buf[p_, 3, cs], Lb[p_, 3, cs], ALU.mult, eng=e1)
            k.tt(t["btT"][p_, p_], buf[p_, 4, cs], Lb[p_, 3, cs], ALU.mult, eng=e2)
            k.ts(t["khT"][p_, p_], t["ktT"][p_, p_], eLC[p_, :], None, ALU.mult)
            k.ts(t["nbhT"][p_, p_], t["btT"][p_, p_], eLC[p_, :], -1.0, ALU.mult, ALU.mult)
            k.copy(t["vT"][p_, p_], buf[p_, 1, cs], eng=e1)

    def A2(ch, n):
        s_ = n % 2
        t = ch.t
        ch.pT3, ch.pPP, ch.pA3 = ps(), ps(), ps()
        k.tr(ch.pT3[:, 0:128], t["khT"].v(), ident)
        k.tr(ch.pT3[:, 128:256], t["nbhT"].v(), ident)
        k.tr(ch.pT3[:, 256:384], t["vT"].v(), ident)
        k.mm(ch.pPP[:, 0:128], t["btT"].v(), ch.kapT[s_].v())
        k.mm(ch.pPP[:, 128:256], ch.kapT[s_].v(), t["btT"].v())
        k.mm(ch.pA3[:, 0:128], t["ktT"].v(), ch.kapT[s_].v())
        k.mm(ch.pA3[:, 128:256], t["ktT"].v(), ch.rT[s_].v())
        k.mm(ch.pA3[:, 256:384], t["btT"].v(), ch.rT[s_].v())

    def A3(ch, n):
        s_ = n % 2
        f = "f" if ch.d == 0 else "b"
        k.copy(ch.TRS[s_].v().re("p a b -> p (a b)"), ch.pT3[:, 0:384], eng="scalar")
        k.tt(ch.PP[0].v().re("p a b -> p (a b)"), ch.pPP[:, 0:256], c["np2" + f].v(), ALU.mult)
        k.tt(ch.AAA[s_].v().re("p a b -> p (a b)"), ch.pA3[:, 0:384], c["m3" + f].v(), ALU.mult)
        k.tt(ch.R[s_][0].v(), ch.PP[0][:, 0, :], ident, ALU.add, eng="gpsimd")

    def Na(ch, n, it):
        cur = it % 2
        ch.pN = ps()
        k.mm(ch.pN[:, 0:128], ch.PP[cur][:, 1, :], ch.PP[cur][:, 0, :])
        k.mm(ch.pN[:, 128:256], ch.PP[cur][:, 0, :], ch.PP[cur][:, 1, :])

    def Nb(ch, n, it):
        nx = 1 - it % 2
        k.copy(ch.PP[nx].v().re("p a b -> p (a b)"), ch.pN[:, 0:256], eng=("scalar" if (it + ch.g) % 2 == 0 else "vector"))

    def Nc(ch, n, it):
        s_ = n % 2
        nx = 1 - it % 2
        ch.pR = ps()
        k.mm(ch.pR[:, 0:128], ch.PP[nx][:, 1, :], ch.R[s_][it % 2].v())

    def Nd(ch, n, it):
        s_ = n % 2
        k.tt(ch.R[s_][1 - it % 2].v(), ch.R[s_][it % 2].v(), ch.pR[:, 0:128], ALU.add)

    def B1(ch, n):
        s_ = n % 2
        ch.pW = ps()
        k.mm(ch.pW[:, 0:128], ch.kapT[s_].v(), ch.H.v(), start=True, stop=False)
        k.mm(ch.pW[:, 0:128], ch.AAA[s_][:, 0, :], ch.TRS[s_][:, 2, :], start=False, stop=True)

    def B2(ch, n):
        k.copy(ch.t["W"].v(), ch.pW[:, 0:128], eng="scalar")

    def B3(ch, n):
        s_ = n % 2
        ch.pU = ps()
        k.mm(ch.pU[:, 0:128], ch.R[s_][1].v(), ch.t["W"].v())

    def B4(ch, n):
        k.copy(ch.t["U"].v(), ch.pU[:, 0:128], eng="vector")

    def B5(ch, n):
        s_ = n % 2
        V_ = ch.TRS[s_][:, 2, :]
        ch.pO, ch.pH = ps(), ps()
        k.mm(ch.pO[:, 0:128], ch.H.v(), ch.rT[s_].v(), start=True, stop=False)
        k.mm(ch.pO[:, 0:128], V_, ch.AAA[s_][:, 1, :], start=False, stop=False)
        k.mm(ch.pO[:, 0:128], ch.t["U"].v(), ch.AAA[s_][:, 2, :], start=False, stop=True)
        k.mm(ch.pH[:, 0:128], ch.TRS[s_][:, 0, :], V_, start=True, stop=False)
        k.mm(ch.pH[:, 0:128], ch.TRS[s_][:, 1, :], ch.t["U"].v(), start=False, stop=True)

    def B6(ch, n):
        i, cs, tok0, last = geom(ch, n)
        eLC = ch.Lb[i % 2][:, 2, last:last + 1]
        wk = wkv[ch.g]
        for h in range(2):
            p_ = slice(h * 64, (h + 1) * 64)
            k.tt(wk[p_, tok0:tok0 + CH], wk[p_, tok0:tok0 + CH], ch.pO[p_, p_], ALU.add)
        k.stt(ch.H.v(), ch.H.v(), eLC, ch.pH[:, 0:128], ALU.mult, ALU.add)

    NU = NBLK * UPB

    def partA(n):
        if n == 0:
            for ch in chains:
                load_block(ch, 0)
        if n % UPB == 1:
            i = n // UPB
            for ch in chains:
                if i + 1 < NBLK:
                    load_block(ch, i + 1)
        for ch in chains:
            A1(ch, n)
        for ch in chains:
            A2(ch, n)
            A3(ch, n)
        for it in range(5):
            for st in (Na, Nb, Nc, Nd):
                for ch in chains:
                    st(ch, n, it)

    def partB(n):
        for st in (B1, B2, B3, B4, B5, B6):
            for ch in chains:
                st(ch, n)

    partA(0)
    for n in range(NU):
        if n + 1 < NU:
            partA(n + 1)
        partB(n)
    sc.__exit__(None, None, None)


LN_EPS = 1e-5
CK = 128
NCHUNK = T_ALL // CK
KSCALE = 128 ** -0.5
NEG = -1.0e30
RQ, RQS, RK, RKS, RV, RG = 0, 2, 4, 6, 8, 10
MQ, MK, MV, MO, GF0, GI0, GF1, GI1 = 12, 14, 16, 18, 20, 21, 22, 23
NC_ODD = 24
ORDER = {0: list(range(NCHUNK)), 1: [1, 0] + list(range(NCHUNK - 1, 1, -1))}


def rope_pass(k, io, pT, QK):
    with k.scope() as sc:
        a = [sc.sb(f"ra{i}", [128, 512], F32) for i in range(2)]
        b = [sc.sb(f"rb{i}", [128, 512], F32) for i in range(2)]
        cc = [sc.sb(f"rcc{i}", [128, 512], F32) for i in range(2)]
        ss = [sc.sb(f"rss{i}", [128, 512], F32) for i in range(2)]
        it = 0
        for (st, N, col) in A_BLOCKS:
            sl = slice(st, st + N)
            i2 = it % 2
            it += 1
            if col == 0:
                k.dma("sync", cc[i2][:, :N], io["ropeC"].v()[:, st - T_CTX:st - T_CTX + N])
                k.dma("sync", ss[i2][:, :N], io["ropeS"].v()[:, st - T_CTX:st - T_CTX + N])
            for hh in range(2):
                for (src, swp, dst, scale) in [(RQ + hh, RQS + hh, 0 + hh, 1.0), (RK + hh, RKS + hh, 2 + hh, KSCALE)]:
                    ta, tb_ = a[(hh) % 2], b[(hh) % 2]
                    k.dma("sync", ta[:, :N], pT.v()[src * 128:(src + 1) * 128, sl])
                    if col == 0:
                        k.dma("sync", tb_[:, :N], pT.v()[swp * 128:(swp + 1) * 128, sl])
                        k.tt(ta[:, :N], ta[:, :N], cc[i2][:, :N], ALU.mult)
                        k.tt(tb_[:, :N], tb_[:, :N], ss[i2][:, :N], ALU.mult, eng="gpsimd")
                        k.tt(ta[:, :N], ta[:, :N], tb_[:, :N], ALU.add)
                    if scale != 1.0:
                        k.ts(ta[:, :N], ta[:, :N], scale, None, ALU.mult)
                    k.dma("sync", QK.v()[dst * 128:(dst + 1) * 128, sl], ta[:, :N])
                ta = a[hh % 2]
                k.dma("sync", ta[:, :N], pT.v()[(MK + hh) * 128:(MK + hh + 1) * 128, sl])
                k.ts(ta[:, :N], ta[:, :N], KSCALE, None, ALU.mult)
                k.dma("sync", QK.v()[(4 + hh) * 128:(5 + hh) * 128, sl], ta[:, :N])


def retention(k, PS, c, io, pT, QK, oacc):
    sc = k.scope()
    sc.__enter__()
    lg = sc.sb("lgc", [128, 4], F32)
    k.dma("sync", lg.v(), io["lgc"].v())
    chains = []
    for hh in range(2):
        for d in range(2):
            ch = type("C", (), {})()
            ch.hh, ch.d = hh, d
            nm = f"r{hh}{d}"
            l = lg[:, hh * 2 + d:hh * 2 + d + 1]
            ch.Dm = sc.sb(nm + "Dm", [128, 128], F32)
            ch.XI = sc.sb(nm + "XI", [128, 128], F32)
            ch.zg = sc.sb(nm + "zg", [128, 2], F32)
            k.act(ch.Dm.v(), c["ABSD"].v(), AF.Exp, scale=l)
            k.tt(ch.Dm.v(), ch.Dm.v(), (c["MUi"] if d == 0 else c["MLi"]).v(), ALU.mult)
            k.act(ch.XI.v(), (c["POS1F"] if d == 0 else c["POS1B"]).v(), AF.Exp, scale=l)
            k.act(ch.zg[:, 0:1], c["ZP"][:, (0 if d == 0 else 1):(1 if d == 0 else 2)], AF.Exp, scale=l)
            k.act(ch.zg[:, 1:2], c["ZP"][:, 2:3], AF.Exp, scale=l)
            ch.R = sc.sb(nm + "R", [128, 128], F32)
            k.memset(ch.R.v(), 0.0)
            ch.q = [sc.sb(nm + f"q{i}", [128, 128], F32) for i in range(2)]
            ch.k = [sc.sb(nm + f"k{i}", [128, 128], F32) for i in range(2)]
            ch.v = [sc.sb(nm + f"v{i}", [128, 128], F32) for i in range(2)]
            ch.Vt = [sc.sb(nm + f"Vt{i}", [128, 128], F32) for i in range(2)]
            ch.Kz = [sc.sb(nm + f"Kz{i}", [128, 128], F32) for i in range(2)]
            ch.St = [sc.sb(nm + f"St{i}", [128, 128], F32) for i in range(2)]
            ch.qx = [sc.sb(nm + f"qx{i}", [128, 128], F32) for i in range(2)]
            chains.append(ch)
    psn = [0]

    def ps():
        p = PS[psn[0] % len(PS)]
        psn[0] += 1
        return p

    def load(ch, i):
        n = ORDER[ch.d][i]
        sl = slice(n * CK, (n + 1) * CK)
        k.dma("sync", ch.q[i % 2].v(), QK.v()[(0 + ch.hh) * 128:(1 + ch.hh) * 128, sl])
        k.dma("sync", ch.k[i % 2].v(), QK.v()[(2 + ch.hh) * 128:(3 + ch.hh) * 128, sl])
        k.dma("sync", ch.v[i % 2].v(), pT.v()[(RV + ch.hh) * 128:(RV + ch.hh + 1) * 128, sl])

    ident = c["ident"].v()

    def R1(ch, i):
        s_ = i % 2
        q, kk, v = ch.q[s_], ch.k[s_], ch.v[s_]
        p1, p2, p3 = ps(), ps(), ps()
        k.tr(p1[:, 0:128], v.v(), ident)
        k.tr(p2[:, 0:128], kk.v(), ident)
        k.mm(p3[:, 0:128], kk.v(), q.v())
        k.copy(ch.Vt[s_].v(), p1[:, 0:128], eng="scalar")
        k.ts(ch.Kz[s_].v(), p2[:, 0:128], ch.zg[:, 0:1], None, ALU.mult)
        k.tt(ch.St[s_].v(), p3[:, 0:128], ch.Dm.v(), ALU.mult)
        k.tt(ch.qx[s_].v(), q.v(), ch.XI.v(), ALU.mult, eng="gpsimd")

    def R2a(ch, i):
        s_ = i % 2
        ch.po, ch.pr = ps(), ps()
        k.mm(ch.po[:, 0:128], ch.Vt[s_].v(), ch.St[s_].v(), start=True, stop=False)
        k.mm(ch.po[:, 0:128], ch.R.v(), ch.qx[s_].v(), start=False, stop=True)
        k.mm(ch.pr[:, 0:128], ch.Kz[s_].v(), ch.Vt[s_].v())

    def R2b(ch, i):
        n = ORDER[ch.d][i]
        sl = slice(n * CK, (n + 1) * CK)
        oa = oacc[ch.hh]
        k.tt(oa[:, sl], oa[:, sl], ch.po[:, 0:128], ALU.add)
        k.stt(ch.R.v(), ch.R.v(), ch.zg[:, 1:2], ch.pr[:, 0:128], ALU.mult, ALU.add)

    for ch in chains:
        load(ch, 0)
        load(ch, 1)
    for ch in chains:
        R1(ch, 0)
    for i in range(NCHUNK):
        if i + 1 < NCHUNK:
            for ch in chains:
                R1(ch, i + 1)
        for ch in chains:
            R2a(ch, i)
        for ch in chains:
            R2b(ch, i)
            if i + 2 < NCHUNK:
                load(ch, i + 2)
    sc.__exit__(None, None, None)


def gn_finish(k, PS, c, gnp, oacc_h, gate_src, gate_func, yT, row0, pre_scale=None):
    with k.scope() as sc:
        tb = [[sc.sb(f"gf{i}_{j}", [128, 512], F32) for j in range(2)] for i in range(4)]
        for bi, (st, N, col) in enumerate(A_BLOCKS):
            sl = slice(st, st + N)
            t = [tb[i][bi % 2] for i in range(4)]
            gate_src(sl, N, t[3])
            k.act(t[3][:, :N], t[3][:, :N], gate_func)
            w = oacc_h
            k.mm(PS[0][:, :N], c["ones128"].v(), w[:, sl])
            k.act(t[0][:, :N], w[:, sl], AF.Square)
            k.mm(PS[1][:, :N], c["ones128"].v(), t[0][:, :N])
            k.copy(t[1][:, :N], PS[0][:, :N], eng="scalar")
            k.tt(t[2][:, :N], t[1][:, :N], t[1][:, :N], ALU.mult)
            k.tt(t[2][:, :N], PS[1][:, :N], t[2][:, :N], ALU.subtract)
            k.act(t[2][:, :N], t[2][:, :N], AF.Sqrt, bias=c["eps"].v())
            k.recip(t[2][:, :N], t[2][:, :N])
            k.tt(t[0][:, :N], w[:, sl], t[1][:, :N], ALU.subtract)
            k.tt(t[0][:, :N], t[0][:, :N], t[2][:, :N], ALU.mult)
            k.act(t[0][:, :N], t[0][:, :N], AF.Identity, scale=gnp[:, 0:1], bias=gnp[:, 1:2])
            k.tt(t[0][:, :N], t[0][:, :N], t[3][:, :N], ALU.mult)
            k.dma("sync", yT.v()[row0:row0 + 128, sl], t[0][:, :N])


NC_ = NCHUNK


def mlstm(k, PS, c, io, pT, QK, hacc):
    T = T_ALL
    sc = k.scope()
    sc.__enter__()
    gbias = sc.sb("gbias", [128, 4], F32)
    k.dma("sync", gbias.v(), io["gbias"].v())
    one = sc.sb("m_one", [128, 1], F32)
    k.memset(one.v(), 1.0)
    D = {}
    for d in range(2):
      for hh in range(2):
        o = type("D", (), {})()
        nm = f"m{d}{hh}"
        NCK = [NC_, CK]
        x = sc.sb(nm + "x", NCK, F32)
        t1 = sc.sb(nm + "t1", NCK, F32)
        t2 = sc.sb(nm + "t2", NCK, F32)
        bb = sc.sb(nm + "b", NCK, F32)
        cm = sc.sb(nm + "cm", NCK, F32)
        o.RQ = sc.sb(nm + "RQ", [NC_, 3, CK], F32)
        o.CW = sc.sb(nm + "CW", [NC_, 2, CK], F32)
        rows = sc.sb(nm + "rows", [1, 6, NC_], F32)
        colsb = sc.sb(nm + "colsb", [NC_, 4], F32)
        o.SOB = sc.sb(nm + "SOB", [128, NC_], F32)
        gb = sc.sb(nm + "gb", [NC_, 2], F32)
        k.dma("sync", gb.v(), io["gbias"].v()[hh:hh + 1, 2 * d:2 * d + 2].bcast([NC_, 2]))
        k.dma("sync", x.v(), pT.v()[(GF0 + 2 * d) * 128 + hh, :].re("(n j) -> n j", j=CK))
        k.dma("sync", t2.v(), pT.v()[(GI0 + 2 * d) * 128 + hh, :].re("(n j) -> n j", j=CK))
        k.ts(x.v(), x.v(), gb[:, 0:1], None, ALU.add)
        k.act(t1.v(), x.v(), AF.Abs)
        k.act(t1.v(), t1.v(), AF.Exp, scale=-1.0)
        k.act(t1.v(), t1.v(), AF.Ln, bias=one[0:NC_, :])
        k.ts(x.v(), x.v(), 0.0, None, ALU.min)
        k.tt(x.v(), x.v(), t1.v(), ALU.subtract)
        k.ts(t2.v(), t2.v(), gb[:, 1:2], None, ALU.add)
        rv = (lambda ap: ap) if d == 0 else (lambda ap: ap[:, ::-1])
        k.op("vector", lambda e: e.tensor_tensor_scan(rv(bb.ap), c["ones"].ap[0:NC_, :], rv(x.ap), 0.0, ALU.mult, ALU.add),
             [x.v(), c["ones"].v()], [bb.v()])
        k.tt(o.CW[:, 0, :], t2.v(), bb.v(), ALU.subtract)
        k.op("vector", lambda e: e.tensor_tensor_scan(rv(cm.ap), rv(o.CW.ap[:, 0, :]), rv(o.CW.ap[:, 0, :]), NEG, ALU.max, ALU.max),
             [o.CW.v()], [cm.v()])
        e0 = CK - 1 if d == 0 else 0
        identN = c["ident"][0:NC_, 0:NC_]
        pa, pb_ = PS[0], PS[1]
        k.tr(pa[0:1, 0:NC_], bb[:, e0:e0 + 1], identN)
        k.tr(pb_[0:1, 0:NC_], cm[:, e0:e0 + 1], identN)
        k.copy(rows[:, 0, :], pa[0:1, 0:NC_], eng="scalar")
        k.copy(rows[:, 1, :], pb_[0:1, 0:NC_], eng="scalar")
        bE, cE, Mn, mp, mu, so = (rows.ap[:, i, :] for i in range(6))
        rw = [rows.v()]
        if d == 0:
            k.op("vector", lambda e: e.tensor_tensor_scan(Mn, cE, bE, NEG, ALU.max, ALU.add), rw, rw)
            k.memset(rows[:, 3, 0:1], NEG)
            k.copy(rows[:, 3, 1:NC_], rows[:, 2, 0:NC_ - 1])
        else:
            k.op("vector", lambda e: e.tensor_tensor_scan(Mn[:, 0:2][:, ::-1], cE[:, 0:2][:, ::-1], bE[:, 0:2][:, ::-1], NEG, ALU.max, ALU.add), rw, rw)
            k.op("vector", lambda e: e.tensor_tensor_scan(Mn[:, 2:NC_][:, ::-1], cE[:, 2:NC_][:, ::-1], bE[:, 2:NC_][:, ::-1], Mn[:, 0:1], ALU.max, ALU.add), rw, rw)
            k.memset(rows[:, 3, 1:2], NEG)
            k.copy(rows[:, 3, 0:1], rows[:, 2, 1:2])
            k.copy(rows[:, 3, NC_ - 1:NC_], rows[:, 2, 0:1])
            k.copy(rows[:, 3, 2:NC_ - 1], rows[:, 2, 3:NC_])
        k.tt(rows[:, 4, :], rows[:, 3, :], rows[:, 1, :], ALU.max)
        k.tt(rows[:, 5, :], rows[:, 3, :], rows[:, 4, :], ALU.subtract)
        k.act(rows[:, 5, :], rows[:, 5, :], AF.Exp)
        ident1 = c["ident"][0:1, 0:1]
        pc_ = PS[2]
        k.tr(pc_[0:NC_, 0:1], rows[:, 3, :], ident1)
        k.tr(pc_[0:NC_, 1:2], rows[:, 4, :], ident1)
        k.copy(colsb[:, 0:2], pc_[0:NC_, 0:2], eng="scalar")
        k.ts(colsb[:, 2:3], colsb[:, 1:2], -1.0, None, ALU.mult)
        k.mm(PS[3][:, 0:NC_], c["ones"][0:1, :], rows[:, 5, :])
        k.copy(o.SOB.v(), PS[3][:, 0:NC_], eng="scalar")
        k.ts(o.RQ[:, 0, :], cm.v(), colsb[:, 0:1], -1.0, ALU.max, ALU.mult)
        k.ts(o.RQ[:, 1, :], o.RQ[:, 0, :], colsb[:, 0:1], None, ALU.add)
        k.tt(o.RQ[:, 2, :], o.RQ[:, 0, :], bb.v(), ALU.subtract)
        k.act(o.CW[:, 1, :], o.CW[:, 0, :], AF.Exp, bias=colsb[:, 2:3])
        D[(d, hh)] = o

    chains = []
    for hh in range(2):
        for d in range(2):
            ch = type("C", (), {})()
            ch.hh, ch.d = hh, d
            nm = f"ml{hh}{d}"
            ch.Ct = sc.sb(nm + "Ct", [128, 128], F32)
            ch.nB = sc.sb(nm + "nB", [128, 128], F32)
            k.memset(ch.Ct.v(), 0.0)
            k.memset(ch.nB.v(), 0.0)
            ch.q = [sc.sb(nm + f"q{i}", [128, 128], F32) for i in range(2)]
            ch.k = [sc.sb(nm + f"k{i}", [128, 128], F32) for i in range(2)]
            ch.v = [sc.sb(nm + f"v{i}", [128, 128], F32) for i in range(2)]
            for n_ in ["Wt", "den", "hh_"]:
                setattr(ch, n_, sc.sb(nm + n_, [128, 128], F32))
            for n_ in ["Vt", "Kw", "Sw", "qs"]:
                setattr(ch, n_, [sc.sb(nm + n_ + str(i_), [128, 128], F32) for i_ in range(2)])
            ch.SE = [sc.sb(nm + f"SE{i_}", [128, 256], F32) for i_ in range(2)]
            chains.append(ch)
    for ch in chains:
        ch.cq = [sc.sb(f"cq{ch.hh}{ch.d}_{i}", [128, 2], F32) for i in range(2)]
    psn = [0]

    def ps():
        p = PS[psn[0] % len(PS)]
        psn[0] += 1
        return p

    def load(ch, i):
        n = ORDER[ch.d][i]
        sl = slice(n * CK, (n + 1) * CK)
        k.dma("sync", ch.q[i % 2].v(), pT.v()[(MQ + ch.hh) * 128:(MQ + ch.hh + 1) * 128, sl])
        k.dma("sync", ch.k[i % 2].v(), QK.v()[(4 + ch.hh) * 128:(5 + ch.hh) * 128, sl])
        k.dma("sync", ch.v[i % 2].v(), pT.v()[(MV + ch.hh) * 128:(MV + ch.hh + 1) * 128, sl])

    ident = c["ident"].v()

    def M1(ch, i):
        d, hh = ch.d, ch.hh
        s_ = i % 2
        n = ORDER[d][i]
        q, kk, v = ch.q[s_], ch.k[s_], ch.v[s_]
        cq = ch.cq[s_]
        Dd = D[(d, hh)]
        oh = c["ident"][0:NC_, n:n + 1]
        p0 = ps()
        k.mm(p0[:, 0:1], Dd.CW[:, 0, :], oh)
        k.mm(p0[:, 1:2], Dd.CW[:, 1, :], oh)
        k.copy(cq.v(), p0[:, 0:2], eng="scalar")
        p1, p2, p3, p4 = ps(), ps(), ps(), ps()
        k.tr(p1[:, 0:128], v.v(), ident)
        k.tr(p2[:, 0:128], kk.v(), ident)
        k.mm(p3[:, 0:128], kk.v(), q.v())
        k.mm(p4[:, 0:384], oh.bcast([NC_, 128]), Dd.RQ.v().re("p a b -> p (a b)"))
        k.copy(ch.Vt[s_].v(), p1[:, 0:128], eng="scalar")
        k.ts(ch.Kw[s_].v(), p2[:, 0:128], cq[:, 1:2], None, ALU.mult)
        k.tt(ch.Wt.v(), p4[:, 0:128], (c["NEGF"] if d == 0 else c["NEGB"]).v(), ALU.add)
        k.act(ch.SE[s_].v(), p4[:, 128:384], AF.Exp)
        k.act(ch.Wt.v(), ch.Wt.v(), AF.Exp, bias=cq[:, 0:1])
        k.tt(ch.Sw[s_].v(), p3[:, 0:128], ch.Wt.v(), ALU.mult)
        k.tt(ch.qs[s_].v(), q.v(), ch.SE[s_][:, 0:128], ALU.mult, eng="gpsimd")

    def M2a(ch, i):
        s_ = i % 2
        ch.pn, ch.pd = ps(), ps()
        k.mm(ch.pn[:, 0:128], ch.Vt[s_].v(), ch.Sw[s_].v(), start=True, stop=False)
        k.mm(ch.pn[:, 0:128], ch.Ct.v(), ch.qs[s_].v(), start=False, stop=True)
        k.mm(ch.pd[:, 0:128], c["ones"].v(), ch.Sw[s_].v(), start=True, stop=False)
        k.mm(ch.pd[:, 0:128], ch.nB.v(), ch.qs[s_].v(), start=False, stop=True)

    def M2b(ch, i):
        s_ = i % 2
        n = ORDER[ch.d][i]
        sl = slice(n * CK, (n + 1) * CK)
        k.act(ch.den.v(), ch.pd[:, 0:128], AF.Abs)
        k.tt(ch.den.v(), ch.den.v(), ch.SE[s_][:, 128:256], ALU.max)
        k.recip(ch.den.v(), ch.den.v())
        k.tt(ch.hh_.v(), ch.pn[:, 0:128], ch.den.v(), ALU.mult)
        ha = hacc[ch.hh]
        k.tt(ha[:, sl], ha[:, sl], ch.hh_.v(), ALU.add, eng="gpsimd")

    def M2c(ch, i):
        s_ = i % 2
        ch.pc, ch.pb = ps(), ps()
        k.mm(ch.pc[:, 0:128], ch.Kw[s_].v(), ch.Vt[s_].v())
        k.mm(ch.pb[:, 0:128], ch.Kw[s_].v(), c["ones"].v())

    def M2d(ch, i):
        n = ORDER[ch.d][i]
        so = D[(ch.d, ch.hh)].SOB[:, n:n + 1]
        k.stt(ch.Ct.v(), ch.Ct.v(), so, ch.pc[:, 0:128], ALU.mult, ALU.add)
        k.stt(ch.nB.v(), ch.nB.v(), so, ch.pb[:, 0:128], ALU.mult, ALU.add)

    for ch in chains:
        load(ch, 0)
        load(ch, 1)
    for ch in chains:
        M1(ch, 0)
    for i in range(NC_):
        if i + 1 < NC_:
            for ch in chains:
                M1(ch, i + 1)
        for stg in (M2a, M2b, M2c, M2d):
            for ch in chains:
                stg(ch, i)
        if i + 2 < NC_:
            for ch in chains:
                load(ch, i + 2)
    sc.__exit__(None, None, None)


def pm(v, nchunk):
    return np.ascontiguousarray(np.asarray(v, np.float32).reshape(nchunk, 128).T)
def c_inputs(inp, layer, b, m, xlT, xcT, ylT, ycT):
    d = {}
    if xcT is not None:
        d["xT"] = np.ascontiguousarray(np.concatenate([xcT[:, 128*m:128*m+128], xlT[:, 2048*m:2048*m+2048]], 1))
        d["yT"] = np.ascontiguousarray(np.concatenate([ycT[:, 128*m:128*m+128], ylT[:, 2048*m:2048*m+2048]], 1))
    else:
        d["xT"] = np.ascontiguousarray(xlT[:, 2048*m:2048*m+2048])
        d["yT"] = np.ascontiguousarray(ylT[:, 2048*m:2048*m+2048])
    sc = np.stack([pm(inp["c"][b], 8), pm(inp["c_ctx"], 8)], -1)
    d["sc"] = np.ascontiguousarray(sc.reshape(128, 16))
    d["w_mod"] = np.ascontiguousarray(inp["w_mod"][layer])
    d["b_mod"] = pm(inp["b_mod"][layer], 48)
    d["lnp"] = np.ascontiguousarray(np.concatenate([pm(inp["ln_g"][layer,0],8), pm(inp["ln_g"][layer,1],8), pm(inp["ln_b"][layer,0],8), pm(inp["ln_b"][layer,1],8)], 1))
    d["w_out"] = np.ascontiguousarray(inp["even_w_out" if layer % 2 == 0 else "odd_w_out"][layer//2])
    d["rw"] = np.ascontiguousarray(inp["router_w"][layer])
    d["rb"] = np.ascontiguousarray(inp["router_b"][layer].reshape(32,1))
    d["w1"] = np.ascontiguousarray(inp["exp_w1"][layer])
    d["w2"] = np.ascontiguousarray(inp["exp_w2"][layer])
    d["b1"] = np.ascontiguousarray(inp["exp_b1"][layer].reshape(32,16,128).transpose(2,0,1).reshape(128, 512))
    d["b2"] = np.ascontiguousarray(inp["exp_b2"][layer].reshape(32,8,128).transpose(2,0,1).reshape(128, 256))
    d["ident"] = np.eye(128, dtype=np.float32)
    return d

def even_cols(m):
    cols = []
    for base in (0, 512, 1024):
        cols += list(range(base + 256*m, base + 256*m + 256))
    cols += list(range(1536, 1920))
    cols += list(range(1920 + 256*m, 1920 + 256*m + 256))
    cols += list(range(1920 + 512 + 256*m, 1920 + 512 + 256*m + 256))
    return np.array(cols)

def a0_inputs(inp, b, m, xallT):
    i = 0; layer = 0
    d = {}
    d["xT"] = np.ascontiguousarray(xallT)
    sc = np.stack([pm(inp["c"][b], 8), pm(inp["c_ctx"], 8)], -1)
    d["sc"] = np.ascontiguousarray(sc.reshape(128, 16))
    d["w_mod"] = np.ascontiguousarray(inp["w_mod"][layer])
    d["b_mod"] = pm(inp["b_mod"][layer], 48)
    d["w_in"] = np.ascontiguousarray(inp["even_w_in"][i][:, even_cols(m)])
    conv = np.zeros((128, 10), np.float32); gb = np.zeros((128, 8), np.float32); lam = np.zeros((128, 4), np.float32)
    gwbd = np.zeros((2, 2, 2, 128, 128), np.float32)
    for cc in range(2):
        ch = 256*m + 128*cc + np.arange(128)
        for j in range(4): conv[:, cc*5 + j] = inp["b_conv_w"][i][j, ch]
        conv[:, cc*5 + 4] = inp["b_conv_b"][i][ch]
        for dd in range(2):
            lam[:, cc*2 + dd] = inp["b_lam"][i][dd, ch]
            for s in range(2):
                gb[:, cc*4 + dd*2 + s] = inp["b_gate_b"][i][dd, s, ch]
                for g2 in range(2):
                    gwbd[cc, dd, s, g2*64:(g2+1)*64, g2*64:(g2+1)*64] = inp["b_gate_w"][i][dd, s, 4*m + 2*cc + g2]
    d["conv"] = conv; d["gb"] = gb; d["lam"] = lam; d["gwbd"] = gwbd
    d["ident"] = np.eye(128, dtype=np.float32)
    return d


def rwkv_consts():
    i = np.arange(128)
    r, c = i[:, None], i[None, :]
    MUs = (c > r).astype(np.float32); MLs = (c < r).astype(np.float32)
    MUi = (c >= r).astype(np.float32); MLi = (c <= r).astype(np.float32)
    bones = ((r // 64) == (c // 64)).astype(np.float32)
    mats = [np.eye(128, dtype=np.float32), bones, bones / 64.0,
            -MUs, -MLs, -MLs, -MUs, MUs, MUi, -MUi, MLs, MLi, -MLi]
    cst = np.ascontiguousarray(np.stack(mats, 1))
    rmask = np.ones((128, 256), np.float32); rmask[:, ::64] = 0.0
    m42 = np.stack([(i % 4 == s) for s in range(4)] + [(i % 2 == s) for s in range(2)], 1).astype(np.float32)
    return cst, rmask, np.ascontiguousarray(m42)

def rwkv_inputs(inp, m):
    i = 0
    rp = np.zeros((2, 128, 16), np.float32)
    wBs = np.zeros((2, 128, 128), np.float32); aBs = np.zeros((2, 128, 128), np.float32); gBs = np.zeros((2, 128, 128), np.float32)
    p = np.arange(128)
    mu = inp["a_mu"][i]
    for g in range(2):
        ch = 256*m + 128*g + p
        rp[g, :, 0] = mu[ch]; rp[g, :, 1] = mu[512 + ch]; rp[g, :, 2] = mu[1024 + ch]
        rp[g, :, 3] = mu[1536 + p]; rp[g, :, 4] = mu[1664 + p]; rp[g, :, 5] = mu[1792 + p]
        for d in range(2):
            rp[g, :, 6 + d] = inp["a_w0"][i][d, ch]; rp[g, :, 8 + d] = inp["a_a0"][i][d, ch]
            wBs[g, d*64:(d+1)*64, :] = inp["a_wB"][i][d][:, ch]
            aBs[g, d*64:(d+1)*64, :] = inp["a_aB"][i][d][:, ch]
        rp[g, :, 10] = inp["a_kk"][i][ch]; rp[g, :, 11] = inp["a_ka"][i][ch]; rp[g, :, 12] = inp["a_rk"][i].reshape(-1)[ch]
        rp[g, :, 13] = inp["a_gn_g"][i][ch]; rp[g, :, 14] = inp["a_gn_b"][i][ch]
        gBs[g] = inp["a_gB"][i][:, ch]
    cst, rmask, m42 = rwkv_consts()
    return {"rp": rp, "wBs": wBs, "aBs": aBs, "gBs": gBs, "cst": cst, "rmask": rmask, "m42": m42}


def odd_wsel(inp, m):
    W = inp["odd_w_in"][0]
    cols = []
    def blk(base, h): return list(range(base + 128*h, base + 128*h + 128))
    def swp(base, h): return list(range(base + 128*h + 64, base + 128*h + 128)) + list(range(base + 128*h, base + 128*h + 64))
    hs = [2*m, 2*m + 1]
    chunks = []
    for f, base in [(blk, 0), (swp, 0), (blk, 512), (swp, 512), (blk, 1024), (blk, 1536),
                    (blk, 2048), (blk, 2560), (blk, 3072), (blk, 3584)]:
        for h in hs:
            chunks.append(W[:, f(base, h)])
    for d in range(2):
        for kind in (1, 0):
            g = np.zeros((1024, 128), np.float32)
            for hh, h in enumerate(hs):
                g[:, hh] = W[:, 4096 + kind*8 + d*4 + h]
            chunks.append(g)
    return np.ascontiguousarray(np.concatenate(chunks, 1))

def odd_consts():
    i = np.arange(128)
    r, c = i[:, None].astype(np.float32), i[None, :].astype(np.float32)
    ABSD = np.abs(c - r)
    MUi = (c >= r).astype(np.float32); MLi = (c <= r).astype(np.float32)
    POS1F = np.broadcast_to(c + 1.0, (128, 128)); POS1B = np.broadcast_to(128.0 - c, (128, 128))
    NEGF = np.where(c >= r, 0.0, -1.0e30); NEGB = np.where(c <= r, 0.0, -1.0e30)
    mats = [np.eye(128), ABSD, MUi, MLi, POS1F, POS1B, np.full((128,128), 1/128.0), NEGF, NEGB, np.ones((128,128))]
    cst = np.ascontiguousarray(np.stack([np.asarray(a, np.float32) for a in mats], 1))
    ZP = np.stack([127.0 - i, i.astype(np.float64), np.full(128, 128.0)], 1).astype(np.float32)
    t = np.arange(4096); row = (t // 64).astype(np.float64); col = (t % 64).astype(np.float64)
    inv = 10000.0 ** (-np.arange(32) / 32.0)
    ang = np.concatenate([row[None, :] * inv[:, None], col[None, :] * inv[:, None]], 0)
    CC = np.concatenate([np.cos(ang), np.cos(ang)], 0).astype(np.float32)
    SS = np.concatenate([-np.sin(ang), np.sin(ang)], 0).astype(np.float32)
    rm128 = np.ones((128, 512), np.float32); rm128[:, ::128] = 0.0
    ng128 = np.zeros((128, 512), np.float32); ng128[:, ::128] = -1.0e30
    return {"ocst": cst, "ZP": ZP, "ropeC": np.ascontiguousarray(CC), "ropeS": np.ascontiguousarray(SS), "rm128": rm128, "ng128": ng128}

def a1_inputs(inp, b, m, xallT):
    layer = 1
    d = {}
    d["xT"] = np.ascontiguousarray(xallT)
    sc = np.stack([pm(inp["c"][b], 8), pm(inp["c_ctx"], 8)], -1)
    d["sc"] = np.ascontiguousarray(sc.reshape(128, 16))
    d["w_mod"] = np.ascontiguousarray(inp["w_mod"][layer])
    d["b_mod"] = pm(inp["b_mod"][layer], 48)
    d["w_in"] = odd_wsel(inp, m)
    p = np.arange(128)
    lgc = np.zeros((128, 4), np.float32); gnp = np.zeros((128, 8), np.float32); gbias = np.zeros((128, 4), np.float32)
    for hh in range(2):
        h = 2*m + hh
        for dd in range(2):
            lgc[:, hh*2 + dd] = inp["c_log_gamma"][0][dd, h]
        gnp[:, hh*2 + 0] = inp["c_gn_g"][0][128*h + p]; gnp[:, hh*2 + 1] = inp["c_gn_b"][0][128*h + p]
        gnp[:, 4 + hh*2 + 0] = inp["d_gn_g"][0][128*h + p]; gnp[:, 4 + hh*2 + 1] = inp["d_gn_b"][0][128*h + p]
    for dd in range(2):
        for hh in range(2):
            gbias[hh, dd*2 + 0] = inp["d_fbias"][0][dd, 2*m + hh]
            gbias[hh, dd*2 + 1] = inp["d_ibias"][0][dd, 2*m + hh]
    d["lgc"] = lgc; d["gnp"] = gnp; d["gbias"] = gbias
    d.update(odd_consts())
    return d

T = T_ALL


def _load_rconsts(k, io):
    c = {}
    cst = k.sb("cst", [128, 13, 128], F32)
    k.dma("sync", cst.v(), io["cst"].v())
    names = ["ident", "bones", "bones64"]
    for i, n in enumerate(names):
        c[n] = Buf(cst.ap[:, i, :], "c_" + n)
    for n, a, b in (("np2f", 3, 5), ("np2b", 5, 7), ("m3f", 7, 10), ("m3b", 10, 13)):
        c[n] = Buf(cst.ap[:, a:b, :].rearrange("p a b -> p (a b)"), "c_" + n)
    for n in c:
        c[n].w = cst.w
    c["rmask"] = k.sb("rmask", [128, 256], F32)
    k.dma("sync", c["rmask"].v(), io["rmask"].v())
    return c


def _load_oconsts(k, io):
    c = {}
    cst = k.sb("ocst", [128, 10, 128], F32)
    k.dma("sync", cst.v(), io["ocst"].v())
    names = ["ident", "ABSD", "MUi", "MLi", "POS1F", "POS1B", "ones128", "NEGF", "NEGB", "ones"]
    for i, n in enumerate(names):
        c[n] = Buf(cst.ap[:, i, :], "c_" + n)
        c[n].w = cst.w
    c["ZP"] = k.sb("ZP", [128, 3], F32)
    k.dma("sync", c["ZP"].v(), io["ZP"].v())
    c["eps"] = k.sb("oeps", [128, 1], F32)
    k.memset(c["eps"].v(), 1e-5)
    return c


SH_IN = [("xT", [1024, T]), ("sc", [128, 16]), ("sel", [128, 2]),
         ("cst", [128, 13, 128]), ("rmask", [128, 256]), ("m42", [128, 6]),
         ("ocst", [128, 10, 128]), ("ZP", [128, 3]), ("ropeC", [128, 4096]), ("ropeS", [128, 4096]), ("ident", [128, 128])]
L_IN = [("w_mod", [1024, 6144]), ("b_mod", [128, 48]), ("lnp", [128, 32]), ("w_out", [1024, 1024]), ("rw", [1024, 32]), ("rb", [32, 1]),
        ("w1", [32, 1024, 2048]), ("b1", [128, 512]), ("w2", [32, 1024, 1024]), ("b2", [128, 256])]
A0M_IN = [("w_in", [1024, 13 * 128]), ("conv", [128, 10]), ("gb", [128, 8]), ("lam", [128, 4]), ("gwbd", [2, 2, 2, 128, 128]),
          ("rp", [2, 128, 16]), ("wBs", [2, 128, 128]), ("aBs", [2, 128, 128]), ("gBs", [2, 128, 128])]
A1M_IN = [("w_in", [1024, 24 * 128]), ("lgc", [128, 4]), ("gnp", [128, 8]), ("gbias", [128, 4])]


def build_fused():
    k = KB(n_dma_sems=48)
    sh = {n: k.dram(n, shp, F32, kind="ExternalInput") for n, shp in SH_IN}
    L = [{n: k.dram(f"L{l}_{n}", shp, F32, kind="ExternalInput") for n, shp in L_IN} for l in range(2)]
    A0 = [{n: k.dram(f"A0m{m}_{n}", shp, F32, kind="ExternalInput") for n, shp in A0M_IN} for m in range(2)]
    A1 = [{n: k.dram(f"A1m{m}_{n}", shp, F32, kind="ExternalInput") for n, shp in A1M_IN} for m in range(2)]
    out = k.dram("out", [1024, 2048], F32, kind="ExternalOutput")
    pT = k.dram("pT", [24 * 128, T], F32, kind="Internal")
    PRE = [k.dram(f"PRE{g}", [9 * 128, T], F32, kind="Internal") for g in range(2)]
    GBd = [k.dram(f"GBd{g}", [2 * 128, T], F32, kind="Internal") for g in range(2)]
    QK = k.dram("QK", [6 * 128, T], F32, kind="Internal")
    yS = k.dram("yS", [1024, T], F32, kind="Internal")
    x1all = k.dram("x1all", [1024, T], F32, kind="Internal")
    x1d0 = k.dram("x1d0", [1024, T], F32, kind="Internal")
    x1d1 = k.dram("x1d1", [1024, 2048], F32, kind="Internal")
    PSb = [k.ps(f"ps{i}", [128, 512], F32) for i in range(8)]
    PSq = [PSView(b) for b in PSb]
    cr = _load_rconsts(k, sh)
    co = _load_oconsts(k, sh)
    cc = load_consts(k, sh)
    sel = k.sb("sel", [128, 2], F32)
    k.dma("sync", sel.v(), sh["sel"].v())

    modvL = [k.sb(f"modvL{l}", [128, 48, 2], F32) for l in range(2)]
    io = dict(sh)
    io.update(L[0])
    compute_mod(k, PSb, io, list(range(6)), modvL[0])
    for m in range(2):
        io = dict(sh)
        io.update(L[0])
        io.update(A0[m])
        io["modv"] = modvL[0]
        in_proj(k, PSb, io, 13, pT)
        cast_weights(k, L[m], f"L{m}")
        rglru(k, PSb, io, pT, 9, 11, yS, 512 + 256 * m)
        for g in range(2):
            rwkv_prep(k, PSb, cr, io, pT, g, PRE[g], GBd[g])
        k.barrier()
        with k.scope() as sc:
            wkv = [sc.sb(f"wkv{g}", [128, T], F32) for g in range(2)]
            for g in range(2):
                k.memset(wkv[g].v(), 0.0)
            rwkv_scan2(k, PSb, cr, PRE, wkv)
            for g in range(2):
                rwkv_finish(k, PSb, cr, io, g, GBd[g], wkv, yS, 256 * m + g * 128)
    io = dict(sh)
    io.update(L[0])
    io["modv"] = modvL[0]
    io["yT"] = yS
    io["out"] = x1all
    io["x1d"] = x1d0
    groups = [[(0, 256, 1), (256, 512, 0), (768, 384, 0)], [(1152, 512, 0), (1664, 512, 0), (2176, 128, 0)],
              [(2304, 512, 0), (2816, 512, 0), (3328, 128, 0)], [(3456, 512, 0), (3968, 384, 0)]]
    phase_C(k, cc, io, groups, 1152, PSb)
    io = dict(sh)
    io.update(L[1])
    compute_mod(k, PSb, io, list(range(6)), modvL[1])
    for m in range(2):
        io = dict(sh)
        io.update(L[1])
        io.update(A1[m])
        io["modv"] = modvL[1]
        io["xT"] = x1all
        in_proj(k, PSb, io, 24, pT)
        rope_pass(k, io, pT, QK)
        k.barrier()
        with k.scope() as sc:
            gnp = sc.sb("gnp", [128, 8], F32)
            k.dma("sync", gnp.v(), io["gnp"].v())
            oacc = [sc.sb(f"oacc{i}", [128, T], F32) for i in range(4)]
            for o in oacc:
                k.memset(o.v(), 0.0)
            retention(k, PSb, co, io, pT, QK, oacc)
            for hh in range(2):
                def gsrc(sl, N, tile, hh=hh):
                    k.dma("sync", tile[:, :N], pT.v()[(RG + hh) * 128:(RG + hh + 1) * 128, sl])
                gn_finish(k, PSb, co, gnp[:, hh * 2:hh * 2 + 2], oacc[hh], gsrc, AF.Silu, yS, 256 * m + hh * 128)
            mlstm(k, PSb, co, io, pT, QK, oacc[2:4])
            for hh in range(2):
                def gsrc(sl, N, tile, hh=hh):
                    k.dma("sync", tile[:, :N], pT.v()[(MO + hh) * 128:(MO + hh + 1) * 128, sl])
                gn_finish(k, PSb, co, gnp[:, 4 + hh * 2:4 + hh * 2 + 2], oacc[2 + hh], gsrc, AF.Sigmoid, yS, 512 + 256 * m + hh * 128)
    io = dict(sh)
    io.update(L[1])
    io["modv"] = modvL[1]
    io["out"] = out
    io["x1d"] = x1d1

    def load_xy(xb, yb, st, N, ta, tb):
        for (src, dst) in ((x1all, xb), (yS, yb)):
            o0, o1 = 256 + st, 256 + 2048 + st
            k.dma("sync", ta[:, :, :N], src.v()[:, o0:o0 + N].re("(c p) t -> p c t", p=128))
            k.dma("sync", tb[:, :, :N], src.v()[:, o1:o1 + N].re("(c p) t -> p c t", p=128))
            k.ts(ta[:, :, :N], ta[:, :, :N], sel[:, 0:1], None, ALU.mult)
            k.stt(dst[:, :, :N], tb[:, :, :N], sel[:, 1:2], ta[:, :, :N], ALU.mult, ALU.add)
    io["load_xy"] = load_xy
    groups = [[(0, 512, 0), (512, 512, 0)], [(1024, 512, 0), (1536, 512, 0)]]
    phase_C(k, cc, io, groups, 1024, PSb)
    k.finish([out])
    return k


def _core_inputs(inp, b, m):
    d = {}
    xall = np.ascontiguousarray(np.concatenate([inp["ctx"][b].T, inp["x"][b].T], 1))
    d["xT"] = xall
    sc = np.stack([pm(inp["c"][b], 8), pm(inp["c_ctx"], 8)], -1)
    d["sc"] = np.ascontiguousarray(sc.reshape(128, 16))
    sel = np.zeros((128, 2), np.float32)
    sel[:, m] = 1.0
    d["sel"] = sel
    cst, rmask, m42 = rwkv_consts()
    d["cst"], d["rmask"], d["m42"] = cst, rmask, m42
    oc = odd_consts()
    for n in ("ocst", "ZP", "ropeC", "ropeS"):
        d[n] = oc[n]
    d["ident"] = np.eye(128, dtype=np.float32)
    for l in range(2):
        ci = c_inputs(inp, l, b, 0, xall[:, 256:], None, xall[:, 256:], None)
        for n, _ in L_IN:
            d[f"L{l}_{n}"] = ci[n]
    for mm in range(2):
        a0 = a0_inputs(inp, b, mm, xall)
        a0.update(rwkv_inputs(inp, mm))
        for n, _ in A0M_IN:
            d[f"A0m{mm}_{n}"] = a0[n]
        a1 = a1_inputs(inp, b, mm, xall)
        for n, _ in A1M_IN:
            d[f"A1m{mm}_{n}"] = a1[n]
    return d


def kernel(**inp):
    inp = {k_: np.asarray(v) for k_, v in inp.items()}
    B = 4
    kf = build_fused()
    maps = []
    for b in range(B):
        d0 = _core_inputs(inp, b, 0)
        d1 = dict(d0)
        sel = np.zeros((128, 2), np.float32)
        sel[:, 1] = 1.0
        d1["sel"] = sel
        maps += [d0, d1]
    res = run_bass_kernel_spmd(kf.nc, maps, core_ids=list(range(8)))
    out = np.zeros((B, 4096, 1024), np.float32)
    for b in range(B):
        for m in range(2):
            out[b, 2048 * m:2048 * m + 2048, :] = res.results[2 * b + m]["out"].T
    return out
```

```python
import numpy as np
import concourse.bass as bass
import concourse.mybir as mybir
from concourse.bass_utils import run_bass_kernel_spmd

F32 = mybir.dt.float32
BF16 = mybir.dt.bfloat16
AF = mybir.ActivationFunctionType
ALU = mybir.AluOpType
AX = mybir.AxisListType


class Buf:
    def __init__(self, ap, name):
        self.ap = ap
        self.name = name
        self.w = None
        self.r = []
        self.excl = False

    def __getitem__(self, idx):
        return V(self, self.ap[idx])

    def v(self, ap=None):
        return V(self, self.ap if ap is None else ap)


class V:
    def __init__(self, buf, ap):
        self.buf = buf
        self.ap = ap

    def __getitem__(self, idx):
        return V(self.buf, self.ap[idx])

    def re(self, pat, **kw):
        return V(self.buf, self.ap.rearrange(pat, **kw))

    def bcast(self, shape):
        return V(self.buf, self.ap.to_broadcast(list(shape)))


class KB:
    ENG = ("tensor", "vector", "scalar", "gpsimd", "sync")

    def __init__(self, n_dma_sems=24, same_engine_sync=True):
        self.nc = bass.Bass("TRN2", target_bir_lowering=False)
        nc = self.nc
        self.e = {n: getattr(nc, n) for n in self.ENG}
        self.sem = {n: nc.alloc_semaphore(name=f"prog_{n}") for n in self.ENG}
        self.cnt = {n: 0 for n in self.ENG}
        self.seen = {n: {} for n in self.ENG}
        self.dsem = [nc.alloc_semaphore(name=f"dma_{i}") for i in range(n_dma_sems)]
        self.dcnt = [0] * n_dma_sems
        self.dnext = 0
        self.same_engine_sync = same_engine_sync
        self.out_tokens = []
        self.n_inst = 0

    def sb(self, name, shape, dtype=F32):
        self.uid = getattr(self, "uid", 0) + 1
        t = self.nc.alloc_sbuf_tensor(f"sb_{name}_p{self.uid}", list(shape), dtype)
        return Buf(t.ap(), name)

    def ps(self, name, shape, dtype=F32):
        t = self.nc.alloc_psum_tensor("psum_" + name, list(shape), dtype)
        b = Buf(t.ap(), name)
        b.excl = True
        return b

    def dram(self, name, shape, dtype=F32, kind="Internal"):
        t = self.nc.dram_tensor(name, list(shape), dtype, kind=kind)
        return Buf(t.ap(), name)

    def split(self, buf, views, name=None):
        return [Buf(v, f"{name or buf.name}_{i}") for i, v in enumerate(views)]

    def _wait(self, eng, tok):
        if tok is None:
            return
        kind = tok[0]
        if kind == "e":
            _, e2, c = tok
            if e2 == eng and (not self.same_engine_sync or eng == "tensor"):
                return
            key = ("e", e2)
            sem = self.sem[e2]
        else:
            _, i, c = tok
            key = ("d", i)
            sem = self.dsem[i]
        if self.seen[eng].get(key, 0) >= c:
            return
        self.e[eng].wait_ge(sem, c)
        self.seen[eng][key] = c

    def _deps(self, eng, reads, writes):
        for v in reads:
            self._wait(eng, v.buf.w)
        for v in writes:
            self._wait(eng, v.buf.w)
            for t in v.buf.r:
                self._wait(eng, t)

    def _mark(self, tok, reads, writes):
        for v in reads:
            b = v.buf
            b.r = [t for t in b.r if not (t[0] == tok[0] and t[1] == tok[1])] + [tok]
        for v in writes:
            v.buf.w = tok
            v.buf.r = []

    def op(self, eng, fn, reads, writes):
        ex = [v for v in reads if v.buf.excl]
        if ex:
            reads = [v for v in reads if not v.buf.excl]
            writes = list(writes) + ex
        self._deps(eng, reads, writes)
        inst = fn(self.e[eng])
        self.cnt[eng] += 1
        inst.then_inc(self.sem[eng], 1)
        self._mark(("e", eng, self.cnt[eng]), reads, writes)
        self.n_inst += 1
        return inst

    def dma(self, q, out, in_, **kw):
        i = self.dnext
        self.dnext = (self.dnext + 1) % len(self.dsem)
        if self.dcnt[i] > 0:
            self._wait(q, ("d", i, self.dcnt[i]))
        self._deps(q, [in_], [out])
        inst = self.e[q].dma_start(out=out.ap, in_=in_.ap, **kw)
        self.dcnt[i] += 16
        inst.then_inc(self.dsem[i], 16)
        tok = ("d", i, self.dcnt[i])
        self._mark(tok, [in_], [out])
        self.n_inst += 1
        return tok

    def dbg(self, name, v, shape, dtype=F32):
        if not getattr(self, "debug", False):
            return
        d = self.dram("dbg_" + name, list(shape), dtype, kind="ExternalOutput")
        self.dma("sync", d.v(), v)
        self.dbg_bufs = getattr(self, "dbg_bufs", []) + [d]

    def finish(self, bufs, eng="sync"):
        bufs = list(bufs) + getattr(self, "dbg_bufs", [])
        for b in bufs:
            self._wait(eng, b.w)

    def mm(self, out, lhsT, rhs, start=True, stop=True):
        return self.op("tensor", lambda e: e.matmul(out.ap, lhsT.ap, rhs.ap, start=start, stop=stop),
                       [lhsT, rhs] + ([] if start else [out]), [out])

    def tr(self, out, in_, ident):
        return self.op("tensor", lambda e: e.transpose(out.ap, in_.ap, ident.ap), [in_, ident], [out])

    def act(self, out, in_, func, bias=None, scale=None, eng="scalar", accum_out=None):
        kw = {}
        reads = [in_]
        writes = [out]
        if bias is not None:
            if isinstance(bias, V):
                kw["bias"] = bias.ap
                reads.append(bias)
            else:
                kw["bias"] = bias
        if scale is not None:
            if isinstance(scale, V):
                kw["scale"] = scale.ap
                reads.append(scale)
            else:
                kw["scale"] = scale
        if accum_out is not None:
            kw["accum_out"] = accum_out.ap
            writes.append(accum_out)
        return self.op(eng, lambda e: e.activation(out.ap, in_.ap, func, **kw), reads, writes)

    def tt(self, out, a, b, op, eng="vector"):
        return self.op(eng, lambda e: e.tensor_tensor(out.ap, a.ap, b.ap, op), [a, b], [out])

    def ts(self, out, a, s1, s2, op0, op1=None, eng="vector", accum_out=None):
        reads = [a]
        writes = [out]

        def g(s):
            if isinstance(s, V):
                reads.append(s)
                return s.ap
            return s
        s1a, s2a = g(s1), g(s2)
        kw = {}
        if op1 is not None:
            kw["op1"] = op1
        if accum_out is not None:
            kw["accum_out"] = accum_out.ap
            writes.append(accum_out)
        return self.op(eng, lambda e: e.tensor_scalar(out.ap, a.ap, s1a, s2a, op0, **kw), reads, writes)

    def stt(self, out, a, s, b, op0, op1, eng="vector"):
        reads = [a, b]
        if isinstance(s, V):
            reads.append(s)
            sa = s.ap
        else:
            sa = s
        return self.op(eng, lambda e: e.scalar_tensor_tensor(out.ap, a.ap, sa, b.ap, op0, op1), reads, [out])

    def copy(self, out, in_, eng="vector"):
        if eng == "scalar":
            return self.op(eng, lambda e: e.copy(out.ap, in_.ap), [in_], [out])
        return self.op(eng, lambda e: e.tensor_copy(out.ap, in_.ap), [in_], [out])

    def memset(self, out, val, eng="vector"):
        return self.op(eng, lambda e: e.memset(out.ap, val), [], [out])

    def reduce(self, out, in_, op, axis=AX.X, eng="vector"):
        return self.op(eng, lambda e: e.tensor_reduce(out.ap, in_.ap, axis, op), [in_], [out])

    def recip(self, out, in_, eng="vector"):
        return self.op(eng, lambda e: e.reciprocal(out.ap, in_.ap), [in_], [out])


import contextlib


class Scope:
    uid = 0

    def __init__(self, k):
        self.k = k
        self.stack = contextlib.ExitStack()

    def sb(self, name, shape, dtype=F32):
        Scope.uid += 1
        name = f"sc_{name}_u{Scope.uid}"
        t = self.stack.enter_context(self.k.nc.sbuf_tensor(name, list(shape), dtype))
        return Buf(t.ap(), name)

    def __enter__(self):
        return self

    def __exit__(self, *a):
        self.k.barrier()
        self.stack.close()
        return False


def _barrier(self):
    for eng in self.ENG:
        for e2 in self.ENG:
            if e2 != eng and self.cnt[e2] > 0:
                self._wait(eng, ("e", e2, self.cnt[e2]))
        for i, c in enumerate(self.dcnt):
            if c > 0:
                self._wait(eng, ("d", i, c))


KB.barrier = _barrier
KB.scope = lambda self: Scope(self)


class PSView:
    def __init__(self, b):
        self.b = b

    def v(self):
        return self.b[:, 0:128]

    def __getitem__(self, idx):
        return self.b[:, 0:128][idx]


D = 1024
NE = 32
DN_ALPHA = 4 ** 0.25
LN_EPS = 1e-5
SW_ALPHA = 1.702
SW_LIM = 7.0


def load_consts(k, cd):
    c = {}
    c["ident"] = k.sb("c_ident", [128, 128], F32)
    k.dma("sync", c["ident"].v(), cd["ident"].v())
    c["onesm"] = k.sb("c_onesm", [128, 128], F32)
    k.memset(c["onesm"].v(), 1.0 / D)
    c["eps"] = k.sb("c_eps", [128, 1], F32)
    k.memset(c["eps"].v(), LN_EPS)
    return c


def ln_block(k, c, s, N, gcol, bcol, out_fn, tmp, ps_mean, ps_ex2):
    sq, mean_sb, rstd, t = tmp["sq"], tmp["mean"], tmp["rstd"], tmp["t"]
    k.act(sq[:, :, :N], s[:, :, :N], AF.Square)
    for ci in range(8):
        k.mm(ps_mean[:, :N], c["onesm"].v(), s[:, ci, :N], start=(ci == 0), stop=(ci == 7))
    for ci in range(8):
        k.mm(ps_ex2[:, :N], c["onesm"].v(), sq[:, ci, :N], start=(ci == 0), stop=(ci == 7))
    k.copy(mean_sb[:, :N], ps_mean[:, :N], eng="scalar")
    k.tt(rstd[:, :N], mean_sb[:, :N], mean_sb[:, :N], ALU.mult)
    k.tt(rstd[:, :N], ps_ex2[:, :N], rstd[:, :N], ALU.subtract)
    k.act(rstd[:, :N], rstd[:, :N], AF.Sqrt, bias=c["eps"].v())
    k.recip(rstd[:, :N], rstd[:, :N])
    for ci in range(8):
        k.tt(t[:, :N], s[:, ci, :N], mean_sb[:, :N], ALU.subtract)
        k.tt(t[:, :N], t[:, :N], rstd[:, :N], ALU.mult)
        out_fn(ci, t[:, :N])


def phase_C(k, c, io, groups, NTG, PS):
    nc = k.nc
    psc = k.scope()
    psc.__enter__()
    modv = psc.sb("modv", [128, 48, 2], F32)
    sc1 = psc.sb("sc1", [128, 8, 2], F32)
    sc4 = psc.sb("sc4", [128, 8, 2], F32)
    lnp = psc.sb("lnp", [128, 32], F32)
    hT = psc.sb("hT", [128, 8, NTG], BF16)
    acc = psc.sb("acc", [128, 8, NTG], F32)
    gatesT = psc.sb("gatesT", [32, NTG], F32)
    b1s = psc.sb("b1s", [128, 32 * 16], F32)
    b2s = psc.sb("b2s", [128, 32 * 8], F32)
    rbs = psc.sb("rbs", [32, 1], F32)
    k.dma("sync", lnp.v(), io["lnp"].v())
    k.dma("sync", b1s.v(), io["b1"].v())
    k.dma("sync", b2s.v(), io["b2"].v())
    k.dma("sync", rbs.v(), io["rb"].v())

    if "modv" in io:
        k.copy(modv.v(), io["modv"].v(), eng="gpsimd")
    else:
        compute_mod(k, PS, io, list(range(6)), modv)
    k.ts(sc1.v(), modv[:, 8:16, :], 1.0, None, ALU.add)
    k.ts(sc4.v(), modv[:, 32:40, :], 1.0, None, ALU.add)

    def mv(m, ci, col):
        return modv[:, m * 8 + ci, col:col + 1]

    k.dbg("modv", modv.v(), [128, 48, 2])
    for blocks in groups:
        _group(k, c, io, blocks, PS, modv, sc4, lnp, hT, acc, gatesT, b1s, b2s, rbs, mv)
    psc.__exit__(None, None, None)


def _group(k, c, io, blocks, PS, modv, sc4, lnp, hT, acc, gatesT, b1s, b2s, rbs, mv):
    nc = k.nc
    g0 = blocks[0][0]
    k.memset(acc.v(), 0.0, eng="gpsimd")

    with k.scope() as sc:
        wo = sc.sb("wo", [128, 8, 1024], BF16)
        rw = sc.sb("rw", [128, 8, 32], F32)
        xb = sc.sb("xb", [128, 8, 512], F32)
        yb = sc.sb("yb", [128, 8, 512], BF16)
        s = sc.sb("s", [128, 8, 512], F32)
        x1 = sc.sb("x1", [128, 8, 512], F32)
        hf = sc.sb("hf", [128, 8, 512], F32)
        tmp = {"sq": sc.sb("sq", [128, 8, 512], F32), "mean": sc.sb("mean", [128, 512], F32),
               "rstd": sc.sb("rstd", [128, 512], F32), "t": sc.sb("t", [128, 512], F32)}
        t2 = sc.sb("t2", [128, 512], F32)
        lg = sc.sb("lg", [32, 512], F32)
        top8 = sc.sb("top8", [128, 8], F32)
        sm = sc.sb("sm", [128, 4], F32)
        ex = sc.sb("ex", [128, 32], F32)
        msk = sc.sb("msk", [128, 32], F32)
        k.dma("gpsimd", wo.v(), io["w_out"].v().re("(c p) n -> p c n", p=128))
        with nc.allow_non_contiguous_dma(reason="router w"):
            k.dma("sync", rw.v(), io["rw"].v().re("(c p) n -> p c n", p=128))
        for (st, N, col) in blocks:
            if "load_xy" in io:
                io["load_xy"](xb, yb, st, N, s, tmp["sq"])
            else:
                k.dma("sync", xb[:, :, :N], io["xT"].v()[:, st:st + N].re("(c p) t -> p c t", p=128))
                k.dma("gpsimd", yb[:, :, :N], io["yT"].v()[:, st:st + N].re("(c p) t -> p c t", p=128))
            for dc in range(8):
                ps = PS[dc % 2]
                for kc in range(8):
                    k.mm(ps[:, :N], wo[:, kc, dc * 128:(dc + 1) * 128], yb[:, kc, :N], start=(kc == 0), stop=(kc == 7))
                k.act(t2[:, :N], ps[:, :N], AF.Identity, scale=mv(2, dc, col))
                k.stt(s[:, dc, :N], xb[:, dc, :N], DN_ALPHA, t2[:, :N], ALU.mult, ALU.add)

            def o1(ci, tv, N=N, col=col):
                k.act(x1[:, ci, :N], tv, AF.Identity, scale=lnp[:, ci:ci + 1], bias=lnp[:, 16 + ci:17 + ci])
                k.act(hf[:, ci, :N], x1[:, ci, :N], AF.Identity, scale=sc4[:, ci, col:col + 1], bias=mv(3, ci, col))
                k.copy(hT[:, ci, st - g0:st - g0 + N], hf[:, ci, :N], eng="gpsimd")
            if st == 128:
                k.dbg("s", s.v(), [128, 8, 512])
            ln_block(k, c, s, N, None, None, o1, tmp, PS[2], PS[3])
            if st == 128:
                k.dbg("x1", x1.v(), [128, 8, 512])
                k.dbg("hf", hf.v(), [128, 8, 512])
                k.dbg("mean", tmp["mean"].v(), [128, 512])
                k.dbg("rstd", tmp["rstd"].v(), [128, 512])
            k.dma("sync", io["x1d"].v()[:, st:st + N].re("(c p) t -> p c t", p=128), x1[:, :, :N])
            for kc in range(8):
                k.mm(PS[4][0:32, :N], rw[:, kc, :], hf[:, kc, :N], start=(kc == 0), stop=(kc == 7))
            k.act(lg[:, :N], PS[4][0:32, :N], AF.Identity, bias=rbs.v())
            for j in range(N // 128):
                pt = PS[5 + (j % 2)]
                k.tr(pt[:, 0:32], lg[:, j * 128:(j + 1) * 128], c["ident"][0:32, 0:32])
                k.op("vector", lambda e: e.max(top8.ap, pt.ap[:, 0:32]), [pt.v()], [top8.v()])
                k.ts(sm[:, 0:1], top8[:, 0:1], -1.0, None, ALU.mult)
                k.act(ex.v(), pt[:, 0:32], AF.Exp, bias=sm[:, 0:1])
                k.ts(msk.v(), pt[:, 0:32], top8[:, 3:4], None, ALU.is_ge)
                k.tt(ex.v(), ex.v(), msk.v(), ALU.mult)
                k.reduce(sm[:, 1:2], ex.v(), ALU.add)
                k.recip(sm[:, 2:3], sm[:, 1:2])
                k.ts(ex.v(), ex.v(), sm[:, 2:3], None, ALU.mult)
                pg = PS[7]
                k.tr(pg[0:32, 0:128], ex.v(), c["ident"].v())
                k.copy(gatesT[:, st - g0 + j * 128: st - g0 + (j + 1) * 128], pg[0:32, 0:128], eng="scalar")

    if g0 == 0:
        k.dbg("gatesT", gatesT.v(), [32, gatesT.ap.shape[1]])
    with k.scope() as sc:
        NR = 3
        w1r = [sc.sb(f"w1r{i}", [128, 8, 2048], BF16) for i in range(2)]
        w2r = [sc.sb(f"w2r{i}", [128, 8, 1024], BF16) for i in range(2)]
        actT = [sc.sb(f"actT{i}", [128, 8, 512], BF16) for i in range(2)]
        g = [sc.sb(f"g{i}", [128, 512], F32) for i in range(2)]
        sg = [sc.sb(f"sg{i}", [128, 512], F32) for i in range(2)]
        l = [sc.sb(f"l{i}", [128, 512], F32) for i in range(2)]
        ty = [sc.sb(f"ty{i}", [128, 512], F32) for i in range(2)]
        items = [(e, bi) for e in range(NE) for bi in range(len(blocks))]
        cnt = [0]

        def loadw(e):
            w1 = w1r[e % 2]
            w2 = w2r[e % 2]
            if "w1b" in io:
                cb1 = io["w1b"][e // 4]
                cb2 = io["w2b"][e // 8]
                k.dma("sync", w1.v(), V(cb1, cb1.ap.rearrange("(e c p h) n -> e p c (h n)", e=4, p=128, h=2)[e % 4]))
                k.dma("sync", w2.v(), V(cb2, cb2.ap.rearrange("(e c p) n -> e p c n", e=8, p=128)[e % 8]))
            else:
                for h in range(2):
                    k.dma("gpsimd", w1[:, :, h * 1024:(h + 1) * 1024],
                          io["w1"].v()[e, :, h * 1024:(h + 1) * 1024].re("(c p) n -> p c n", p=128))
                k.dma("gpsimd", w2.v(), io["w2"].v()[e].re("(c p) n -> p c n", p=128))

        def W1(j):
            e, bi = items[j]
            if bi == 0:
                loadw(e)
            w1 = w1r[e % 2]
            (st, N, col) = blocks[bi]
            a = actT[j % 2]
            for fc in range(8):
                pg_, pl_ = PS[(fc % 2) * 2], PS[(fc % 2) * 2 + 1]
                for kc in range(8):
                    k.mm(pg_[:, :N], w1[:, kc, fc * 128:(fc + 1) * 128], hT[:, kc, st - g0:st - g0 + N], start=(kc == 0), stop=(kc == 7))
                for kc in range(8):
                    k.mm(pl_[:, :N], w1[:, kc, 1024 + fc * 128:1024 + (fc + 1) * 128], hT[:, kc, st - g0:st - g0 + N], start=(kc == 0), stop=(kc == 7))
                i2 = cnt[0] % 2
                cnt[0] += 1
                k.ts(g[i2][:, :N], pg_[:, :N], b1s[:, e * 16 + fc:e * 16 + fc + 1], SW_LIM, ALU.add, ALU.min)
                k.act(sg[i2][:, :N], g[i2][:, :N], AF.Silu, scale=SW_ALPHA)
                k.ts(l[i2][:, :N], pl_[:, :N], b1s[:, e * 16 + 8 + fc:e * 16 + 8 + fc + 1], SW_LIM, ALU.add, ALU.min)
                k.ts(l[i2][:, :N], l[i2][:, :N], -SW_LIM, 1.0, ALU.max, ALU.add)
                k.stt(a[:, fc, :N], l[i2][:, :N], 1.0 / SW_ALPHA, sg[i2][:, :N], ALU.mult, ALU.mult)

        def W2(j):
            e, bi = items[j]
            w2 = w2r[e % 2]
            (st, N, col) = blocks[bi]
            a = actT[j % 2]
            pgb = PS[6]
            k.mm(pgb[:, :N], c["ident"][0:32, e:e + 1].bcast([32, 128]), gatesT[:, st - g0:st - g0 + N])
            for dc in range(8):
                py = PS[4 + (dc % 2)]
                for fc in range(8):
                    k.mm(py[:, :N], w2[:, fc, dc * 128:(dc + 1) * 128], a[:, fc, :N], start=(fc == 0), stop=(fc == 7))
                t = ty[dc % 2]
                k.act(t[:, :N], py[:, :N], AF.Identity, bias=b2s[:, e * 8 + dc:e * 8 + dc + 1])
                k.tt(t[:, :N], t[:, :N], pgb[:, :N], ALU.mult)
                k.tt(acc[:, dc, st - g0:st - g0 + N], acc[:, dc, st - g0:st - g0 + N], t[:, :N], ALU.add, eng="gpsimd")

        W1(0)
        for j in range(len(items)):
            if j + 1 < len(items):
                W1(j + 1)
            W2(j)

    if g0 == 0:
        k.dbg("acc", acc.v(), list(acc.ap.shape))
    with k.scope() as sc:
        x1 = sc.sb("x1b", [128, 8, 512], F32)
        s = sc.sb("s2", [128, 8, 512], F32)
        xo = sc.sb("xo", [128, 8, 512], F32)
        tmp = {"sq": sc.sb("sq2", [128, 8, 512], F32), "mean": sc.sb("mean2", [128, 512], F32),
               "rstd": sc.sb("rstd2", [128, 512], F32), "t": sc.sb("tt2", [128, 512], F32)}
        t2 = sc.sb("t22", [128, 512], F32)
        for (st, N, col) in blocks:
            k.dma("sync", x1[:, :, :N], io["x1d"].v()[:, st:st + N].re("(c p) t -> p c t", p=128))
            for dc in range(8):
                k.act(t2[:, :N], acc[:, dc, st - g0:st - g0 + N], AF.Identity, scale=mv(5, dc, col))
                k.stt(s[:, dc, :N], x1[:, dc, :N], DN_ALPHA, t2[:, :N], ALU.mult, ALU.add)

            def o2(ci, tv, N=N):
                k.act(xo[:, ci, :N], tv, AF.Identity, scale=lnp[:, 8 + ci:9 + ci], bias=lnp[:, 24 + ci:25 + ci])
            ln_block(k, c, s, N, None, None, o2, tmp, PS[2], PS[3])
            k.dma("sync", io["out"].v()[:, st:st + N].re("(c p) t -> p c t", p=128), xo[:, :, :N])


def cast_weights(k, io, tag):
    w1f = io["w1"].ap.rearrange("e k (h n) -> (e k h) n", h=2)
    w2f = io["w2"].ap.rearrange("e k n -> (e k) n")
    w1b = k.dram(f"w1b_{tag}", [65536, 1024], BF16, kind="Internal")
    w2b = k.dram(f"w2b_{tag}", [32768, 1024], BF16, kind="Internal")
    io["w1b"] = k.split(w1b, [w1b.ap[i * 8192:(i + 1) * 8192, :] for i in range(8)], name=f"w1b_{tag}")
    io["w2b"] = k.split(w2b, [w2b.ap[i * 8192:(i + 1) * 8192, :] for i in range(4)], name=f"w2b_{tag}")
    for i in range(8):
        k.dma("gpsimd", io["w1b"][i].v(), V(io["w1"], w1f[i * 8192:(i + 1) * 8192, :]))
    for i in range(4):
        k.dma("gpsimd", io["w2b"][i].v(), V(io["w2"], w2f[i * 8192:(i + 1) * 8192, :]))


T_CTX = 256
T_LAT = 4096
T_ALL = T_CTX + T_LAT
A_BLOCKS = [(0, 256, 1)] + [(256 + 512 * i, 512, 0) for i in range(8)]


def compute_mod(k, PS, io, mlist, modv):
    with k.scope() as sc:
        scs = sc.sb("scs", [128, 8, 2], F32)
        bm = sc.sb("bm", [128, 48], F32)
        wm = [sc.sb(f"wm{i}", [128, 8, 512], F32) for i in range(4)]
        k.dma("sync", scs.v(), io["sc"].v().re("p (c j) -> p c j", j=2))
        k.dma("sync", bm.v(), io["b_mod"].v())
        k.act(scs.v(), scs.v(), AF.Silu)
        pieces = [(m, h) for m in mlist for h in range(2)]

        def load(i):
            m, h = pieces[i]
            c0 = m * 1024 + h * 512
            k.dma("sync", wm[i % 4].v(), io["w_mod"].v()[:, c0:c0 + 512].re("(c p) n -> p c n", p=128))
        for i in range(min(3, len(pieces))):
            load(i)
        for i, (m, h) in enumerate(pieces):
            if i + 3 < len(pieces):
                load(i + 3)
            w = wm[i % 4]
            for d4 in range(4):
                dc = h * 4 + d4
                ps = PS[dc % 2]
                for kc in range(8):
                    k.mm(ps[:, 0:2], w[:, kc, d4 * 128:(d4 + 1) * 128], scs[:, kc, :], start=(kc == 0), stop=(kc == 7))
                j = m * 8 + dc
                k.act(modv[:, j, :], ps[:, 0:2], AF.Identity, bias=bm[:, j:j + 1])


def in_proj(k, PS, io, NC, pT):
    nc = k.nc
    with k.scope() as sc:
        if "modv" in io:
            modv = io["modv"]
        else:
            modv = sc.sb("modvA", [128, 48, 2], F32)
            compute_mod(k, PS, io, [0, 1], modv)
        sc1 = sc.sb("sc1A", [128, 8, 2], F32)
        k.ts(sc1.v(), modv[:, 8:16, :], 1.0, None, ALU.add)
        hT = sc.sb("hTA", [128, 8, T_ALL], BF16)
        xb = [sc.sb(f"xbA{i}", [128, 8, 512], F32) for i in range(2)]
        for bi, (st, N, col) in enumerate(A_BLOCKS):
            x = xb[bi % 2]
            k.dma("sync", x[:, :, :N], io["xT"].v()[:, st:st + N].re("(c p) t -> p c t", p=128))
            for ci in range(8):
                k.act(hT[:, ci, st:st + N], x[:, ci, :N], AF.Identity, scale=sc1[:, ci, col:col + 1],
                      bias=modv[:, ci, col:col + 1], eng=("scalar" if ci % 2 == 0 else "scalar"))
        wr = [sc.sb(f"wA{i}", [128, 8, 128], BF16) for i in range(3)]
        ob = [sc.sb(f"obA{i}", [128, 512], F32) for i in range(4)]
        it = 0
        for c in range(NC):
            w = wr[c % 3]
            with nc.allow_non_contiguous_dma(reason="w_in col chunk"):
                k.dma("gpsimd", w.v(), io["w_in"].v()[:, c * 128:(c + 1) * 128].re("(c p) n -> p c n", p=128))
            for (st, N, col) in A_BLOCKS:
                ps = PS[it % 4]
                o = ob[it % 4]
                it += 1
                for kc in range(8):
                    k.mm(ps[:, :N], w[:, kc, :], hT[:, kc, st:st + N], start=(kc == 0), stop=(kc == 7))
                k.copy(o[:, :N], ps[:, :N], eng=("scalar" if it % 2 == 0 else "vector"))
                k.dma("sync", pT.v()[c * 128:(c + 1) * 128, st:st + N], o[:, :N])


def rglru(k, PS, io, pT, c_u0, c_g0, yT, y_row0):
    T = T_ALL
    SEG = [(0, T_CTX), (T_CTX, T_ALL)]
    with k.scope() as sc:
        cv = sc.sb("cv", [128, 10], F32)
        gb = sc.sb("gb", [128, 8], F32)
        lam = sc.sb("lam", [128, 4], F32)
        one = sc.sb("one", [128, 1], F32)
        k.memset(one.v(), 1.0)
        k.dma("sync", cv.v(), io["conv"].v())
        k.dma("sync", gb.v(), io["gb"].v())
        k.dma("sync", lam.v(), io["lam"].v())
        k.act(lam.v(), lam.v(), AF.Exp, scale=-1.0)
        k.act(lam.v(), lam.v(), AF.Ln, bias=one.v())
        k.ts(lam.v(), lam.v(), -8.0, None, ALU.mult)
        urs = [sc.sb(f"ur{i}", [128, T], F32) for i in range(2)]
        u = sc.sb("u", [128, T], F32)
        gts = [sc.sb(f"gt{i}", [128, T], F32) for i in range(2)]
        HT = T // 2

        def ldrows(dst, row0):
            for hf in range(2):
                k.dma("sync", dst[:, hf * HT:(hf + 1) * HT], pT.v()[row0:row0 + 128, hf * HT:(hf + 1) * HT])
        for cc_ in range(2):
            ldrows(urs[cc_], (c_u0 + cc_) * 128)
            ldrows(gts[cc_], (c_g0 + cc_) * 128)
        a = sc.sb("a", [128, T], F32)
        bx = sc.sb("bx", [128, T], F32)
        hs = sc.sb("hs", [128, T], F32)
        hb = sc.sb("hb", [128, T], F32)
        wbd = sc.sb("wbd", [128, 4, 128], F32)
        t1 = [sc.sb(f"t1_{i}", [128, 512], F32) for i in range(2)]
        t2 = [sc.sb(f"t2_{i}", [128, 512], F32) for i in range(2)]
        for cc in range(2):
            ur, gt = urs[cc], gts[cc]
            k.dma("sync", wbd.v(), io["gwbd"].v()[cc].re("d s p n -> p (d s) n"))
            w = lambda j: cv[:, cc * 5 + j:cc * 5 + j + 1]
            k.ts(u.v(), ur.v(), w(2), w(4), ALU.mult, ALU.add)
            for (s0, s1) in SEG:
                k.stt(u[:, s0 + 2:s1], ur[:, s0:s1 - 2], w(0), u[:, s0 + 2:s1], ALU.mult, ALU.add)
                k.stt(u[:, s0 + 1:s1], ur[:, s0:s1 - 1], w(1), u[:, s0 + 1:s1], ALU.mult, ALU.add)
                k.stt(u[:, s0:s1 - 1], ur[:, s0 + 1:s1], w(3), u[:, s0:s1 - 1], ALU.mult, ALU.add)
            k.act(ur.v(), gt.v(), AF.Square)
            k.ts(ur.v(), ur.v(), 0.044715, 1.0, ALU.mult, ALU.add, eng="gpsimd")
            k.tt(ur.v(), ur.v(), gt.v(), ALU.mult, eng="gpsimd")
            k.act(ur.v(), ur.v(), AF.Sigmoid, scale=1.5957691216057308)
            k.tt(gt.v(), gt.v(), ur.v(), ALU.mult, eng="gpsimd")
            for d in range(2):
                it = 0
                for (st, N, col) in A_BLOCKS:
                    pr, pi = PS[(it % 2) * 2], PS[(it % 2) * 2 + 1]
                    r_, i_ = t1[it % 2], t2[it % 2]
                    it += 1
                    k.mm(pr[:, :N], wbd[:, d * 2 + 0, :], u[:, st:st + N])
                    k.mm(pi[:, :N], wbd[:, d * 2 + 1, :], u[:, st:st + N])
                    k.act(r_[:, :N], pr[:, :N], AF.Sigmoid, bias=gb[:, cc * 4 + d * 2:cc * 4 + d * 2 + 1])
                    k.act(i_[:, :N], pi[:, :N], AF.Sigmoid, bias=gb[:, cc * 4 + d * 2 + 1:cc * 4 + d * 2 + 2])
                    k.act(a[:, st:st + N], r_[:, :N], AF.Exp, scale=lam[:, cc * 2 + d:cc * 2 + d + 1])
                    k.act(r_[:, :N], a[:, st:st + N], AF.Square)
                    k.ts(r_[:, :N], r_[:, :N], -1.0, 1.0, ALU.mult, ALU.add)
                    k.act(r_[:, :N], r_[:, :N], AF.Sqrt)
                    k.tt(i_[:, :N], i_[:, :N], r_[:, :N], ALU.mult)
                    k.tt(bx[:, st:st + N], i_[:, :N], u[:, st:st + N], ALU.mult)
                if d == 0:
                    k.op("vector", lambda e: e.tensor_tensor_scan(hs.ap, a.ap, bx.ap, 0.0, ALU.mult, ALU.add),
                         [a.v(), bx.v()], [hs.v()])
                else:
                    ra, rb_, rh = a.ap[:, 0:T_CTX][:, ::-1], bx.ap[:, 0:T_CTX][:, ::-1], hb.ap[:, 0:T_CTX][:, ::-1]
                    k.op("vector", lambda e: e.tensor_tensor_scan(rh, ra, rb_, 0.0, ALU.mult, ALU.add),
                         [a.v(), bx.v()], [hb.v()])
                    ra, rb_, rh = a.ap[:, T_CTX:T][:, ::-1], bx.ap[:, T_CTX:T][:, ::-1], hb.ap[:, T_CTX:T][:, ::-1]
                    k.op("vector", lambda e: e.tensor_tensor_scan(rh, ra, rb_, hb.ap[:, 0:1], ALU.mult, ALU.add),
                         [a.v(), bx.v(), hb.v()], [hb.v()])
            k.tt(hs.v(), hs.v(), hb.v(), ALU.add)
            k.tt(hs.v(), hs.v(), gt.v(), ALU.mult)
            k.dma("sync", yT.v()[y_row0 + cc * 128:y_row0 + (cc + 1) * 128, :], hs.v())


A_DECAY = -0.6065306597126334
GN_EPS_RWKV = 64e-5
CH = 64
RB = 256


def rwkv_prep(k, PS, c, io, pT, g, PRE, GBd):
    T = T_ALL
    with k.scope() as sc:
        rp = sc.sb("rp", [128, 16], F32)
        m42 = sc.sb("m42", [128, 6], F32)
        mus = sc.sb("mus", [128, 6, 7], F32)
        wBs = sc.sb("wBs", [128, 128], F32)
        aBs = sc.sb("aBs", [128, 128], F32)
        gBs = sc.sb("gBs", [128, 128], F32)
        k.dma("sync", rp.v(), io["rp"].v()[g])
        k.dma("sync", m42.v(), io["m42"].v())
        k.dma("sync", wBs.v(), io["wBs"].v()[g])
        k.dma("sync", aBs.v(), io["aBs"].v()[g])
        k.dma("sync", gBs.v(), io["gBs"].v()[g])
        for ty in range(6):
            k.ts(mus[:, ty, 0:1], rp[:, ty:ty + 1], -1.0, 1.0, ALU.mult, ALU.add)
            k.ts(mus[:, ty, 1:7], m42.v(), rp[:, ty:ty + 1], None, ALU.mult)
        pts = [sc.sb(f"ptmp{i}", [128, T], F32) for i in range(2)]
        z = [sc.sb(f"z{ty}", [128, T], F32) for ty in range(6)]
        chunk_ids = [g, 2 + g, 4 + g, 6, 7, 8]
        HT = T // 2

        def ldp(ty_):
            cid_ = chunk_ids[ty_]
            for hf in range(2):
                k.dma("sync", pts[ty_ % 2][:, hf * HT:(hf + 1) * HT], pT.v()[cid_ * 128:(cid_ + 1) * 128, hf * HT:(hf + 1) * HT])
        ldp(0)
        for ty in range(6):
            if ty + 1 < 6:
                ldp(ty + 1)
            pt = pts[ty % 2]
            zz = z[ty]
            eng = "vector"
            k.ts(zz.v(), pt.v(), mus[:, ty, 0:1], None, ALU.mult, eng=eng)
            pl = pt[:, T_CTX:T].re("p (r c) -> p r c", c=64)
            zl = zz[:, T_CTX:T].re("p (r c) -> p r c", c=64)
            k.stt(zl[:, :, 1:64], pl[:, :, 0:63], mus[:, ty, 1:2], zl[:, :, 1:64], ALU.mult, ALU.add)
            k.stt(zl[:, :, 0:63], pl[:, :, 1:64], mus[:, ty, 2:3], zl[:, :, 0:63], ALU.mult, ALU.add)
            k.stt(zl[:, 1:64, :], pl[:, 0:63, :], mus[:, ty, 3:4], zl[:, 1:64, :], ALU.mult, ALU.add)
            k.stt(zl[:, 0:63, :], pl[:, 1:64, :], mus[:, ty, 4:5], zl[:, 0:63, :], ALU.mult, ALU.add)
            k.stt(zz[:, 1:T_CTX], pt[:, 0:T_CTX - 1], mus[:, ty, 5:6], zz[:, 1:T_CTX], ALU.mult, ALU.add)
            k.stt(zz[:, 0:T_CTX - 1], pt[:, 1:T_CTX], mus[:, ty, 6:7], zz[:, 0:T_CTX - 1], ALU.mult, ALU.add)
        zr, zk, zv, zwd, zad, zg = z
        k.dma("sync", PRE.v()[0:128, :], zr.v())
        k.dma("sync", PRE.v()[128:256, :], zv.v())
        k.act(zwd.v(), zwd.v(), AF.Tanh)
        k.act(zg.v(), zg.v(), AF.Sigmoid)
        pt = pts[0]
        k.ts(pt.v(), zk.v(), rp[:, 10:11], None, ALU.mult)
        tb = [[sc.sb(f"tb{i}_{j}", [128, 512], F32) for j in range(2)] for i in range(10)]
        for bi, (st, N, col) in enumerate(A_BLOCKS):
            j2 = bi % 2
            t = [tb[i][j2] for i in range(10)]
            sl = slice(st, st + N)
            k.act(t[0][:, :N], pt[:, sl], AF.Square)
            k.mm(PS[0][:, :N], c["bones"].v(), t[0][:, :N])
            k.act(t[0][:, :N], PS[0][:, :N], AF.Sqrt)
            k.ts(t[0][:, :N], t[0][:, :N], 1e-12, None, ALU.max)
            k.recip(t[0][:, :N], t[0][:, :N])
            k.tt(t[0][:, :N], pt[:, sl], t[0][:, :N], ALU.mult)
            k.dma("sync", PRE.v()[256:384, sl], t[0][:, :N])
            k.mm(PS[1][:, :N], gBs.v(), zg[:, sl])
            k.copy(t[1][:, :N], PS[1][:, :N], eng="scalar")
            k.dma("sync", GBd.v()[0:128, sl], t[1][:, :N])
            for d in range(2):
                ds = slice(d * 64, (d + 1) * 64)
                pw, pa = PS[2 + d * 2], PS[3 + d * 2]
                k.mm(pw[:, :N], wBs[ds, :], zwd[ds, sl])
                k.mm(pa[:, :N], aBs[ds, :], zad[ds, sl])
                lw, av, kd, bb = t[2 + d * 3], t[8], t[3 + d * 3], t[4 + d * 3]
                k.act(lw[:, :N], pw[:, :N], AF.Sigmoid, bias=rp[:, 6 + d:7 + d])
                k.ts(lw[:, :N], lw[:, :N], A_DECAY, None, ALU.mult, eng="gpsimd")
                k.dma("sync", PRE.v()[(5 + 3 * d) * 128:(6 + 3 * d) * 128, sl], lw[:, :N])
                k.act(av[:, :N], pa[:, :N], AF.Sigmoid, bias=rp[:, 8 + d:9 + d])
                k.tt(bb[:, :N], t[0][:, :N], av[:, :N], ALU.mult)
                k.dma("sync", PRE.v()[(4 + 3 * d) * 128:(5 + 3 * d) * 128, sl], bb[:, :N])
                k.ts(kd[:, :N], av[:, :N], -1.0, rp[:, 11:12], ALU.add, ALU.mult)
                k.stt(kd[:, :N], kd[:, :N], 1.0, zk[:, sl], ALU.add, ALU.mult)
                k.dma("sync", PRE.v()[(3 + 3 * d) * 128:(4 + 3 * d) * 128, sl], kd[:, :N])
            k.tt(t[9][:, :N], t[3][:, :N], t[6][:, :N], ALU.add)
            k.stt(t[9][:, :N], zr[:, sl], rp[:, 12:13], t[9][:, :N], ALU.mult, ALU.mult)
            k.mm(PS[6][:, :N], c["bones"].v(), t[9][:, :N])
            k.tt(t[1][:, :N], PS[6][:, :N], zv[:, sl], ALU.mult)
            k.dma("sync", GBd.v()[128:256, sl], t[1][:, :N])


import os
USTEP = int(os.environ.get("USTEP", "5"))
NCH = int(os.environ.get("NCH", "4"))
NBK = int(os.environ.get("NBK", "100"))


class Chain:
    pass


def rwkv_scan(k, PSq, c, PREs, wkv):
    T = T_ALL
    NBLK = T // RB
    chains = []
    sc = k.scope()
    sc.__enter__()
    for g in range(2):
        for d in range(2):
            ch = Chain()
            ch.g, ch.d = g, d
            nm = f"c{g}{d}"
            ch.H = sc.sb(nm + "H", [128, 128], F32)
            k.memset(ch.H.v(), 0.0)
            ch.blk = [sc.sb(nm + f"blk{i}", [128, 6, RB], F32) for i in range(2)]
            ch.Lb = [sc.sb(nm + f"L{i}", [128, 4, RB], F32) for i in range(2)]
            names = ["kapT", "rT", "ktT", "btT", "khT", "bhT", "vT", "kh", "nbh", "V", "AkkT", "ArkT", "nArbT", "W", "U"]
            ch.t = {n: sc.sb(nm + n, [128, 128], F32) for n in names}
            for n in ["kapT", "rT", "ktT", "btT", "khT", "bhT", "vT"]:
                k.memset(ch.t[n].v(), 0.0, eng="gpsimd")
            ch.P = [sc.sb(nm + f"P{i}", [128, 128], F32) for i in range(2)]
            ch.PT = [sc.sb(nm + f"PT{i}", [128, 128], F32) for i in range(2)]
            ch.R = [sc.sb(nm + f"R{i}", [128, 128], F32) for i in range(2)]
            if d == 0:
                ch.order = list(range(NBLK))
            else:
                ch.order = [0] + list(range(NBLK - 1, 0, -1))
            chains.append(ch)
    psn = [0]

    def ps():
        p = PSq[psn[0] % len(PSq)]
        psn[0] += 1
        return p

    def load_block(ch, i):
        b = ch.order[i]
        buf = ch.blk[i % 2]
        rows = [0, 1, 2, 3 + 3 * ch.d, 4 + 3 * ch.d, 5 + 3 * ch.d]
        for j, r in enumerate(rows):
            k.dma("sync", buf[:, j, :], PREs[ch.g].v()[r * 128:(r + 1) * 128, b * RB:(b + 1) * RB])
        Lb = ch.Lb[i % 2]
        lw = buf[:, 5, :]
        if ch.d == 0:
            k.op("vector", lambda e: e.tensor_tensor_scan(Lb.ap[:, 0, :], c["rmask"].ap[:, 0:RB], lw.ap, 0.0, ALU.mult, ALU.add),
                 [lw, c["rmask"].v()], [Lb.v()])
        else:
            k.op("vector", lambda e: e.tensor_tensor_scan(Lb.ap[:, 0, :][:, ::-1], c["rmask"].ap[:, 0:RB], lw.ap[:, ::-1], 0.0, ALU.mult, ALU.add),
                 [lw, c["rmask"].v()], [Lb.v()])
        k.tt(Lb[:, 1, :], Lb[:, 0, :], lw, ALU.subtract, eng="gpsimd")
        k.act(Lb[:, 1, :], Lb[:, 1, :], AF.Exp)
        k.act(Lb[:, 2, :], Lb[:, 0, :], AF.Exp)
        k.act(Lb[:, 3, :], Lb[:, 0, :], AF.Exp, scale=-1.0)

    def unit(ch, i, j):
        d = ch.d
        buf = ch.blk[i % 2]
        Lb = ch.Lb[i % 2]
        cj = j if d == 0 else (RB // CH - 1 - j)
        c0 = cj * CH
        cs = slice(c0, c0 + CH)
        b = ch.order[i]
        tok0 = b * RB + c0
        last = c0 + CH - 1 if d == 0 else c0
        t = ch.t
        eLC = Lb[:, 2, last:last + 1]
        for h in range(2):
            ps_ = slice(h * 64, (h + 1) * 64)
            e1 = "vector" if h == 0 else "gpsimd"
            e2 = "gpsimd" if h == 0 else "vector"
            k.tt(t["kapT"][ps_, ps_], buf[ps_, 2, cs], Lb[ps_, 1, cs], ALU.mult, eng=e1)
            k.tt(t["rT"][ps_, ps_], buf[ps_, 0, cs], Lb[ps_, 2, cs], ALU.mult, eng=e2)
            k.tt(t["ktT"][ps_, ps_], buf[ps_, 3, cs], Lb[ps_, 3, cs], ALU.mult, eng=e1)
            k.tt(t["btT"][ps_, ps_], buf[ps_, 4, cs], Lb[ps_, 3, cs], ALU.mult, eng=e2)
            k.ts(t["khT"][ps_, ps_], t["ktT"][ps_, ps_], eLC[ps_, :], None, ALU.mult, eng=e1)
            k.ts(t["bhT"][ps_, ps_], t["btT"][ps_, ps_], eLC[ps_, :], None, ALU.mult, eng=e2)
            k.copy(t["vT"][ps_, ps_], buf[ps_, 1, cs], eng=e1)
        ident = c["ident"].v()
        if USTEP < 2:
            return
        p1, p2, p3 = ps(), ps(), ps()
        USUB = int(os.environ.get("USUB", "9"))
        k.tr(p1.v(), t["khT"].v(), ident)
        if USUB >= 2:
            k.tr(p2.v(), t["bhT"].v(), ident)
            k.tr(p3.v(), t["vT"].v(), ident)
        if USUB >= 3:
            k.copy(t["kh"].v(), p1.v(), eng="scalar")
        if USUB >= 4:
            k.act(t["nbh"].v(), p2.v(), AF.Identity, scale=-1.0)
            k.copy(t["V"].v(), p3.v(), eng="scalar")
        if USTEP < 3:
            return
        if d == 0:
            ms, msT, mi, nms, nmsT, nmi = c["MUs"], c["MLs"], c["MUi"], c["nMUs"], c["nMLs"], c["nMUi"]
        else:
            ms, msT, mi, nms, nmsT, nmi = c["MLs"], c["MUs"], c["MLi"], c["nMLs"], c["nMUs"], c["nMLi"]
        q1, q2, q3, q4, q5 = ps(), ps(), ps(), ps(), ps()
        k.mm(q1.v(), t["btT"].v(), t["kapT"].v())
        k.mm(q2.v(), t["kapT"].v(), t["btT"].v())
        k.mm(q3.v(), t["ktT"].v(), t["kapT"].v())
        k.mm(q4.v(), t["ktT"].v(), t["rT"].v())
        k.mm(q5.v(), t["btT"].v(), t["rT"].v())
        P, PT, R = ch.P, ch.PT, ch.R
        k.tt(P[0].v(), q1.v(), nms.v(), ALU.mult)
        k.tt(PT[0].v(), q2.v(), nmsT.v(), ALU.mult)
        k.tt(t["AkkT"].v(), q3.v(), ms.v(), ALU.mult)
        k.tt(t["ArkT"].v(), q4.v(), mi.v(), ALU.mult)
        k.tt(t["nArbT"].v(), q5.v(), nmi.v(), ALU.mult)
        k.tt(R[0].v(), P[0].v(), ident, ALU.add, eng="gpsimd")
        cur = 0
        if USTEP < 4:
            return
        for it in range(5):
            nx = 1 - cur
            a1, a2 = ps(), ps()
            k.mm(a1.v(), PT[cur].v(), P[cur].v())
            k.mm(a2.v(), P[cur].v(), PT[cur].v())
            k.copy(PT[nx].v(), a2.v(), eng="scalar")
            k.copy(P[nx].v(), a1.v(), eng="vector")
            a3 = ps()
            k.mm(a3.v(), PT[nx].v(), R[cur].v())
            k.tt(R[nx].v(), R[cur].v(), a3.v(), ALU.add)
            cur = nx
        Tt = R[cur]
        if USTEP < 5:
            return
        w_ps = ps()
        k.mm(w_ps.v(), t["kapT"].v(), ch.H.v(), start=True, stop=False)
        k.mm(w_ps.v(), t["AkkT"].v(), t["V"].v(), start=False, stop=True)
        k.copy(t["W"].v(), w_ps.v(), eng="scalar")
        u_ps = ps()
        k.mm(u_ps.v(), Tt.v(), t["W"].v())
        k.copy(t["U"].v(), u_ps.v(), eng="vector")
        o_ps = ps()
        k.mm(o_ps.v(), ch.H.v(), t["rT"].v(), start=True, stop=False)
        k.mm(o_ps.v(), t["V"].v(), t["ArkT"].v(), start=False, stop=False)
        k.mm(o_ps.v(), t["U"].v(), t["nArbT"].v(), start=False, stop=True)
        h_ps = ps()
        k.mm(h_ps.v(), t["kh"].v(), t["V"].v(), start=True, stop=False)
        k.mm(h_ps.v(), t["nbh"].v(), t["U"].v(), start=False, stop=True)
        wk = wkv[ch.g]
        for h in range(2):
            ps_ = slice(h * 64, (h + 1) * 64)
            k.tt(wk[ps_, tok0:tok0 + CH], wk[ps_, tok0:tok0 + CH], o_ps[ps_, ps_], ALU.add)
        k.stt(ch.H.v(), ch.H.v(), eLC, h_ps.v(), ALU.mult, ALU.add)

    NB = min(T // RB, NBK)
    chains = chains[:NCH]
    for ch in chains:
        load_block(ch, 0)
    for i in range(NB):
        for ch in chains:
            if i + 1 < NB:
                load_block(ch, i + 1)
        for j in range(RB // CH):
            for ch in chains:
                unit(ch, i, j)
    sc.__exit__(None, None, None)


def rwkv_finish(k, PS, c, io, g, GBd, wkv, yT, row0):
    T = T_ALL
    with k.scope() as sc:
        rp = sc.sb("rpf", [128, 16], F32)
        k.dma("sync", rp.v(), io["rp"].v()[g])
        eps = sc.sb("epsf", [128, 1], F32)
        k.memset(eps.v(), GN_EPS_RWKV)
        tb = [[sc.sb(f"tf{i}_{j}", [128, 512], F32) for j in range(2)] for i in range(5)]
        for bi, (st, N, col) in enumerate(A_BLOCKS):
            sl = slice(st, st + N)
            t = [tb[i][bi % 2] for i in range(5)]
            k.dma("sync", t[3][:, :N], GBd.v()[0:128, sl])
            k.dma("sync", t[4][:, :N], GBd.v()[128:256, sl])
            w = wkv[g]
            k.mm(PS[0][:, :N], c["bones64"].v(), w[:, sl])
            k.act(t[0][:, :N], w[:, sl], AF.Square)
            k.mm(PS[1][:, :N], c["bones64"].v(), t[0][:, :N])
            k.copy(t[1][:, :N], PS[0][:, :N], eng="scalar")
            k.tt(t[2][:, :N], t[1][:, :N], t[1][:, :N], ALU.mult)
            k.tt(t[2][:, :N], PS[1][:, :N], t[2][:, :N], ALU.subtract)
            k.act(t[2][:, :N], t[2][:, :N], AF.Sqrt, bias=eps.v())
            k.recip(t[2][:, :N], t[2][:, :N])
            k.tt(t[0][:, :N], w[:, sl], t[1][:, :N], ALU.subtract)
            k.tt(t[0][:, :N], t[0][:, :N], t[2][:, :N], ALU.mult)
            k.act(t[0][:, :N], t[0][:, :N], AF.Identity, scale=rp[:, 13:14], bias=rp[:, 14:15])
            k.tt(t[0][:, :N], t[0][:, :N], t[4][:, :N], ALU.add)
            k.tt(t[0][:, :N], t[0][:, :N], t[3][:, :N], ALU.mult)
            k.dma("sync", yT.v()[row0:row0 + 128, sl], t[0][:, :N])


def rwkv_scan2(k, PSb, c, PREs, wkv):
    T = T_ALL
    NBLK = T // RB
    UPB = RB // CH
    sc = k.scope()
    sc.__enter__()
    chains = []
    for g in range(2):
        for d in range(2):
            ch = Chain()
            ch.g, ch.d = g, d
            nm = f"s{g}{d}"
            ch.H = sc.sb(nm + "H", [128, 128], F32)
            k.memset(ch.H.v(), 0.0)
            ch.blk = [sc.sb(nm + f"blk{i}", [128, 6, RB], F32) for i in range(2)]
            ch.Lb = [sc.sb(nm + f"L{i}", [128, 4, RB], F32) for i in range(2)]
            ch.kapT = [sc.sb(nm + f"kapT{i}", [128, 128], F32) for i in range(2)]
            ch.rT = [sc.sb(nm + f"rT{i}", [128, 128], F32) for i in range(2)]
            ch.t = {n: sc.sb(nm + n, [128, 128], F32) for n in ["ktT", "btT", "khT", "nbhT", "vT", "W", "U"]}
            for tl in ch.kapT + ch.rT + [ch.t[n] for n in ["ktT", "btT", "khT", "nbhT", "vT"]]:
                k.memset(tl.v(), 0.0, eng="gpsimd")
            ch.TRS = [sc.sb(nm + f"TRS{i}", [128, 3, 128], F32) for i in range(2)]
            ch.AAA = [sc.sb(nm + f"AAA{i}", [128, 3, 128], F32) for i in range(2)]
            ch.PP = [sc.sb(nm + f"PP{i}", [128, 2, 128], F32) for i in range(2)]
            ch.R = [[sc.sb(nm + f"R{s_}{i}", [128, 128], F32) for i in range(2)] for s_ in range(2)]
            ch.order = list(range(NBLK)) if d == 0 else [0] + list(range(NBLK - 1, 0, -1))
            chains.append(ch)
    psn = [0]

    def ps():
        p = PSb[psn[0] % len(PSb)]
        psn[0] += 1
        return p

    def load_block(ch, i):
        b = ch.order[i]
        buf = ch.blk[i % 2]
        rows = [0, 1, 2, 3 + 3 * ch.d, 4 + 3 * ch.d, 5 + 3 * ch.d]
        for j, r in enumerate(rows):
            k.dma("sync", buf[:, j, :], PREs[ch.g].v()[r * 128:(r + 1) * 128, b * RB:(b + 1) * RB])
        Lb = ch.Lb[i % 2]
        lw = buf[:, 5, :]
        if ch.d == 0:
            k.op("vector", lambda e: e.tensor_tensor_scan(Lb.ap[:, 0, :], c["rmask"].ap[:, 0:RB], lw.ap, 0.0, ALU.mult, ALU.add),
                 [lw, c["rmask"].v()], [Lb.v()])
        else:
            k.op("vector", lambda e: e.tensor_tensor_scan(Lb.ap[:, 0, :][:, ::-1], c["rmask"].ap[:, 0:RB], lw.ap[:, ::-1], 0.0, ALU.mult, ALU.add),
                 [lw, c["rmask"].v()], [Lb.v()])
        k.tt(Lb[:, 1, :], Lb[:, 0, :], lw, ALU.subtract)
        k.act(Lb[:, 1, :], Lb[:, 1, :], AF.Exp)
        k.act(Lb[:, 2, :], Lb[:, 0, :], AF.Exp)
        k.act(Lb[:, 3, :], Lb[:, 0, :], AF.Exp, scale=-1.0)

    def geom(ch, n):
        i, j = n // UPB, n % UPB
        d = ch.d
        cj = j if d == 0 else (UPB - 1 - j)
        c0 = cj * CH
        last = c0 + CH - 1 if d == 0 else c0
        return i, slice(c0, c0 + CH), ch.order[i] * RB + c0, last

    ident = c["ident"].v()

    def A1(ch, n):
        i, cs, tok0, last = geom(ch, n)
        s_ = n % 2
        buf, Lb, t = ch.blk[i % 2], ch.Lb[i % 2], ch.t
        eLC = Lb[:, 2, last:last + 1]
        for h in range(2):
            p_ = slice(h * 64, (h + 1) * 64)
            e1 = "vector" if h == 0 else "gpsimd"
            e2 = "gpsimd" if h == 0 else "vector"
            k.tt(ch.kapT[s_][p_, p_], buf[p_, 2, cs], Lb[p_, 1, cs], ALU.mult, eng=e1)
            k.tt(ch.rT[s_][p_, p_], buf[p_, 0, cs], Lb[p_, 2, cs], ALU.mult, eng=e2)
            k.tt(t["ktT"][p_, p_], buf[p_, 3, cs], Lb[p_, 3, cs], ALU.mult, eng=e1)
            k.tt(t["btT"][p_, p_], buf[p_, 4, cs], Lb[p_, 3, cs], ALU.mult, eng=e2)
            k.act(t["khT"][p_, p_], t["ktT"][p_, p_], AF.Identity, scale=eLC[p_, :])
            k.act(t["nbhT"][p_, p_], t["btT"][p_, p_], AF.Identity, scale=eLC[p_, :])
            k.copy(t["vT"][p_, p_], buf[p_, 1, cs], eng=e1)

    def A2(ch, n):
        s_ = n % 2
        t = ch.t
        ch.pT3, ch.pPP, ch.pA3 = ps(), ps(), ps()
        k.tr(ch.pT3[:, 0:128], t["khT"].v(), ident)
        k.tr(ch.pT3[:, 128:256], t["nbhT"].v(), ident)
        k.tr(ch.pT3[:, 256:384], t["vT"].v(), ident)
        k.mm(ch.pPP[:, 0:128], t["btT"].v(), ch.kapT[s_].v())
        k.mm(ch.pPP[:, 128:256], ch.kapT[s_].v(), t["btT"].v())
        k.mm(ch.pA3[:, 0:128], t["ktT"].v(), ch.kapT[s_].v())
        k.mm(ch.pA3[:, 128:256], t["ktT"].v(), ch.rT[s_].v())
        k.mm(ch.pA3[:, 256:384], t["btT"].v(), ch.rT[s_].v())

    def A3(ch, n):
        s_ = n % 2
        f = "f" if ch.d == 0 else "b"
        k.copy(ch.TRS[s_].v().re("p a b -> p (a b)"), ch.pT3[:, 0:384], eng="scalar")
        k.tt(ch.PP[0].v().re("p a b -> p (a b)"), ch.pPP[:, 0:256], c["np2" + f].v(), ALU.mult)
        k.tt(ch.AAA[s_].v().re("p a b -> p (a b)"), ch.pA3[:, 0:384], c["m3" + f].v(), ALU.mult)
        k.tt(ch.R[s_][0].v(), ch.PP[0][:, 0, :], ident, ALU.add, eng="gpsimd")

    def Na(ch, n, it):
        cur = it % 2
        ch.pN = ps()
        k.mm(ch.pN[:, 0:128], ch.PP[cur][:, 1, :], ch.PP[cur][:, 0, :])
        k.mm(ch.pN[:, 128:256], ch.PP[cur][:, 0, :], ch.PP[cur][:, 1, :])

    def Nb(ch, n, it):
        nx = 1 - it % 2
        k.copy(ch.PP[nx].v().re("p a b -> p (a b)"), ch.pN[:, 0:256], eng=("scalar" if (it + ch.g) % 2 == 0 else "vector"))

    def Nc(ch, n, it):
        s_ = n % 2
        nx = 1 - it % 2
        ch.pR = ps()
        k.mm(ch.pR[:, 0:128], ch.PP[nx][:, 1, :], ch.R[s_][it % 2].v())

    def Nd(ch, n, it):
        s_ = n % 2
        k.tt(ch.R[s_][1 - it % 2].v(), ch.R[s_][it % 2].v(), ch.pR[:, 0:128], ALU.add)

    def B1(ch, n):
        s_ = n % 2
        ch.pW = ps()
        k.mm(ch.pW[:, 0:128], ch.kapT[s_].v(), ch.H.v(), start=True, stop=False)
        k.mm(ch.pW[:, 0:128], ch.AAA[s_][:, 0, :], ch.TRS[s_][:, 2, :], start=False, stop=True)

    def B2(ch, n):
        k.copy(ch.t["W"].v(), ch.pW[:, 0:128], eng="scalar")

    def B3(ch, n):
        s_ = n % 2
        ch.pU = ps()
        k.mm(ch.pU[:, 0:128], ch.R[s_][1].v(), ch.t["W"].v())

    def B4(ch, n):
        k.act(ch.t["U"].v(), ch.pU[:, 0:128], AF.Identity, scale=-1.0)

    def B5(ch, n):
        s_ = n % 2
        V_ = ch.TRS[s_][:, 2, :]
        ch.pO, ch.pH = ps(), ps()
        k.mm(ch.pO[:, 0:128], ch.H.v(), ch.rT[s_].v(), start=True, stop=False)
        k.mm(ch.pO[:, 0:128], V_, ch.AAA[s_][:, 1, :], start=False, stop=False)
        k.mm(ch.pO[:, 0:128], ch.t["U"].v(), ch.AAA[s_][:, 2, :], start=False, stop=True)
        k.mm(ch.pH[:, 0:128], ch.TRS[s_][:, 0, :], V_, start=True, stop=False)
        k.mm(ch.pH[:, 0:128], ch.TRS[s_][:, 1, :], ch.t["U"].v(), start=False, stop=True)

    def B6(ch, n):
        i, cs, tok0, last = geom(ch, n)
        eLC = ch.Lb[i % 2][:, 2, last:last + 1]
        wk = wkv[ch.g]
        for h in range(2):
            p_ = slice(h * 64, (h + 1) * 64)
            k.tt(wk[p_, tok0:tok0 + CH], wk[p_, tok0:tok0 + CH], ch.pO[p_, p_], ALU.add)
        k.stt(ch.H.v(), ch.H.v(), eLC, ch.pH[:, 0:128], ALU.mult, ALU.add)

    NU = NBLK * UPB

    def partA(n):
        if n == 0:
            for ch in chains:
                load_block(ch, 0)
        if n % UPB == 1:
            i = n // UPB
            for ch in chains:
                if i + 1 < NBLK:
                    load_block(ch, i + 1)
        for ch in chains:
            A1(ch, n)
        for ch in chains:
            A2(ch, n)
            A3(ch, n)
        for it in range(5):
            for st in (Na, Nb, Nc, Nd):
                for ch in chains:
                    st(ch, n, it)

    def partB(n):
        for st in (B1, B2, B3, B4, B5, B6):
            for ch in chains:
                st(ch, n)

    partA(0)
    for n in range(NU):
        if n + 1 < NU:
            partA(n + 1)
        partB(n)
    sc.__exit__(None, None, None)


LN_EPS = 1e-5
CK = 128
NCHUNK = T_ALL // CK
KSCALE = 128 ** -0.5
NEG = -1.0e30
RQ, RQS, RK, RKS, RV, RG = 0, 2, 4, 6, 8, 10
MQ, MK, MV, MO, GF0, GI0, GF1, GI1 = 12, 14, 16, 18, 20, 21, 22, 23
NC_ODD = 24
ORDER = {0: list(range(NCHUNK)), 1: [1, 0] + list(range(NCHUNK - 1, 1, -1))}


def rope_pass(k, io, pT, QK):
    with k.scope() as sc:
        a = [sc.sb(f"ra{i}", [128, 512], F32) for i in range(2)]
        b = [sc.sb(f"rb{i}", [128, 512], F32) for i in range(2)]
        cc = [sc.sb(f"rcc{i}", [128, 512], F32) for i in range(2)]
        ss = [sc.sb(f"rss{i}", [128, 512], F32) for i in range(2)]
        it = 0
        for (st, N, col) in A_BLOCKS:
            sl = slice(st, st + N)
            i2 = it % 2
            it += 1
            if col == 0:
                k.dma("sync", cc[i2][:, :N], io["ropeC"].v()[:, st - T_CTX:st - T_CTX + N])
                k.dma("sync", ss[i2][:, :N], io["ropeS"].v()[:, st - T_CTX:st - T_CTX + N])
            for hh in range(2):
                for (src, swp, dst, scale) in [(RQ + hh, RQS + hh, 0 + hh, 1.0), (RK + hh, RKS + hh, 2 + hh, KSCALE)]:
                    ta, tb_ = a[(hh) % 2], b[(hh) % 2]
                    k.dma("sync", ta[:, :N], pT.v()[src * 128:(src + 1) * 128, sl])
                    if col == 0:
                        k.dma("sync", tb_[:, :N], pT.v()[swp * 128:(swp + 1) * 128, sl])
                        k.tt(ta[:, :N], ta[:, :N], cc[i2][:, :N], ALU.mult)
                        k.tt(tb_[:, :N], tb_[:, :N], ss[i2][:, :N], ALU.mult, eng="gpsimd")
                        k.tt(ta[:, :N], ta[:, :N], tb_[:, :N], ALU.add)
                    if scale != 1.0:
                        k.ts(ta[:, :N], ta[:, :N], scale, None, ALU.mult)
                    k.dma("sync", QK.v()[dst * 128:(dst + 1) * 128, sl], ta[:, :N])
                ta = a[hh % 2]
                k.dma("sync", ta[:, :N], pT.v()[(MK + hh) * 128:(MK + hh + 1) * 128, sl])
                k.ts(ta[:, :N], ta[:, :N], KSCALE, None, ALU.mult)
                k.dma("sync", QK.v()[(4 + hh) * 128:(5 + hh) * 128, sl], ta[:, :N])


def retention(k, PS, c, io, pT, QK, oacc):
    sc = k.scope()
    sc.__enter__()
    lg = sc.sb("lgc", [128, 4], F32)
    k.dma("sync", lg.v(), io["lgc"].v())
    chains = []
    for hh in range(2):
        for d in range(2):
            ch = type("C", (), {})()
            ch.hh, ch.d = hh, d
            nm = f"r{hh}{d}"
            l = lg[:, hh * 2 + d:hh * 2 + d + 1]
            ch.Dm = sc.sb(nm + "Dm", [128, 128], F32)
            ch.XI = sc.sb(nm + "XI", [128, 128], F32)
            ch.zg = sc.sb(nm + "zg", [128, 2], F32)
            k.act(ch.Dm.v(), c["ABSD"].v(), AF.Exp, scale=l)
            k.tt(ch.Dm.v(), ch.Dm.v(), (c["MUi"] if d == 0 else c["MLi"]).v(), ALU.mult)
            k.act(ch.XI.v(), (c["POS1F"] if d == 0 else c["POS1B"]).v(), AF.Exp, scale=l)
            k.act(ch.zg[:, 0:1], c["ZP"][:, (0 if d == 0 else 1):(1 if d == 0 else 2)], AF.Exp, scale=l)
            k.act(ch.zg[:, 1:2], c["ZP"][:, 2:3], AF.Exp, scale=l)
            ch.R = sc.sb(nm + "R", [128, 128], F32)
            k.memset(ch.R.v(), 0.0)
            ch.q = [sc.sb(nm + f"q{i}", [128, 128], F32) for i in range(2)]
            ch.k = [sc.sb(nm + f"k{i}", [128, 128], F32) for i in range(2)]
            ch.v = [sc.sb(nm + f"v{i}", [128, 128], F32) for i in range(2)]
            ch.Vt = [sc.sb(nm + f"Vt{i}", [128, 128], F32) for i in range(2)]
            ch.Kz = [sc.sb(nm + f"Kz{i}", [128, 128], F32) for i in range(2)]
            ch.St = [sc.sb(nm + f"St{i}", [128, 128], F32) for i in range(2)]
            ch.qx = [sc.sb(nm + f"qx{i}", [128, 128], F32) for i in range(2)]
            chains.append(ch)
    psn = [0]

    def ps():
        p = PS[psn[0] % len(PS)]
        psn[0] += 1
        return p

    def load(ch, i):
        n = ORDER[ch.d][i]
        sl = slice(n * CK, (n + 1) * CK)
        k.dma("sync", ch.q[i % 2].v(), QK.v()[(0 + ch.hh) * 128:(1 + ch.hh) * 128, sl])
        k.dma("sync", ch.k[i % 2].v(), QK.v()[(2 + ch.hh) * 128:(3 + ch.hh) * 128, sl])
        k.dma("sync", ch.v[i % 2].v(), pT.v()[(RV + ch.hh) * 128:(RV + ch.hh + 1) * 128, sl])

    ident = c["ident"].v()

    def R1(ch, i):
        s_ = i % 2
        q, kk, v = ch.q[s_], ch.k[s_], ch.v[s_]
        p1, p2, p3 = ps(), ps(), ps()
        k.tr(p1[:, 0:128], v.v(), ident)
        k.tr(p2[:, 0:128], kk.v(), ident)
        k.mm(p3[:, 0:128], kk.v(), q.v())
        k.copy(ch.Vt[s_].v(), p1[:, 0:128], eng="scalar")
        k.ts(ch.Kz[s_].v(), p2[:, 0:128], ch.zg[:, 0:1], None, ALU.mult)
        k.tt(ch.St[s_].v(), p3[:, 0:128], ch.Dm.v(), ALU.mult)
        k.tt(ch.qx[s_].v(), q.v(), ch.XI.v(), ALU.mult, eng="gpsimd")

    def R2a(ch, i):
        s_ = i % 2
        ch.po, ch.pr = ps(), ps()
        k.mm(ch.po[:, 0:128], ch.Vt[s_].v(), ch.St[s_].v(), start=True, stop=False)
        k.mm(ch.po[:, 0:128], ch.R.v(), ch.qx[s_].v(), start=False, stop=True)
        k.mm(ch.pr[:, 0:128], ch.Kz[s_].v(), ch.Vt[s_].v())

    def R2b(ch, i):
        n = ORDER[ch.d][i]
        sl = slice(n * CK, (n + 1) * CK)
        oa = oacc[ch.hh]
        k.tt(oa[:, sl], oa[:, sl], ch.po[:, 0:128], ALU.add)
        k.stt(ch.R.v(), ch.R.v(), ch.zg[:, 1:2], ch.pr[:, 0:128], ALU.mult, ALU.add)

    for ch in chains:
        load(ch, 0)
        load(ch, 1)
    for ch in chains:
        R1(ch, 0)
    for i in range(NCHUNK):
        if i + 1 < NCHUNK:
            for ch in chains:
                R1(ch, i + 1)
        for ch in chains:
            R2a(ch, i)
        for ch in chains:
            R2b(ch, i)
            if i + 2 < NCHUNK:
                load(ch, i + 2)
    sc.__exit__(None, None, None)


def gn_finish(k, PS, c, gnp, oacc_h, gate_src, gate_func, yT, row0, pre_scale=None):
    with k.scope() as sc:
        tb = [[sc.sb(f"gf{i}_{j}", [128, 512], F32) for j in range(2)] for i in range(4)]
        for bi, (st, N, col) in enumerate(A_BLOCKS):
            sl = slice(st, st + N)
            t = [tb[i][bi % 2] for i in range(4)]
            gate_src(sl, N, t[3])
            k.act(t[3][:, :N], t[3][:, :N], gate_func)
            w = oacc_h
            k.mm(PS[0][:, :N], c["ones128"].v(), w[:, sl])
            k.act(t[0][:, :N], w[:, sl], AF.Square)
            k.mm(PS[1][:, :N], c["ones128"].v(), t[0][:, :N])
            k.copy(t[1][:, :N], PS[0][:, :N], eng="scalar")
            k.tt(t[2][:, :N], t[1][:, :N], t[1][:, :N], ALU.mult)
            k.tt(t[2][:, :N], PS[1][:, :N], t[2][:, :N], ALU.subtract)
            k.act(t[2][:, :N], t[2][:, :N], AF.Sqrt, bias=c["eps"].v())
            k.recip(t[2][:, :N], t[2][:, :N])
            k.tt(t[0][:, :N], w[:, sl], t[1][:, :N], ALU.subtract)
            k.tt(t[0][:, :N], t[0][:, :N], t[2][:, :N], ALU.mult)
            k.act(t[0][:, :N], t[0][:, :N], AF.Identity, scale=gnp[:, 0:1], bias=gnp[:, 1:2])
            k.tt(t[0][:, :N], t[0][:, :N], t[3][:, :N], ALU.mult)
            k.dma("sync", yT.v()[row0:row0 + 128, sl], t[0][:, :N])


NC_ = NCHUNK


def mlstm(k, PS, c, io, pT, QK, hacc):
    T = T_ALL
    sc = k.scope()
    sc.__enter__()
    gbias = sc.sb("gbias", [128, 4], F32)
    k.dma("sync", gbias.v(), io["gbias"].v())
    one = sc.sb("m_one", [128, 1], F32)
    k.memset(one.v(), 1.0)
    D = {}
    for d in range(2):
      for hh in range(2):
        o = type("D", (), {})()
        nm = f"m{d}{hh}"
        NCK = [NC_, CK]
        x = sc.sb(nm + "x", NCK, F32)
        t1 = sc.sb(nm + "t1", NCK, F32)
        t2 = sc.sb(nm + "t2", NCK, F32)
        bb = sc.sb(nm + "b", NCK, F32)
        cm = sc.sb(nm + "cm", NCK, F32)
        o.RQ = sc.sb(nm + "RQ", [NC_, 3, CK], F32)
        o.CW = sc.sb(nm + "CW", [NC_, 2, CK], F32)
        rows = sc.sb(nm + "rows", [1, 6, NC_], F32)
        colsb = sc.sb(nm + "colsb", [NC_, 4], F32)
        o.SOB = sc.sb(nm + "SOB", [128, NC_], F32)
        gb = sc.sb(nm + "gb", [NC_, 2], F32)
        k.dma("sync", gb.v(), io["gbias"].v()[hh:hh + 1, 2 * d:2 * d + 2].bcast([NC_, 2]))
        k.dma("sync", x.v(), pT.v()[(GF0 + 2 * d) * 128 + hh, :].re("(n j) -> n j", j=CK))
        k.dma("sync", t2.v(), pT.v()[(GI0 + 2 * d) * 128 + hh, :].re("(n j) -> n j", j=CK))
        k.ts(x.v(), x.v(), gb[:, 0:1], None, ALU.add)
        k.act(t1.v(), x.v(), AF.Abs)
        k.act(t1.v(), t1.v(), AF.Exp, scale=-1.0)
        k.act(t1.v(), t1.v(), AF.Ln, bias=one[0:NC_, :])
        k.ts(x.v(), x.v(), 0.0, None, ALU.min)
        k.tt(x.v(), x.v(), t1.v(), ALU.subtract)
        k.ts(t2.v(), t2.v(), gb[:, 1:2], None, ALU.add)
        rv = (lambda ap: ap) if d == 0 else (lambda ap: ap[:, ::-1])
        k.op("vector", lambda e: e.tensor_tensor_scan(rv(bb.ap), c["ones"].ap[0:NC_, :], rv(x.ap), 0.0, ALU.mult, ALU.add),
             [x.v(), c["ones"].v()], [bb.v()])
        k.tt(o.CW[:, 0, :], t2.v(), bb.v(), ALU.subtract)
        k.op("vector", lambda e: e.tensor_tensor_scan(rv(cm.ap), rv(o.CW.ap[:, 0, :]), rv(o.CW.ap[:, 0, :]), NEG, ALU.max, ALU.max),
             [o.CW.v()], [cm.v()])
        e0 = CK - 1 if d == 0 else 0
        identN = c["ident"][0:NC_, 0:NC_]
        pa, pb_ = PS[0], PS[1]
        k.tr(pa[0:1, 0:NC_], bb[:, e0:e0 + 1], identN)
        k.tr(pb_[0:1, 0:NC_], cm[:, e0:e0 + 1], identN)
        k.copy(rows[:, 0, :], pa[0:1, 0:NC_], eng="scalar")
        k.copy(rows[:, 1, :], pb_[0:1, 0:NC_], eng="scalar")
        bE, cE, Mn, mp, mu, so = (rows.ap[:, i, :] for i in range(6))
        rw = [rows.v()]
        if d == 0:
            k.op("vector", lambda e: e.tensor_tensor_scan(Mn, cE, bE, NEG, ALU.max, ALU.add), rw, rw)
            k.memset(rows[:, 3, 0:1], NEG)
            k.copy(rows[:, 3, 1:NC_], rows[:, 2, 0:NC_ - 1])
        else:
            k.op("vector", lambda e: e.tensor_tensor_scan(Mn[:, 0:2][:, ::-1], cE[:, 0:2][:, ::-1], bE[:, 0:2][:, ::-1], NEG, ALU.max, ALU.add), rw, rw)
            k.op("vector", lambda e: e.tensor_tensor_scan(Mn[:, 2:NC_][:, ::-1], cE[:, 2:NC_][:, ::-1], bE[:, 2:NC_][:, ::-1], Mn[:, 0:1], ALU.max, ALU.add), rw, rw)
            k.memset(rows[:, 3, 1:2], NEG)
            k.copy(rows[:, 3, 0:1], rows[:, 2, 1:2])
            k.copy(rows[:, 3, NC_ - 1:NC_], rows[:, 2, 0:1])
            k.copy(rows[:, 3, 2:NC_ - 1], rows[:, 2, 3:NC_])
        k.tt(rows[:, 4, :], rows[:, 3, :], rows[:, 1, :], ALU.max)
        k.tt(rows[:, 5, :], rows[:, 3, :], rows[:, 4, :], ALU.subtract)
        k.act(rows[:, 5, :], rows[:, 5, :], AF.Exp)
        ident1 = c["ident"][0:1, 0:1]
        pc_ = PS[2]
        k.tr(pc_[0:NC_, 0:1], rows[:, 3, :], ident1)
        k.tr(pc_[0:NC_, 1:2], rows[:, 4, :], ident1)
        k.copy(colsb[:, 0:2], pc_[0:NC_, 0:2], eng="scalar")
        k.ts(colsb[:, 2:3], colsb[:, 1:2], -1.0, None, ALU.mult)
        k.mm(PS[3][:, 0:NC_], c["ones"][0:1, :], rows[:, 5, :])
        k.copy(o.SOB.v(), PS[3][:, 0:NC_], eng="scalar")
        k.ts(o.RQ[:, 0, :], cm.v(), colsb[:, 0:1], -1.0, ALU.max, ALU.mult)
        k.ts(o.RQ[:, 1, :], o.RQ[:, 0, :], colsb[:, 0:1], None, ALU.add)
        k.tt(o.RQ[:, 2, :], o.RQ[:, 0, :], bb.v(), ALU.subtract)
        k.act(o.CW[:, 1, :], o.CW[:, 0, :], AF.Exp, bias=colsb[:, 2:3])
        D[(d, hh)] = o

    chains = []
    for hh in range(2):
        for d in range(2):
            ch = type("C", (), {})()
            ch.hh, ch.d = hh, d
            nm = f"ml{hh}{d}"
            ch.Ct = sc.sb(nm + "Ct", [128, 128], F32)
            ch.nB = sc.sb(nm + "nB", [128, 128], F32)
            k.memset(ch.Ct.v(), 0.0)
            k.memset(ch.nB.v(), 0.0)
            ch.q = [sc.sb(nm + f"q{i}", [128, 128], F32) for i in range(2)]
            ch.k = [sc.sb(nm + f"k{i}", [128, 128], F32) for i in range(2)]
            ch.v = [sc.sb(nm + f"v{i}", [128, 128], F32) for i in range(2)]
            for n_ in ["Wt", "den", "hh_"]:
                setattr(ch, n_, sc.sb(nm + n_, [128, 128], F32))
            for n_ in ["Vt", "Kw", "Sw", "qs"]:
                setattr(ch, n_, [sc.sb(nm + n_ + str(i_), [128, 128], F32) for i_ in range(2)])
            ch.SE = [sc.sb(nm + f"SE{i_}", [128, 256], F32) for i_ in range(2)]
            chains.append(ch)
    for ch in chains:
        ch.cq = [sc.sb(f"cq{ch.hh}{ch.d}_{i}", [128, 2], F32) for i in range(2)]
    psn = [0]

    def ps():
        p = PS[psn[0] % len(PS)]
        psn[0] += 1
        return p

    def load(ch, i):
        n = ORDER[ch.d][i]
        sl = slice(n * CK, (n + 1) * CK)
        k.dma("sync", ch.q[i % 2].v(), pT.v()[(MQ + ch.hh) * 128:(MQ + ch.hh + 1) * 128, sl])
        k.dma("sync", ch.k[i % 2].v(), QK.v()[(4 + ch.hh) * 128:(5 + ch.hh) * 128, sl])
        k.dma("sync", ch.v[i % 2].v(), pT.v()[(MV + ch.hh) * 128:(MV + ch.hh + 1) * 128, sl])

    ident = c["ident"].v()

    def M1(ch, i):
        d, hh = ch.d, ch.hh
        s_ = i % 2
        n = ORDER[d][i]
        q, kk, v = ch.q[s_], ch.k[s_], ch.v[s_]
        cq = ch.cq[s_]
        Dd = D[(d, hh)]
        oh = c["ident"][0:NC_, n:n + 1]
        p0 = ps()
        k.mm(p0[:, 0:1], Dd.CW[:, 0, :], oh)
        k.mm(p0[:, 1:2], Dd.CW[:, 1, :], oh)
        k.copy(cq.v(), p0[:, 0:2], eng="scalar")
        p1, p2, p3, p4 = ps(), ps(), ps(), ps()
        k.tr(p1[:, 0:128], v.v(), ident)
        k.tr(p2[:, 0:128], kk.v(), ident)
        k.mm(p3[:, 0:128], kk.v(), q.v())
        k.mm(p4[:, 0:384], oh.bcast([NC_, 128]), Dd.RQ.v().re("p a b -> p (a b)"))
        k.copy(ch.Vt[s_].v(), p1[:, 0:128], eng="scalar")
        k.ts(ch.Kw[s_].v(), p2[:, 0:128], cq[:, 1:2], None, ALU.mult)
        k.tt(ch.Wt.v(), p4[:, 0:128], (c["NEGF"] if d == 0 else c["NEGB"]).v(), ALU.add)
        k.act(ch.SE[s_].v(), p4[:, 128:384], AF.Exp)
        k.act(ch.Wt.v(), ch.Wt.v(), AF.Exp, bias=cq[:, 0:1])
        k.tt(ch.Sw[s_].v(), p3[:, 0:128], ch.Wt.v(), ALU.mult)
        k.tt(ch.qs[s_].v(), q.v(), ch.SE[s_][:, 0:128], ALU.mult, eng="gpsimd")

    def M2a(ch, i):
        s_ = i % 2
        ch.pn, ch.pd = ps(), ps()
        k.mm(ch.pn[:, 0:128], ch.Vt[s_].v(), ch.Sw[s_].v(), start=True, stop=False)
        k.mm(ch.pn[:, 0:128], ch.Ct.v(), ch.qs[s_].v(), start=False, stop=True)
        k.mm(ch.pd[:, 0:128], c["ones"].v(), ch.Sw[s_].v(), start=True, stop=False)
        k.mm(ch.pd[:, 0:128], ch.nB.v(), ch.qs[s_].v(), start=False, stop=True)

    def M2b(ch, i):
        s_ = i % 2
        n = ORDER[ch.d][i]
        sl = slice(n * CK, (n + 1) * CK)
        k.act(ch.den.v(), ch.pd[:, 0:128], AF.Abs)
        k.tt(ch.den.v(), ch.den.v(), ch.SE[s_][:, 128:256], ALU.max)
        k.recip(ch.den.v(), ch.den.v())
        k.tt(ch.hh_.v(), ch.pn[:, 0:128], ch.den.v(), ALU.mult)
        ha = hacc[ch.hh]
        k.tt(ha[:, sl], ha[:, sl], ch.hh_.v(), ALU.add, eng="gpsimd")

    def M2c(ch, i):
        s_ = i % 2
        ch.pc, ch.pb = ps(), ps()
        k.mm(ch.pc[:, 0:128], ch.Kw[s_].v(), ch.Vt[s_].v())
        k.mm(ch.pb[:, 0:128], ch.Kw[s_].v(), c["ones"].v())

    def M2d(ch, i):
        n = ORDER[ch.d][i]
        so = D[(ch.d, ch.hh)].SOB[:, n:n + 1]
        k.stt(ch.Ct.v(), ch.Ct.v(), so, ch.pc[:, 0:128], ALU.mult, ALU.add)
        k.stt(ch.nB.v(), ch.nB.v(), so, ch.pb[:, 0:128], ALU.mult, ALU.add)

    for ch in chains:
        load(ch, 0)
        load(ch, 1)
    for ch in chains:
        M1(ch, 0)
    for i in range(NC_):
        if i + 1 < NC_:
            for ch in chains:
                M1(ch, i + 1)
        for stg in (M2a, M2b, M2c, M2d):
            for ch in chains:
                stg(ch, i)
        if i + 2 < NC_:
            for ch in chains:
                load(ch, i + 2)
    sc.__exit__(None, None, None)


def pm(v, nchunk):
    return np.ascontiguousarray(np.asarray(v, np.float32).reshape(nchunk, 128).T)
def c_inputs(inp, layer, b, m, xlT, xcT, ylT, ycT):
    d = {}
    if xcT is not None:
        d["xT"] = np.ascontiguousarray(np.concatenate([xcT[:, 128*m:128*m+128], xlT[:, 2048*m:2048*m+2048]], 1))
        d["yT"] = np.ascontiguousarray(np.concatenate([ycT[:, 128*m:128*m+128], ylT[:, 2048*m:2048*m+2048]], 1))
    else:
        d["xT"] = np.ascontiguousarray(xlT[:, 2048*m:2048*m+2048])
        d["yT"] = np.ascontiguousarray(ylT[:, 2048*m:2048*m+2048])
    sc = np.stack([pm(inp["c"][b], 8), pm(inp["c_ctx"], 8)], -1)
    d["sc"] = np.ascontiguousarray(sc.reshape(128, 16))
    d["w_mod"] = np.ascontiguousarray(inp["w_mod"][layer])
    d["b_mod"] = pm(inp["b_mod"][layer], 48)
    d["lnp"] = np.ascontiguousarray(np.concatenate([pm(inp["ln_g"][layer,0],8), pm(inp["ln_g"][layer,1],8), pm(inp["ln_b"][layer,0],8), pm(inp["ln_b"][layer,1],8)], 1))
    d["w_out"] = np.ascontiguousarray(inp["even_w_out" if layer % 2 == 0 else "odd_w_out"][layer//2])
    d["rw"] = np.ascontiguousarray(inp["router_w"][layer])
    d["rb"] = np.ascontiguousarray(inp["router_b"][layer].reshape(32,1))
    d["w1"] = np.ascontiguousarray(inp["exp_w1"][layer])
    d["w2"] = np.ascontiguousarray(inp["exp_w2"][layer])
    d["b1"] = np.ascontiguousarray(inp["exp_b1"][layer].reshape(32,16,128).transpose(2,0,1).reshape(128, 512))
    d["b2"] = np.ascontiguousarray(inp["exp_b2"][layer].reshape(32,8,128).transpose(2,0,1).reshape(128, 256))
    d["ident"] = np.eye(128, dtype=np.float32)
    return d

def even_cols(m):
    cols = []
    for base in (0, 512, 1024):
        cols += list(range(base + 256*m, base + 256*m + 256))
    cols += list(range(1536, 1920))
    cols += list(range(1920 + 256*m, 1920 + 256*m + 256))
    cols += list(range(1920 + 512 + 256*m, 1920 + 512 + 256*m + 256))
    return np.array(cols)

def a0_inputs(inp, b, m, xallT):
    i = 0; layer = 0
    d = {}
    d["xT"] = np.ascontiguousarray(xallT)
    sc = np.stack([pm(inp["c"][b], 8), pm(inp["c_ctx"], 8)], -1)
    d["sc"] = np.ascontiguousarray(sc.reshape(128, 16))
    d["w_mod"] = np.ascontiguousarray(inp["w_mod"][layer])
    d["b_mod"] = pm(inp["b_mod"][layer], 48)
    d["w_in"] = np.ascontiguousarray(inp["even_w_in"][i][:, even_cols(m)])
    conv = np.zeros((128, 10), np.float32); gb = np.zeros((128, 8), np.float32); lam = np.zeros((128, 4), np.float32)
    gwbd = np.zeros((2, 2, 2, 128, 128), np.float32)
    for cc in range(2):
        ch = 256*m + 128*cc + np.arange(128)
        for j in range(4): conv[:, cc*5 + j] = inp["b_conv_w"][i][j, ch]
        conv[:, cc*5 + 4] = inp["b_conv_b"][i][ch]
        for dd in range(2):
            lam[:, cc*2 + dd] = inp["b_lam"][i][dd, ch]
            for s in range(2):
                gb[:, cc*4 + dd*2 + s] = inp["b_gate_b"][i][dd, s, ch]
                for g2 in range(2):
                    gwbd[cc, dd, s, g2*64:(g2+1)*64, g2*64:(g2+1)*64] = inp["b_gate_w"][i][dd, s, 4*m + 2*cc + g2]
    d["conv"] = conv; d["gb"] = gb; d["lam"] = lam; d["gwbd"] = gwbd
    d["ident"] = np.eye(128, dtype=np.float32)
    return d


def rwkv_consts():
    i = np.arange(128)
    r, c = i[:, None], i[None, :]
    MUs = (c > r).astype(np.float32); MLs = (c < r).astype(np.float32)
    MUi = (c >= r).astype(np.float32); MLi = (c <= r).astype(np.float32)
    bones = ((r // 64) == (c // 64)).astype(np.float32)
    mats = [np.eye(128, dtype=np.float32), bones, bones / 64.0,
            -MUs, -MLs, -MLs, -MUs, MUs, MUi, MUi, MLs, MLi, MLi]
    cst = np.ascontiguousarray(np.stack(mats, 1))
    rmask = np.ones((128, 256), np.float32); rmask[:, ::64] = 0.0
    m42 = np.stack([(i % 4 == s) for s in range(4)] + [(i % 2 == s) for s in range(2)], 1).astype(np.float32)
    return cst, rmask, np.ascontiguousarray(m42)

def rwkv_inputs(inp, m):
    i = 0
    rp = np.zeros((2, 128, 16), np.float32)
    wBs = np.zeros((2, 128, 128), np.float32); aBs = np.zeros((2, 128, 128), np.float32); gBs = np.zeros((2, 128, 128), np.float32)
    p = np.arange(128)
    mu = inp["a_mu"][i]
    for g in range(2):
        ch = 256*m + 128*g + p
        rp[g, :, 0] = mu[ch]; rp[g, :, 1] = mu[512 + ch]; rp[g, :, 2] = mu[1024 + ch]
        rp[g, :, 3] = mu[1536 + p]; rp[g, :, 4] = mu[1664 + p]; rp[g, :, 5] = mu[1792 + p]
        for d in range(2):
            rp[g, :, 6 + d] = inp["a_w0"][i][d, ch]; rp[g, :, 8 + d] = inp["a_a0"][i][d, ch]
            wBs[g, d*64:(d+1)*64, :] = inp["a_wB"][i][d][:, ch]
            aBs[g, d*64:(d+1)*64, :] = inp["a_aB"][i][d][:, ch]
        rp[g, :, 10] = inp["a_kk"][i][ch]; rp[g, :, 11] = inp["a_ka"][i][ch]; rp[g, :, 12] = inp["a_rk"][i].reshape(-1)[ch]
        rp[g, :, 13] = inp["a_gn_g"][i][ch]; rp[g, :, 14] = inp["a_gn_b"][i][ch]
        gBs[g] = inp["a_gB"][i][:, ch]
    cst, rmask, m42 = rwkv_consts()
    return {"rp": rp, "wBs": wBs, "aBs": aBs, "gBs": gBs, "cst": cst, "rmask": rmask, "m42": m42}


def odd_wsel(inp, m):
    W = inp["odd_w_in"][0]
    cols = []
    def blk(base, h): return list(range(base + 128*h, base + 128*h + 128))
    def swp(base, h): return list(range(base + 128*h + 64, base + 128*h + 128)) + list(range(base + 128*h, base + 128*h + 64))
    hs = [2*m, 2*m + 1]
    chunks = []
    for f, base in [(blk, 0), (swp, 0), (blk, 512), (swp, 512), (blk, 1024), (blk, 1536),
                    (blk, 2048), (blk, 2560), (blk, 3072), (blk, 3584)]:
        for h in hs:
            chunks.append(W[:, f(base, h)])
    for d in range(2):
        for kind in (1, 0):
            g = np.zeros((1024, 128), np.float32)
            for hh, h in enumerate(hs):
                g[:, hh] = W[:, 4096 + kind*8 + d*4 + h]
            chunks.append(g)
    return np.ascontiguousarray(np.concatenate(chunks, 1))

def odd_consts():
    i = np.arange(128)
    r, c = i[:, None].astype(np.float32), i[None, :].astype(np.float32)
    ABSD = np.abs(c - r)
    MUi = (c >= r).astype(np.float32); MLi = (c <= r).astype(np.float32)
    POS1F = np.broadcast_to(c + 1.0, (128, 128)); POS1B = np.broadcast_to(128.0 - c, (128, 128))
    NEGF = np.where(c >= r, 0.0, -1.0e30); NEGB = np.where(c <= r, 0.0, -1.0e30)
    mats = [np.eye(128), ABSD, MUi, MLi, POS1F, POS1B, np.full((128,128), 1/128.0), NEGF, NEGB, np.ones((128,128))]
    cst = np.ascontiguousarray(np.stack([np.asarray(a, np.float32) for a in mats], 1))
    ZP = np.stack([127.0 - i, i.astype(np.float64), np.full(128, 128.0)], 1).astype(np.float32)
    t = np.arange(4096); row = (t // 64).astype(np.float64); col = (t % 64).astype(np.float64)
    inv = 10000.0 ** (-np.arange(32) / 32.0)
    ang = np.concatenate([row[None, :] * inv[:, None], col[None, :] * inv[:, None]], 0)
    CC = np.concatenate([np.cos(ang), np.cos(ang)], 0).astype(np.float32)
    SS = np.concatenate([-np.sin(ang), np.sin(ang)], 0).astype(np.float32)
    rm128 = np.ones((128, 512), np.float32); rm128[:, ::128] = 0.0
    ng128 = np.zeros((128, 512), np.float32); ng128[:, ::128] = -1.0e30
    return {"ocst": cst, "ZP": ZP, "ropeC": np.ascontiguousarray(CC), "ropeS": np.ascontiguousarray(SS), "rm128": rm128, "ng128": ng128}

def a1_inputs(inp, b, m, xallT):
    layer = 1
    d = {}
    d["xT"] = np.ascontiguousarray(xallT)
    sc = np.stack([pm(inp["c"][b], 8), pm(inp["c_ctx"], 8)], -1)
    d["sc"] = np.ascontiguousarray(sc.reshape(128, 16))
    d["w_mod"] = np.ascontiguousarray(inp["w_mod"][layer])
    d["b_mod"] = pm(inp["b_mod"][layer], 48)
    d["w_in"] = odd_wsel(inp, m)
    p = np.arange(128)
    lgc = np.zeros((128, 4), np.float32); gnp = np.zeros((128, 8), np.float32); gbias = np.zeros((128, 4), np.float32)
    for hh in range(2):
        h = 2*m + hh
        for dd in range(2):
            lgc[:, hh*2 + dd] = inp["c_log_gamma"][0][dd, h]
        gnp[:, hh*2 + 0] = inp["c_gn_g"][0][128*h + p]; gnp[:, hh*2 + 1] = inp["c_gn_b"][0][128*h + p]
        gnp[:, 4 + hh*2 + 0] = inp["d_gn_g"][0][128*h + p]; gnp[:, 4 + hh*2 + 1] = inp["d_gn_b"][0][128*h + p]
    for dd in range(2):
        for hh in range(2):
            gbias[hh, dd*2 + 0] = inp["d_fbias"][0][dd, 2*m + hh]
            gbias[hh, dd*2 + 1] = inp["d_ibias"][0][dd, 2*m + hh]
    d["lgc"] = lgc; d["gnp"] = gnp; d["gbias"] = gbias
    d.update(odd_consts())
    return d

T = T_ALL


def _load_rconsts(k, io):
    c = {}
    cst = k.sb("cst", [128, 13, 128], F32)
    k.dma("sync", cst.v(), io["cst"].v())
    names = ["ident", "bones", "bones64"]
    for i, n in enumerate(names):
        c[n] = Buf(cst.ap[:, i, :], "c_" + n)
    for n, a, b in (("np2f", 3, 5), ("np2b", 5, 7), ("m3f", 7, 10), ("m3b", 10, 13)):
        c[n] = Buf(cst.ap[:, a:b, :].rearrange("p a b -> p (a b)"), "c_" + n)
    for n in c:
        c[n].w = cst.w
    c["rmask"] = k.sb("rmask", [128, 256], F32)
    k.dma("sync", c["rmask"].v(), io["rmask"].v())
    return c


def _load_oconsts(k, io):
    c = {}
    cst = k.sb("ocst", [128, 10, 128], F32)
    k.dma("sync", cst.v(), io["ocst"].v())
    names = ["ident", "ABSD", "MUi", "MLi", "POS1F", "POS1B", "ones128", "NEGF", "NEGB", "ones"]
    for i, n in enumerate(names):
        c[n] = Buf(cst.ap[:, i, :], "c_" + n)
        c[n].w = cst.w
    c["ZP"] = k.sb("ZP", [128, 3], F32)
    k.dma("sync", c["ZP"].v(), io["ZP"].v())
    c["eps"] = k.sb("oeps", [128, 1], F32)
    k.memset(c["eps"].v(), 1e-5)
    return c


SH_IN = [("xT", [1024, T]), ("sc", [128, 16]), ("sel", [128, 2]),
         ("cst", [128, 13, 128]), ("rmask", [128, 256]), ("m42", [128, 6]),
         ("ocst", [128, 10, 128]), ("ZP", [128, 3]), ("ropeC", [128, 4096]), ("ropeS", [128, 4096]), ("ident", [128, 128])]
L_IN = [("w_mod", [1024, 6144]), ("b_mod", [128, 48]), ("lnp", [128, 32]), ("w_out", [1024, 1024]), ("rw", [1024, 32]), ("rb", [32, 1]),
        ("w1", [32, 1024, 2048]), ("b1", [128, 512]), ("w2", [32, 1024, 1024]), ("b2", [128, 256])]
A0M_IN = [("w_in", [1024, 13 * 128]), ("conv", [128, 10]), ("gb", [128, 8]), ("lam", [128, 4]), ("gwbd", [2, 2, 2, 128, 128]),
          ("rp", [2, 128, 16]), ("wBs", [2, 128, 128]), ("aBs", [2, 128, 128]), ("gBs", [2, 128, 128])]
A1M_IN = [("w_in", [1024, 24 * 128]), ("lgc", [128, 4]), ("gnp", [128, 8]), ("gbias", [128, 4])]


MARKS = []


def build_fused():
    k = KB(n_dma_sems=48)
    del MARKS[:]
    mark = lambda n_: MARKS.append((n_, dict(k.cnt)))
    sh = {n: k.dram(n, shp, F32, kind="ExternalInput") for n, shp in SH_IN}
    L = [{n: k.dram(f"L{l}_{n}", shp, F32, kind="ExternalInput") for n, shp in L_IN} for l in range(2)]
    A0 = [{n: k.dram(f"A0m{m}_{n}", shp, F32, kind="ExternalInput") for n, shp in A0M_IN} for m in range(2)]
    A1 = [{n: k.dram(f"A1m{m}_{n}", shp, F32, kind="ExternalInput") for n, shp in A1M_IN} for m in range(2)]
    out = k.dram("out", [1024, 2048], F32, kind="ExternalOutput")
    pT = k.dram("pT", [24 * 128, T], F32, kind="Internal")
    PRE = [k.dram(f"PRE{g}", [9 * 128, T], F32, kind="Internal") for g in range(2)]
    GBd = [k.dram(f"GBd{g}", [2 * 128, T], F32, kind="Internal") for g in range(2)]
    QK = k.dram("QK", [6 * 128, T], F32, kind="Internal")
    yS = k.dram("yS", [1024, T], F32, kind="Internal")
    x1all = k.dram("x1all", [1024, T], F32, kind="Internal")
    x1d0 = k.dram("x1d0", [1024, T], F32, kind="Internal")
    x1d1 = k.dram("x1d1", [1024, 2048], F32, kind="Internal")
    PSb = [k.ps(f"ps{i}", [128, 512], F32) for i in range(8)]
    PSq = [PSView(b) for b in PSb]
    cr = _load_rconsts(k, sh)
    co = _load_oconsts(k, sh)
    cc = load_consts(k, sh)
    sel = k.sb("sel", [128, 2], F32)
    k.dma("sync", sel.v(), sh["sel"].v())

    modvL = [k.sb(f"modvL{l}", [128, 48, 2], F32) for l in range(2)]
    io = dict(sh)
    io.update(L[0])
    compute_mod(k, PSb, io, list(range(6)), modvL[0])
    mark('mod0')
    for m in range(2):
        io = dict(sh)
        io.update(L[0])
        io.update(A0[m])
        io["modv"] = modvL[0]
        in_proj(k, PSb, io, 13, pT)
        cast_weights(k, L[m], f"L{m}")
        rglru(k, PSb, io, pT, 9, 11, yS, 512 + 256 * m)
        for g in range(2):
            rwkv_prep(k, PSb, cr, io, pT, g, PRE[g], GBd[g])
        k.barrier()
        with k.scope() as sc:
            wkv = [sc.sb(f"wkv{g}", [128, T], F32) for g in range(2)]
            for g in range(2):
                k.memset(wkv[g].v(), 0.0)
            rwkv_scan2(k, PSb, cr, PRE, wkv)
            for g in range(2):
                rwkv_finish(k, PSb, cr, io, g, GBd[g], wkv, yS, 256 * m + g * 128)
        mark(f'A0m{m}')
    io = dict(sh)
    io.update(L[0])
    io["modv"] = modvL[0]
    io["yT"] = yS
    io["out"] = x1all
    io["x1d"] = x1d0
    groups = [[(0, 256, 1), (256, 512, 0), (768, 384, 0)], [(1152, 512, 0), (1664, 512, 0), (2176, 128, 0)],
              [(2304, 512, 0), (2816, 512, 0), (3328, 128, 0)], [(3456, 512, 0), (3968, 384, 0)]]
    phase_C(k, cc, io, groups, 1152, PSb)
    mark('C0')
    io = dict(sh)
    io.update(L[1])
    compute_mod(k, PSb, io, list(range(6)), modvL[1])
    mark('mod1')
    for m in range(2):
        io = dict(sh)
        io.update(L[1])
        io.update(A1[m])
        io["modv"] = modvL[1]
        io["xT"] = x1all
        in_proj(k, PSb, io, 24, pT)
        rope_pass(k, io, pT, QK)
        k.barrier()
        with k.scope() as sc:
            gnp = sc.sb("gnp", [128, 8], F32)
            k.dma("sync", gnp.v(), io["gnp"].v())
            oacc = [sc.sb(f"oacc{i}", [128, T], F32) for i in range(4)]
            for o in oacc:
                k.memset(o.v(), 0.0)
            retention(k, PSb, co, io, pT, QK, oacc)
            for hh in range(2):
                def gsrc(sl, N, tile, hh=hh):
                    k.dma("sync", tile[:, :N], pT.v()[(RG + hh) * 128:(RG + hh + 1) * 128, sl])
                gn_finish(k, PSb, co, gnp[:, hh * 2:hh * 2 + 2], oacc[hh], gsrc, AF.Silu, yS, 256 * m + hh * 128)
            mlstm(k, PSb, co, io, pT, QK, oacc[2:4])
            for hh in range(2):
                def gsrc(sl, N, tile, hh=hh):
                    k.dma("sync", tile[:, :N], pT.v()[(MO + hh) * 128:(MO + hh + 1) * 128, sl])
                gn_finish(k, PSb, co, gnp[:, 4 + hh * 2:4 + hh * 2 + 2], oacc[2 + hh], gsrc, AF.Sigmoid, yS, 512 + 256 * m + hh * 128)
        mark(f'A1m{m}')
    io = dict(sh)
    io.update(L[1])
    io["modv"] = modvL[1]
    io["out"] = out
    io["x1d"] = x1d1

    def load_xy(xb, yb, st, N, ta, tb):
        for (src, dst) in ((x1all, xb), (yS, yb)):
            o0, o1 = 256 + st, 256 + 2048 + st
            k.dma("sync", ta[:, :, :N], src.v()[:, o0:o0 + N].re("(c p) t -> p c t", p=128))
            k.dma("sync", tb[:, :, :N], src.v()[:, o1:o1 + N].re("(c p) t -> p c t", p=128))
            k.ts(ta[:, :, :N], ta[:, :, :N], sel[:, 0:1], None, ALU.mult)
            k.stt(dst[:, :, :N], tb[:, :, :N], sel[:, 1:2], ta[:, :, :N], ALU.mult, ALU.add)
    io["load_xy"] = load_xy
    groups = [[(0, 512, 0), (512, 512, 0)], [(1024, 512, 0), (1536, 512, 0)]]
    phase_C(k, cc, io, groups, 1024, PSb)
    mark('C1')
    k.finish([out])
    return k


def _core_inputs(inp, b, m):
    d = {}
    xall = np.ascontiguousarray(np.concatenate([inp["ctx"][b].T, inp["x"][b].T], 1))
    d["xT"] = xall
    sc = np.stack([pm(inp["c"][b], 8), pm(inp["c_ctx"], 8)], -1)
    d["sc"] = np.ascontiguousarray(sc.reshape(128, 16))
    sel = np.zeros((128, 2), np.float32)
    sel[:, m] = 1.0
    d["sel"] = sel
    cst, rmask, m42 = rwkv_consts()
    d["cst"], d["rmask"], d["m42"] = cst, rmask, m42
    oc = odd_consts()
    for n in ("ocst", "ZP", "ropeC", "ropeS"):
        d[n] = oc[n]
    d["ident"] = np.eye(128, dtype=np.float32)
    for l in range(2):
        ci = c_inputs(inp, l, b, 0, xall[:, 256:], None, xall[:, 256:], None)
        for n, _ in L_IN:
            d[f"L{l}_{n}"] = ci[n]
    for mm in range(2):
        a0 = a0_inputs(inp, b, mm, xall)
        a0.update(rwkv_inputs(inp, mm))
        for n, _ in A0M_IN:
            d[f"A0m{mm}_{n}"] = a0[n]
        a1 = a1_inputs(inp, b, mm, xall)
        for n, _ in A1M_IN:
            d[f"A1m{mm}_{n}"] = a1[n]
    return d


def kernel(**inp):
    inp = {k_: np.asarray(v) for k_, v in inp.items()}
    B = 4
    kf = build_fused()
    maps = []
    for b in range(B):
        d0 = _core_inputs(inp, b, 0)
        d1 = dict(d0)
        sel = np.zeros((128, 2), np.float32)
        sel[:, 1] = 1.0
        d1["sel"] = sel
        maps += [d0, d1]
    res = run_bass_kernel_spmd(kf.nc, maps, core_ids=list(range(8)))
    out = np.zeros((B, 4096, 1024), np.float32)
    for b in range(B):
        for m in range(2):
            out[b, 2048 * m:2048 * m + 2048, :] = res.results[2 * b + m]["out"].T
    return out
```

```python
import numpy as np
import concourse.bass as bass
import concourse.mybir as mybir
from concourse.bass_utils import run_bass_kernel_spmd

F32 = mybir.dt.float32
BF16 = mybir.dt.bfloat16
AF = mybir.ActivationFunctionType
ALU = mybir.AluOpType
AX = mybir.AxisListType


class Buf:
    def __init__(self, ap, name):
        self.ap = ap
        self.name = name
        self.w = None
        self.r = []
        self.excl = False

    def __getitem__(self, idx):
        return V(self, self.ap[idx])

    def v(self, ap=None):
        return V(self, self.ap if ap is None else ap)


class V:
    def __init__(self, buf, ap):
        self.buf = buf
        self.ap = ap

    def __getitem__(self, idx):
        return V(self.buf, self.ap[idx])

    def re(self, pat, **kw):
        return V(self.buf, self.ap.rearrange(pat, **kw))

    def bcast(self, shape):
        return V(self.buf, self.ap.to_broadcast(list(shape)))


class KB:
    ENG = ("tensor", "vector", "scalar", "gpsimd", "sync")

    def __init__(self, n_dma_sems=24, same_engine_sync=True):
        self.nc = bass.Bass("TRN2", target_bir_lowering=False)
        nc = self.nc
        self.e = {n: getattr(nc, n) for n in self.ENG}
        self.sem = {n: nc.alloc_semaphore(name=f"prog_{n}") for n in self.ENG}
        self.cnt = {n: 0 for n in self.ENG}
        self.seen = {n: {} for n in self.ENG}
        self.dsem = [nc.alloc_semaphore(name=f"dma_{i}") for i in range(n_dma_sems)]
        self.dcnt = [0] * n_dma_sems
        self.dnext = 0
        self.same_engine_sync = same_engine_sync
        self.out_tokens = []
        self.n_inst = 0

    def sb(self, name, shape, dtype=F32):
        self.uid = getattr(self, "uid", 0) + 1
        t = self.nc.alloc_sbuf_tensor(f"sb_{name}_p{self.uid}", list(shape), dtype)
        return Buf(t.ap(), name)

    def ps(self, name, shape, dtype=F32):
        t = self.nc.alloc_psum_tensor("psum_" + name, list(shape), dtype)
        b = Buf(t.ap(), name)
        b.excl = True
        return b

    def dram(self, name, shape, dtype=F32, kind="Internal"):
        t = self.nc.dram_tensor(name, list(shape), dtype, kind=kind)
        return Buf(t.ap(), name)

    def split(self, buf, views, name=None):
        return [Buf(v, f"{name or buf.name}_{i}") for i, v in enumerate(views)]

    def _wait(self, eng, tok):
        if tok is None:
            return
        kind = tok[0]
        if kind == "e":
            _, e2, c = tok
            if e2 == eng and (not self.same_engine_sync or eng == "tensor"):
                return
            key = ("e", e2)
            sem = self.sem[e2]
        else:
            _, i, c = tok
            key = ("d", i)
            sem = self.dsem[i]
        if self.seen[eng].get(key, 0) >= c:
            return
        self.e[eng].wait_ge(sem, c)
        self.seen[eng][key] = c

    def _deps(self, eng, reads, writes):
        for v in reads:
            self._wait(eng, v.buf.w)
        for v in writes:
            self._wait(eng, v.buf.w)
            for t in v.buf.r:
                self._wait(eng, t)

    def _mark(self, tok, reads, writes):
        for v in reads:
            b = v.buf
            b.r = [t for t in b.r if not (t[0] == tok[0] and t[1] == tok[1])] + [tok]
        for v in writes:
            v.buf.w = tok
            v.buf.r = []

    def op(self, eng, fn, reads, writes):
        ex = [v for v in reads if v.buf.excl]
        if ex:
            reads = [v for v in reads if not v.buf.excl]
            writes = list(writes) + ex
        self._deps(eng, reads, writes)
        inst = fn(self.e[eng])
        self.cnt[eng] += 1
        inst.then_inc(self.sem[eng], 1)
        self._mark(("e", eng, self.cnt[eng]), reads, writes)
        self.n_inst += 1
        return inst

    def dma(self, q, out, in_, **kw):
        i = self.dnext
        self.dnext = (self.dnext + 1) % len(self.dsem)
        if self.dcnt[i] > 0:
            self._wait(q, ("d", i, self.dcnt[i]))
        self._deps(q, [in_], [out])
        inst = self.e[q].dma_start(out=out.ap, in_=in_.ap, **kw)
        self.dcnt[i] += 16
        inst.then_inc(self.dsem[i], 16)
        tok = ("d", i, self.dcnt[i])
        self._mark(tok, [in_], [out])
        self.n_inst += 1
        return tok

    def dbg(self, name, v, shape, dtype=F32):
        if not getattr(self, "debug", False):
            return
        d = self.dram("dbg_" + name, list(shape), dtype, kind="ExternalOutput")
        self.dma("sync", d.v(), v)
        self.dbg_bufs = getattr(self, "dbg_bufs", []) + [d]

    def finish(self, bufs, eng="sync"):
        bufs = list(bufs) + getattr(self, "dbg_bufs", [])
        for b in bufs:
            self._wait(eng, b.w)

    def mm(self, out, lhsT, rhs, start=True, stop=True):
        return self.op("tensor", lambda e: e.matmul(out.ap, lhsT.ap, rhs.ap, start=start, stop=stop),
                       [lhsT, rhs] + ([] if start else [out]), [out])

    def tr(self, out, in_, ident):
        return self.op("tensor", lambda e: e.transpose(out.ap, in_.ap, ident.ap), [in_, ident], [out])

    def act(self, out, in_, func, bias=None, scale=None, eng="scalar", accum_out=None):
        kw = {}
        reads = [in_]
        writes = [out]
        if bias is not None:
            if isinstance(bias, V):
                kw["bias"] = bias.ap
                reads.append(bias)
            else:
                kw["bias"] = bias
        if scale is not None:
            if isinstance(scale, V):
                kw["scale"] = scale.ap
                reads.append(scale)
            else:
                kw["scale"] = scale
        if accum_out is not None:
            kw["accum_out"] = accum_out.ap
            writes.append(accum_out)
        return self.op(eng, lambda e: e.activation(out.ap, in_.ap, func, **kw), reads, writes)

    def tt(self, out, a, b, op, eng="vector"):
        return self.op(eng, lambda e: e.tensor_tensor(out.ap, a.ap, b.ap, op), [a, b], [out])

    def ts(self, out, a, s1, s2, op0, op1=None, eng="vector", accum_out=None):
        reads = [a]
        writes = [out]

        def g(s):
            if isinstance(s, V):
                reads.append(s)
                return s.ap
            return s
        s1a, s2a = g(s1), g(s2)
        kw = {}
        if op1 is not None:
            kw["op1"] = op1
        if accum_out is not None:
            kw["accum_out"] = accum_out.ap
            writes.append(accum_out)
        return self.op(eng, lambda e: e.tensor_scalar(out.ap, a.ap, s1a, s2a, op0, **kw), reads, writes)

    def stt(self, out, a, s, b, op0, op1, eng="vector"):
        reads = [a, b]
        if isinstance(s, V):
            reads.append(s)
            sa = s.ap
        else:
            sa = s
        return self.op(eng, lambda e: e.scalar_tensor_tensor(out.ap, a.ap, sa, b.ap, op0, op1), reads, [out])

    def copy(self, out, in_, eng="vector"):
        if eng == "scalar":
            return self.op(eng, lambda e: e.copy(out.ap, in_.ap), [in_], [out])
        return self.op(eng, lambda e: e.tensor_copy(out.ap, in_.ap), [in_], [out])

    def memset(self, out, val, eng="vector"):
        return self.op(eng, lambda e: e.memset(out.ap, val), [], [out])

    def reduce(self, out, in_, op, axis=AX.X, eng="vector"):
        return self.op(eng, lambda e: e.tensor_reduce(out.ap, in_.ap, axis, op), [in_], [out])

    def recip(self, out, in_, eng="vector"):
        return self.op(eng, lambda e: e.reciprocal(out.ap, in_.ap), [in_], [out])


import contextlib


class Scope:
    uid = 0

    def __init__(self, k):
        self.k = k
        self.stack = contextlib.ExitStack()

    def sb(self, name, shape, dtype=F32):
        Scope.uid += 1
        name = f"sc_{name}_u{Scope.uid}"
        t = self.stack.enter_context(self.k.nc.sbuf_tensor(name, list(shape), dtype))
        return Buf(t.ap(), name)

    def __enter__(self):
        return self

    def __exit__(self, *a):
        self.k.barrier()
        self.stack.close()
        return False


def _barrier(self):
    for eng in self.ENG:
        for e2 in self.ENG:
            if e2 != eng and self.cnt[e2] > 0:
                self._wait(eng, ("e", e2, self.cnt[e2]))
        for i, c in enumerate(self.dcnt):
            if c > 0:
                self._wait(eng, ("d", i, c))


KB.barrier = _barrier
KB.scope = lambda self: Scope(self)


class PSView:
    def __init__(self, b):
        self.b = b

    def v(self):
        return self.b[:, 0:128]

    def __getitem__(self, idx):
        return self.b[:, 0:128][idx]


D = 1024
NE = 32
DN_ALPHA = 4 ** 0.25
LN_EPS = 1e-5
SW_ALPHA = 1.702
SW_LIM = 7.0


def load_consts(k, cd):
    c = {}
    c["ident"] = k.sb("c_ident", [128, 128], F32)
    k.dma("sync", c["ident"].v(), cd["ident"].v())
    c["onesm"] = k.sb("c_onesm", [128, 128], F32)
    k.memset(c["onesm"].v(), 1.0 / D)
    c["eps"] = k.sb("c_eps", [128, 1], F32)
    k.memset(c["eps"].v(), LN_EPS)
    return c


def ln_block(k, c, s, N, gcol, bcol, out_fn, tmp, ps_mean, ps_ex2):
    sq, mean_sb, rstd, t = tmp["sq"], tmp["mean"], tmp["rstd"], tmp["t"]
    k.act(sq[:, :, :N], s[:, :, :N], AF.Square)
    for ci in range(8):
        k.mm(ps_mean[:, :N], c["onesm"].v(), s[:, ci, :N], start=(ci == 0), stop=(ci == 7))
    for ci in range(8):
        k.mm(ps_ex2[:, :N], c["onesm"].v(), sq[:, ci, :N], start=(ci == 0), stop=(ci == 7))
    k.copy(mean_sb[:, :N], ps_mean[:, :N], eng="scalar")
    k.tt(rstd[:, :N], mean_sb[:, :N], mean_sb[:, :N], ALU.mult)
    k.tt(rstd[:, :N], ps_ex2[:, :N], rstd[:, :N], ALU.subtract)
    k.act(rstd[:, :N], rstd[:, :N], AF.Sqrt, bias=c["eps"].v())
    k.recip(rstd[:, :N], rstd[:, :N])
    for ci in range(8):
        k.tt(t[:, :N], s[:, ci, :N], mean_sb[:, :N], ALU.subtract)
        k.tt(t[:, :N], t[:, :N], rstd[:, :N], ALU.mult)
        out_fn(ci, t[:, :N])


def phase_C(k, c, io, groups, NTG, PS):
    nc = k.nc
    psc = k.scope()
    psc.__enter__()
    modv = psc.sb("modv", [128, 48, 2], F32)
    sc1 = psc.sb("sc1", [128, 8, 2], F32)
    sc4 = psc.sb("sc4", [128, 8, 2], F32)
    lnp = psc.sb("lnp", [128, 32], F32)
    hT = psc.sb("hT", [128, 8, NTG], BF16)
    acc = psc.sb("acc", [128, 8, NTG], F32)
    gatesT = psc.sb("gatesT", [32, NTG], F32)
    b1s = psc.sb("b1s", [128, 32 * 16], F32)
    b2s = psc.sb("b2s", [128, 32 * 8], F32)
    rbs = psc.sb("rbs", [32, 1], F32)
    k.dma("sync", lnp.v(), io["lnp"].v())
    k.dma("sync", b1s.v(), io["b1"].v())
    k.dma("sync", b2s.v(), io["b2"].v())
    k.dma("sync", rbs.v(), io["rb"].v())

    if "modv" in io:
        k.copy(modv.v(), io["modv"].v(), eng="gpsimd")
    else:
        compute_mod(k, PS, io, list(range(6)), modv)
    k.ts(sc1.v(), modv[:, 8:16, :], 1.0, None, ALU.add)
    k.ts(sc4.v(), modv[:, 32:40, :], 1.0, None, ALU.add)

    def mv(m, ci, col):
        return modv[:, m * 8 + ci, col:col + 1]

    k.dbg("modv", modv.v(), [128, 48, 2])
    for blocks in groups:
        _group(k, c, io, blocks, PS, modv, sc4, lnp, hT, acc, gatesT, b1s, b2s, rbs, mv)
    psc.__exit__(None, None, None)


def _group(k, c, io, blocks, PS, modv, sc4, lnp, hT, acc, gatesT, b1s, b2s, rbs, mv):
    nc = k.nc
    g0 = blocks[0][0]
    k.memset(acc.v(), 0.0, eng="gpsimd")

    with k.scope() as sc:
        wo = sc.sb("wo", [128, 8, 1024], BF16)
        rw = sc.sb("rw", [128, 8, 32], F32)
        xb = sc.sb("xb", [128, 8, 512], F32)
        yb = sc.sb("yb", [128, 8, 512], BF16)
        s = sc.sb("s", [128, 8, 512], F32)
        x1 = sc.sb("x1", [128, 8, 512], F32)
        hf = sc.sb("hf", [128, 8, 512], F32)
        tmp = {"sq": sc.sb("sq", [128, 8, 512], F32), "mean": sc.sb("mean", [128, 512], F32),
               "rstd": sc.sb("rstd", [128, 512], F32), "t": sc.sb("t", [128, 512], F32)}
        t2 = sc.sb("t2", [128, 512], F32)
        lg = sc.sb("lg", [32, 512], F32)
        top8 = sc.sb("top8", [128, 8], F32)
        sm = sc.sb("sm", [128, 4], F32)
        ex = sc.sb("ex", [128, 32], F32)
        msk = sc.sb("msk", [128, 32], F32)
        k.dma("gpsimd", wo.v(), io["w_out"].v().re("(c p) n -> p c n", p=128))
        with nc.allow_non_contiguous_dma(reason="router w"):
            k.dma("sync", rw.v(), io["rw"].v().re("(c p) n -> p c n", p=128))
        for (st, N, col) in blocks:
            if "load_xy" in io:
                io["load_xy"](xb, yb, st, N, s, tmp["sq"])
            else:
                k.dma("sync", xb[:, :, :N], io["xT"].v()[:, st:st + N].re("(c p) t -> p c t", p=128))
                k.dma("gpsimd", yb[:, :, :N], io["yT"].v()[:, st:st + N].re("(c p) t -> p c t", p=128))
            for dc in range(8):
                ps = PS[dc % 2]
                for kc in range(8):
                    k.mm(ps[:, :N], wo[:, kc, dc * 128:(dc + 1) * 128], yb[:, kc, :N], start=(kc == 0), stop=(kc == 7))
                k.act(t2[:, :N], ps[:, :N], AF.Identity, scale=mv(2, dc, col))
                k.stt(s[:, dc, :N], xb[:, dc, :N], DN_ALPHA, t2[:, :N], ALU.mult, ALU.add)

            def o1(ci, tv, N=N, col=col):
                k.act(x1[:, ci, :N], tv, AF.Identity, scale=lnp[:, ci:ci + 1], bias=lnp[:, 16 + ci:17 + ci])
                k.act(hf[:, ci, :N], x1[:, ci, :N], AF.Identity, scale=sc4[:, ci, col:col + 1], bias=mv(3, ci, col))
                k.copy(hT[:, ci, st - g0:st - g0 + N], hf[:, ci, :N], eng="gpsimd")
            if st == 128:
                k.dbg("s", s.v(), [128, 8, 512])
            ln_block(k, c, s, N, None, None, o1, tmp, PS[2], PS[3])
            if st == 128:
                k.dbg("x1", x1.v(), [128, 8, 512])
                k.dbg("hf", hf.v(), [128, 8, 512])
                k.dbg("mean", tmp["mean"].v(), [128, 512])
                k.dbg("rstd", tmp["rstd"].v(), [128, 512])
            k.dma("sync", io["x1d"].v()[:, st:st + N].re("(c p) t -> p c t", p=128), x1[:, :, :N])
            for kc in range(8):
                k.mm(PS[4][0:32, :N], rw[:, kc, :], hf[:, kc, :N], start=(kc == 0), stop=(kc == 7))
            k.act(lg[:, :N], PS[4][0:32, :N], AF.Identity, bias=rbs.v())
            for j in range(N // 128):
                pt = PS[5 + (j % 2)]
                k.tr(pt[:, 0:32], lg[:, j * 128:(j + 1) * 128], c["ident"][0:32, 0:32])
                k.op("vector", lambda e: e.max(top8.ap, pt.ap[:, 0:32]), [pt.v()], [top8.v()])
                k.ts(sm[:, 0:1], top8[:, 0:1], -1.0, None, ALU.mult)
                k.act(ex.v(), pt[:, 0:32], AF.Exp, bias=sm[:, 0:1])
                k.ts(msk.v(), pt[:, 0:32], top8[:, 3:4], None, ALU.is_ge)
                k.tt(ex.v(), ex.v(), msk.v(), ALU.mult)
                k.reduce(sm[:, 1:2], ex.v(), ALU.add)
                k.recip(sm[:, 2:3], sm[:, 1:2])
                k.ts(ex.v(), ex.v(), sm[:, 2:3], None, ALU.mult)
                pg = PS[7]
                k.tr(pg[0:32, 0:128], ex.v(), c["ident"].v())
                k.copy(gatesT[:, st - g0 + j * 128: st - g0 + (j + 1) * 128], pg[0:32, 0:128], eng="scalar")

    if g0 == 0:
        k.dbg("gatesT", gatesT.v(), [32, gatesT.ap.shape[1]])
    with k.scope() as sc:
        NR = 3
        w1r = [sc.sb(f"w1r{i}", [128, 8, 2048], BF16) for i in range(2)]
        w2r = [sc.sb(f"w2r{i}", [128, 8, 1024], BF16) for i in range(2)]
        actT = [sc.sb(f"actT{i}", [128, 8, 512], BF16) for i in range(2)]
        g = [sc.sb(f"g{i}", [128, 512], F32) for i in range(2)]
        sg = [sc.sb(f"sg{i}", [128, 512], F32) for i in range(2)]
        l = [sc.sb(f"l{i}", [128, 512], F32) for i in range(2)]
        ty = [sc.sb(f"ty{i}", [128, 512], F32) for i in range(2)]
        items = [(e, bi) for e in range(NE) for bi in range(len(blocks))]
        cnt = [0]

        def loadw(e):
            w1 = w1r[e % 2]
            w2 = w2r[e % 2]
            if "w1b" in io:
                cb1 = io["w1b"][e // 4]
                cb2 = io["w2b"][e // 8]
                k.dma("sync", w1.v(), V(cb1, cb1.ap.rearrange("(e c p h) n -> e p c (h n)", e=4, p=128, h=2)[e % 4]))
                k.dma("sync", w2.v(), V(cb2, cb2.ap.rearrange("(e c p) n -> e p c n", e=8, p=128)[e % 8]))
            else:
                for h in range(2):
                    k.dma("gpsimd", w1[:, :, h * 1024:(h + 1) * 1024],
                          io["w1"].v()[e, :, h * 1024:(h + 1) * 1024].re("(c p) n -> p c n", p=128))
                k.dma("gpsimd", w2.v(), io["w2"].v()[e].re("(c p) n -> p c n", p=128))

        def W1(j):
            e, bi = items[j]
            if bi == 0:
                loadw(e)
            w1 = w1r[e % 2]
            (st, N, col) = blocks[bi]
            a = actT[j % 2]
            for fc in range(8):
                pg_, pl_ = PS[(fc % 2) * 2], PS[(fc % 2) * 2 + 1]
                for kc in range(8):
                    k.mm(pg_[:, :N], w1[:, kc, fc * 128:(fc + 1) * 128], hT[:, kc, st - g0:st - g0 + N], start=(kc == 0), stop=(kc == 7))
                for kc in range(8):
                    k.mm(pl_[:, :N], w1[:, kc, 1024 + fc * 128:1024 + (fc + 1) * 128], hT[:, kc, st - g0:st - g0 + N], start=(kc == 0), stop=(kc == 7))
                i2 = cnt[0] % 2
                cnt[0] += 1
                k.ts(g[i2][:, :N], pg_[:, :N], b1s[:, e * 16 + fc:e * 16 + fc + 1], SW_LIM, ALU.add, ALU.min)
                k.act(sg[i2][:, :N], g[i2][:, :N], AF.Silu, scale=SW_ALPHA)
                k.ts(l[i2][:, :N], pl_[:, :N], b1s[:, e * 16 + 8 + fc:e * 16 + 8 + fc + 1], SW_LIM, ALU.add, ALU.min)
                k.ts(l[i2][:, :N], l[i2][:, :N], -SW_LIM, 1.0, ALU.max, ALU.add)
                k.stt(a[:, fc, :N], l[i2][:, :N], 1.0 / SW_ALPHA, sg[i2][:, :N], ALU.mult, ALU.mult)

        def W2(j):
            e, bi = items[j]
            w2 = w2r[e % 2]
            (st, N, col) = blocks[bi]
            a = actT[j % 2]
            pgb = PS[6]
            k.mm(pgb[:, :N], c["ident"][0:32, e:e + 1].bcast([32, 128]), gatesT[:, st - g0:st - g0 + N])
            for dc in range(8):
                py = PS[4 + (dc % 2)]
                for fc in range(8):
                    k.mm(py[:, :N], w2[:, fc, dc * 128:(dc + 1) * 128], a[:, fc, :N], start=(fc == 0), stop=(fc == 7))
                t = ty[dc % 2]
                k.act(t[:, :N], py[:, :N], AF.Identity, bias=b2s[:, e * 8 + dc:e * 8 + dc + 1])
                k.tt(t[:, :N], t[:, :N], pgb[:, :N], ALU.mult)
                k.tt(acc[:, dc, st - g0:st - g0 + N], acc[:, dc, st - g0:st - g0 + N], t[:, :N], ALU.add, eng="gpsimd")

        W1(0)
        for j in range(len(items)):
            if j + 1 < len(items):
                W1(j + 1)
            W2(j)

    if g0 == 0:
        k.dbg("acc", acc.v(), list(acc.ap.shape))
    with k.scope() as sc:
        x1 = sc.sb("x1b", [128, 8, 512], F32)
        s = sc.sb("s2", [128, 8, 512], F32)
        xo = sc.sb("xo", [128, 8, 512], F32)
        tmp = {"sq": sc.sb("sq2", [128, 8, 512], F32), "mean": sc.sb("mean2", [128, 512], F32),
               "rstd": sc.sb("rstd2", [128, 512], F32), "t": sc.sb("tt2", [128, 512], F32)}
        t2 = sc.sb("t22", [128, 512], F32)
        for (st, N, col) in blocks:
            k.dma("sync", x1[:, :, :N], io["x1d"].v()[:, st:st + N].re("(c p) t -> p c t", p=128))
            for dc in range(8):
                k.act(t2[:, :N], acc[:, dc, st - g0:st - g0 + N], AF.Identity, scale=mv(5, dc, col))
                k.stt(s[:, dc, :N], x1[:, dc, :N], DN_ALPHA, t2[:, :N], ALU.mult, ALU.add)

            def o2(ci, tv, N=N):
                k.act(xo[:, ci, :N], tv, AF.Identity, scale=lnp[:, 8 + ci:9 + ci], bias=lnp[:, 24 + ci:25 + ci])
            ln_block(k, c, s, N, None, None, o2, tmp, PS[2], PS[3])
            k.dma("sync", io["out"].v()[:, st:st + N].re("(c p) t -> p c t", p=128), xo[:, :, :N])


def cast_weights(k, io, tag):
    w1f = io["w1"].ap.rearrange("e k (h n) -> (e k h) n", h=2)
    w2f = io["w2"].ap.rearrange("e k n -> (e k) n")
    w1b = k.dram(f"w1b_{tag}", [65536, 1024], BF16, kind="Internal")
    w2b = k.dram(f"w2b_{tag}", [32768, 1024], BF16, kind="Internal")
    io["w1b"] = k.split(w1b, [w1b.ap[i * 8192:(i + 1) * 8192, :] for i in range(8)], name=f"w1b_{tag}")
    io["w2b"] = k.split(w2b, [w2b.ap[i * 8192:(i + 1) * 8192, :] for i in range(4)], name=f"w2b_{tag}")
    for i in range(8):
        k.dma("gpsimd", io["w1b"][i].v(), V(io["w1"], w1f[i * 8192:(i + 1) * 8192, :]))
    for i in range(4):
        k.dma("gpsimd", io["w2b"][i].v(), V(io["w2"], w2f[i * 8192:(i + 1) * 8192, :]))


T_CTX = 256
T_LAT = 4096
T_ALL = T_CTX + T_LAT
A_BLOCKS = [(0, 256, 1)] + [(256 + 512 * i, 512, 0) for i in range(8)]


def compute_mod(k, PS, io, mlist, modv):
    with k.scope() as sc:
        scs = sc.sb("scs", [128, 8, 2], F32)
        bm = sc.sb("bm", [128, 48], F32)
        wm = [sc.sb(f"wm{i}", [128, 8, 512], F32) for i in range(4)]
        k.dma("sync", scs.v(), io["sc"].v().re("p (c j) -> p c j", j=2))
        k.dma("sync", bm.v(), io["b_mod"].v())
        k.act(scs.v(), scs.v(), AF.Silu)
        pieces = [(m, h) for m in mlist for h in range(2)]

        def load(i):
            m, h = pieces[i]
            c0 = m * 1024 + h * 512
            k.dma("sync", wm[i % 4].v(), io["w_mod"].v()[:, c0:c0 + 512].re("(c p) n -> p c n", p=128))
        for i in range(min(3, len(pieces))):
            load(i)
        for i, (m, h) in enumerate(pieces):
            if i + 3 < len(pieces):
                load(i + 3)
            w = wm[i % 4]
            for d4 in range(4):
                dc = h * 4 + d4
                ps = PS[dc % 2]
                for kc in range(8):
                    k.mm(ps[:, 0:2], w[:, kc, d4 * 128:(d4 + 1) * 128], scs[:, kc, :], start=(kc == 0), stop=(kc == 7))
                j = m * 8 + dc
                k.act(modv[:, j, :], ps[:, 0:2], AF.Identity, bias=bm[:, j:j + 1])


def in_proj(k, PS, io, NC, pT):
    nc = k.nc
    with k.scope() as sc:
        if "modv" in io:
            modv = io["modv"]
        else:
            modv = sc.sb("modvA", [128, 48, 2], F32)
            compute_mod(k, PS, io, [0, 1], modv)
        sc1 = sc.sb("sc1A", [128, 8, 2], F32)
        k.ts(sc1.v(), modv[:, 8:16, :], 1.0, None, ALU.add)
        hT = sc.sb("hTA", [128, 8, T_ALL], BF16)
        xb = [sc.sb(f"xbA{i}", [128, 8, 512], F32) for i in range(2)]
        for bi, (st, N, col) in enumerate(A_BLOCKS):
            x = xb[bi % 2]
            k.dma("sync", x[:, :, :N], io["xT"].v()[:, st:st + N].re("(c p) t -> p c t", p=128))
            for ci in range(8):
                k.act(hT[:, ci, st:st + N], x[:, ci, :N], AF.Identity, scale=sc1[:, ci, col:col + 1],
                      bias=modv[:, ci, col:col + 1], eng=("scalar" if ci % 2 == 0 else "scalar"))
        wr = [sc.sb(f"wA{i}", [128, 8, 128], BF16) for i in range(3)]
        ob = [sc.sb(f"obA{i}", [128, 512], F32) for i in range(4)]
        it = 0
        for c in range(NC):
            w = wr[c % 3]
            with nc.allow_non_contiguous_dma(reason="w_in col chunk"):
                k.dma("gpsimd", w.v(), io["w_in"].v()[:, c * 128:(c + 1) * 128].re("(c p) n -> p c n", p=128))
            for (st, N, col) in A_BLOCKS:
                ps = PS[it % 4]
                o = ob[it % 4]
                it += 1
                for kc in range(8):
                    k.mm(ps[:, :N], w[:, kc, :], hT[:, kc, st:st + N], start=(kc == 0), stop=(kc == 7))
                k.copy(o[:, :N], ps[:, :N], eng=("scalar" if it % 2 == 0 else "vector"))
                k.dma("sync", pT.v()[c * 128:(c + 1) * 128, st:st + N], o[:, :N])


def rglru(k, PS, io, pT, c_u0, c_g0, yT, y_row0):
    T = T_ALL
    SEG = [(0, T_CTX), (T_CTX, T_ALL)]
    with k.scope() as sc:
        cv = sc.sb("cv", [128, 10], F32)
        gb = sc.sb("gb", [128, 8], F32)
        lam = sc.sb("lam", [128, 4], F32)
        one = sc.sb("one", [128, 1], F32)
        k.memset(one.v(), 1.0)
        k.dma("sync", cv.v(), io["conv"].v())
        k.dma("sync", gb.v(), io["gb"].v())
        k.dma("sync", lam.v(), io["lam"].v())
        k.act(lam.v(), lam.v(), AF.Exp, scale=-1.0)
        k.act(lam.v(), lam.v(), AF.Ln, bias=one.v())
        k.ts(lam.v(), lam.v(), -8.0, None, ALU.mult)
        urs = [sc.sb(f"ur{i}", [128, T], F32) for i in range(2)]
        u = sc.sb("u", [128, T], F32)
        gts = [sc.sb(f"gt{i}", [128, T], F32) for i in range(2)]
        HT = T // 2

        def ldrows(dst, row0):
            for hf in range(2):
                k.dma("sync", dst[:, hf * HT:(hf + 1) * HT], pT.v()[row0:row0 + 128, hf * HT:(hf + 1) * HT])
        for cc_ in range(2):
            ldrows(urs[cc_], (c_u0 + cc_) * 128)
            ldrows(gts[cc_], (c_g0 + cc_) * 128)
        a = sc.sb("a", [128, T], F32)
        bx = sc.sb("bx", [128, T], F32)
        hs = sc.sb("hs", [128, T], F32)
        hb = sc.sb("hb", [128, T], F32)
        wbd = sc.sb("wbd", [128, 4, 128], F32)
        t1 = [sc.sb(f"t1_{i}", [128, 512], F32) for i in range(2)]
        t2 = [sc.sb(f"t2_{i}", [128, 512], F32) for i in range(2)]
        for cc in range(2):
            ur, gt = urs[cc], gts[cc]
            k.dma("sync", wbd.v(), io["gwbd"].v()[cc].re("d s p n -> p (d s) n"))
            w = lambda j: cv[:, cc * 5 + j:cc * 5 + j + 1]
            k.ts(u.v(), ur.v(), w(2), w(4), ALU.mult, ALU.add)
            for (s0, s1) in SEG:
                k.stt(u[:, s0 + 2:s1], ur[:, s0:s1 - 2], w(0), u[:, s0 + 2:s1], ALU.mult, ALU.add)
                k.stt(u[:, s0 + 1:s1], ur[:, s0:s1 - 1], w(1), u[:, s0 + 1:s1], ALU.mult, ALU.add)
                k.stt(u[:, s0:s1 - 1], ur[:, s0 + 1:s1], w(3), u[:, s0:s1 - 1], ALU.mult, ALU.add)
            k.act(ur.v(), gt.v(), AF.Square)
            k.ts(ur.v(), ur.v(), 0.044715, 1.0, ALU.mult, ALU.add, eng="gpsimd")
            k.tt(ur.v(), ur.v(), gt.v(), ALU.mult, eng="gpsimd")
            k.act(ur.v(), ur.v(), AF.Sigmoid, scale=1.5957691216057308)
            k.tt(gt.v(), gt.v(), ur.v(), ALU.mult, eng="gpsimd")
            for d in range(2):
                it = 0
                for (st, N, col) in A_BLOCKS:
                    pr, pi = PS[(it % 2) * 2], PS[(it % 2) * 2 + 1]
                    r_, i_ = t1[it % 2], t2[it % 2]
                    it += 1
                    k.mm(pr[:, :N], wbd[:, d * 2 + 0, :], u[:, st:st + N])
                    k.mm(pi[:, :N], wbd[:, d * 2 + 1, :], u[:, st:st + N])
                    k.act(r_[:, :N], pr[:, :N], AF.Sigmoid, bias=gb[:, cc * 4 + d * 2:cc * 4 + d * 2 + 1])
                    k.act(i_[:, :N], pi[:, :N], AF.Sigmoid, bias=gb[:, cc * 4 + d * 2 + 1:cc * 4 + d * 2 + 2])
                    k.act(a[:, st:st + N], r_[:, :N], AF.Exp, scale=lam[:, cc * 2 + d:cc * 2 + d + 1])
                    k.act(r_[:, :N], a[:, st:st + N], AF.Square)
                    k.ts(r_[:, :N], r_[:, :N], -1.0, 1.0, ALU.mult, ALU.add)
                    k.act(r_[:, :N], r_[:, :N], AF.Sqrt)
                    k.tt(i_[:, :N], i_[:, :N], r_[:, :N], ALU.mult)
                    k.tt(bx[:, st:st + N], i_[:, :N], u[:, st:st + N], ALU.mult)
                if d == 0:
                    k.op("vector", lambda e: e.tensor_tensor_scan(hs.ap, a.ap, bx.ap, 0.0, ALU.mult, ALU.add),
                         [a.v(), bx.v()], [hs.v()])
                else:
                    ra, rb_, rh = a.ap[:, 0:T_CTX][:, ::-1], bx.ap[:, 0:T_CTX][:, ::-1], hb.ap[:, 0:T_CTX][:, ::-1]
                    k.op("vector", lambda e: e.tensor_tensor_scan(rh, ra, rb_, 0.0, ALU.mult, ALU.add),
                         [a.v(), bx.v()], [hb.v()])
                    ra, rb_, rh = a.ap[:, T_CTX:T][:, ::-1], bx.ap[:, T_CTX:T][:, ::-1], hb.ap[:, T_CTX:T][:, ::-1]
                    k.op("vector", lambda e: e.tensor_tensor_scan(rh, ra, rb_, hb.ap[:, 0:1], ALU.mult, ALU.add),
                         [a.v(), bx.v(), hb.v()], [hb.v()])
            k.tt(hs.v(), hs.v(), hb.v(), ALU.add)
            k.tt(hs.v(), hs.v(), gt.v(), ALU.mult)
            k.dma("sync", yT.v()[y_row0 + cc * 128:y_row0 + (cc + 1) * 128, :], hs.v())


A_DECAY = -0.6065306597126334
GN_EPS_RWKV = 64e-5
CH = 64
RB = 256


def rwkv_prep(k, PS, c, io, pT, g, PRE, GBd):
    T = T_ALL
    with k.scope() as sc:
        rp = sc.sb("rp", [128, 16], F32)
        m42 = sc.sb("m42", [128, 6], F32)
        mus = sc.sb("mus", [128, 6, 7], F32)
        wBs = sc.sb("wBs", [128, 128], F32)
        aBs = sc.sb("aBs", [128, 128], F32)
        gBs = sc.sb("gBs", [128, 128], F32)
        k.dma("sync", rp.v(), io["rp"].v()[g])
        k.dma("sync", m42.v(), io["m42"].v())
        k.dma("sync", wBs.v(), io["wBs"].v()[g])
        k.dma("sync", aBs.v(), io["aBs"].v()[g])
        k.dma("sync", gBs.v(), io["gBs"].v()[g])
        for ty in range(6):
            k.ts(mus[:, ty, 0:1], rp[:, ty:ty + 1], -1.0, 1.0, ALU.mult, ALU.add)
            k.ts(mus[:, ty, 1:7], m42.v(), rp[:, ty:ty + 1], None, ALU.mult)
        pts = [sc.sb(f"ptmp{i}", [128, T], F32) for i in range(2)]
        z = [sc.sb(f"z{ty}", [128, T], F32) for ty in range(6)]
        chunk_ids = [g, 2 + g, 4 + g, 6, 7, 8]
        HT = T // 2

        def ldp(ty_):
            cid_ = chunk_ids[ty_]
            for hf in range(2):
                k.dma("sync", pts[ty_ % 2][:, hf * HT:(hf + 1) * HT], pT.v()[cid_ * 128:(cid_ + 1) * 128, hf * HT:(hf + 1) * HT])
        ldp(0)
        for ty in range(6):
            if ty + 1 < 6:
                ldp(ty + 1)
            pt = pts[ty % 2]
            zz = z[ty]
            eng = "vector"
            k.ts(zz.v(), pt.v(), mus[:, ty, 0:1], None, ALU.mult, eng=eng)
            pl = pt[:, T_CTX:T].re("p (r c) -> p r c", c=64)
            zl = zz[:, T_CTX:T].re("p (r c) -> p r c", c=64)
            k.stt(zl[:, :, 1:64], pl[:, :, 0:63], mus[:, ty, 1:2], zl[:, :, 1:64], ALU.mult, ALU.add)
            k.stt(zl[:, :, 0:63], pl[:, :, 1:64], mus[:, ty, 2:3], zl[:, :, 0:63], ALU.mult, ALU.add)
            k.stt(zl[:, 1:64, :], pl[:, 0:63, :], mus[:, ty, 3:4], zl[:, 1:64, :], ALU.mult, ALU.add)
            k.stt(zl[:, 0:63, :], pl[:, 1:64, :], mus[:, ty, 4:5], zl[:, 0:63, :], ALU.mult, ALU.add)
            k.stt(zz[:, 1:T_CTX], pt[:, 0:T_CTX - 1], mus[:, ty, 5:6], zz[:, 1:T_CTX], ALU.mult, ALU.add)
            k.stt(zz[:, 0:T_CTX - 1], pt[:, 1:T_CTX], mus[:, ty, 6:7], zz[:, 0:T_CTX - 1], ALU.mult, ALU.add)
        zr, zk, zv, zwd, zad, zg = z
        k.dma("sync", PRE.v()[0:128, :], zr.v())
        k.dma("sync", PRE.v()[128:256, :], zv.v())
        k.act(zwd.v(), zwd.v(), AF.Tanh)
        k.act(zg.v(), zg.v(), AF.Sigmoid)
        pt = pts[0]
        k.ts(pt.v(), zk.v(), rp[:, 10:11], None, ALU.mult)
        tb = [[sc.sb(f"tb{i}_{j}", [128, 512], F32) for j in range(2)] for i in range(10)]
        for bi, (st, N, col) in enumerate(A_BLOCKS):
            j2 = bi % 2
            t = [tb[i][j2] for i in range(10)]
            sl = slice(st, st + N)
            k.act(t[0][:, :N], pt[:, sl], AF.Square)
            k.mm(PS[0][:, :N], c["bones"].v(), t[0][:, :N])
            k.act(t[0][:, :N], PS[0][:, :N], AF.Sqrt)
            k.ts(t[0][:, :N], t[0][:, :N], 1e-12, None, ALU.max)
            k.recip(t[0][:, :N], t[0][:, :N])
            k.tt(t[0][:, :N], pt[:, sl], t[0][:, :N], ALU.mult)
            k.dma("sync", PRE.v()[256:384, sl], t[0][:, :N])
            k.mm(PS[1][:, :N], gBs.v(), zg[:, sl])
            k.copy(t[1][:, :N], PS[1][:, :N], eng="scalar")
            k.dma("sync", GBd.v()[0:128, sl], t[1][:, :N])
            for d in range(2):
                ds = slice(d * 64, (d + 1) * 64)
                pw, pa = PS[2 + d * 2], PS[3 + d * 2]
                k.mm(pw[:, :N], wBs[ds, :], zwd[ds, sl])
                k.mm(pa[:, :N], aBs[ds, :], zad[ds, sl])
                lw, av, kd, bb = t[2 + d * 3], t[8], t[3 + d * 3], t[4 + d * 3]
                k.act(lw[:, :N], pw[:, :N], AF.Sigmoid, bias=rp[:, 6 + d:7 + d])
                k.ts(lw[:, :N], lw[:, :N], A_DECAY, None, ALU.mult, eng="gpsimd")
                k.dma("sync", PRE.v()[(5 + 3 * d) * 128:(6 + 3 * d) * 128, sl], lw[:, :N])
                k.act(av[:, :N], pa[:, :N], AF.Sigmoid, bias=rp[:, 8 + d:9 + d])
                k.tt(bb[:, :N], t[0][:, :N], av[:, :N], ALU.mult)
                k.dma("sync", PRE.v()[(4 + 3 * d) * 128:(5 + 3 * d) * 128, sl], bb[:, :N])
                k.ts(kd[:, :N], av[:, :N], -1.0, rp[:, 11:12], ALU.add, ALU.mult)
                k.stt(kd[:, :N], kd[:, :N], 1.0, zk[:, sl], ALU.add, ALU.mult)
                k.dma("sync", PRE.v()[(3 + 3 * d) * 128:(4 + 3 * d) * 128, sl], kd[:, :N])
            k.tt(t[9][:, :N], t[3][:, :N], t[6][:, :N], ALU.add)
            k.stt(t[9][:, :N], zr[:, sl], rp[:, 12:13], t[9][:, :N], ALU.mult, ALU.mult)
            k.mm(PS[6][:, :N], c["bones"].v(), t[9][:, :N])
            k.tt(t[1][:, :N], PS[6][:, :N], zv[:, sl], ALU.mult)
            k.dma("sync", GBd.v()[128:256, sl], t[1][:, :N])


import os
USTEP = int(os.environ.get("USTEP", "5"))
NCH = int(os.environ.get("NCH", "4"))
NBK = int(os.environ.get("NBK", "100"))


class Chain:
    pass


def rwkv_scan(k, PSq, c, PREs, wkv):
    T = T_ALL
    NBLK = T // RB
    chains = []
    sc = k.scope()
    sc.__enter__()
    for g in range(2):
        for d in range(2):
            ch = Chain()
            ch.g, ch.d = g, d
            nm = f"c{g}{d}"
            ch.H = sc.sb(nm + "H", [128, 128], F32)
            k.memset(ch.H.v(), 0.0)
            ch.blk = [sc.sb(nm + f"blk{i}", [128, 6, RB], F32) for i in range(2)]
            ch.Lb = [sc.sb(nm + f"L{i}", [128, 4, RB], F32) for i in range(2)]
            names = ["kapT", "rT", "ktT", "btT", "khT", "bhT", "vT", "kh", "nbh", "V", "AkkT", "ArkT", "nArbT", "W", "U"]
            ch.t = {n: sc.sb(nm + n, [128, 128], F32) for n in names}
            for n in ["kapT", "rT", "ktT", "btT", "khT", "bhT", "vT"]:
                k.memset(ch.t[n].v(), 0.0, eng="gpsimd")
            ch.P = [sc.sb(nm + f"P{i}", [128, 128], F32) for i in range(2)]
            ch.PT = [sc.sb(nm + f"PT{i}", [128, 128], F32) for i in range(2)]
            ch.R = [sc.sb(nm + f"R{i}", [128, 128], F32) for i in range(2)]
            if d == 0:
                ch.order = list(range(NBLK))
            else:
                ch.order = [0] + list(range(NBLK - 1, 0, -1))
            chains.append(ch)
    psn = [0]

    def ps():
        p = PSq[psn[0] % len(PSq)]
        psn[0] += 1
        return p

    def load_block(ch, i):
        b = ch.order[i]
        buf = ch.blk[i % 2]
        rows = [0, 1, 2, 3 + 3 * ch.d, 4 + 3 * ch.d, 5 + 3 * ch.d]
        for j, r in enumerate(rows):
            k.dma("sync", buf[:, j, :], PREs[ch.g].v()[r * 128:(r + 1) * 128, b * RB:(b + 1) * RB])
        Lb = ch.Lb[i % 2]
        lw = buf[:, 5, :]
        if ch.d == 0:
            k.op("vector", lambda e: e.tensor_tensor_scan(Lb.ap[:, 0, :], c["rmask"].ap[:, 0:RB], lw.ap, 0.0, ALU.mult, ALU.add),
                 [lw, c["rmask"].v()], [Lb.v()])
        else:
            k.op("vector", lambda e: e.tensor_tensor_scan(Lb.ap[:, 0, :][:, ::-1], c["rmask"].ap[:, 0:RB], lw.ap[:, ::-1], 0.0, ALU.mult, ALU.add),
                 [lw, c["rmask"].v()], [Lb.v()])
        k.tt(Lb[:, 1, :], Lb[:, 0, :], lw, ALU.subtract, eng="gpsimd")
        k.act(Lb[:, 1, :], Lb[:, 1, :], AF.Exp)
        k.act(Lb[:, 2, :], Lb[:, 0, :], AF.Exp)
        k.act(Lb[:, 3, :], Lb[:, 0, :], AF.Exp, scale=-1.0)

    def unit(ch, i, j):
        d = ch.d
        buf = ch.blk[i % 2]
        Lb = ch.Lb[i % 2]
        cj = j if d == 0 else (RB // CH - 1 - j)
        c0 = cj * CH
        cs = slice(c0, c0 + CH)
        b = ch.order[i]
        tok0 = b * RB + c0
        last = c0 + CH - 1 if d == 0 else c0
        t = ch.t
        eLC = Lb[:, 2, last:last + 1]
        for h in range(2):
            ps_ = slice(h * 64, (h + 1) * 64)
            e1 = "vector" if h == 0 else "gpsimd"
            e2 = "gpsimd" if h == 0 else "vector"
            k.tt(t["kapT"][ps_, ps_], buf[ps_, 2, cs], Lb[ps_, 1, cs], ALU.mult, eng=e1)
            k.tt(t["rT"][ps_, ps_], buf[ps_, 0, cs], Lb[ps_, 2, cs], ALU.mult, eng=e2)
            k.tt(t["ktT"][ps_, ps_], buf[ps_, 3, cs], Lb[ps_, 3, cs], ALU.mult, eng=e1)
            k.tt(t["btT"][ps_, ps_], buf[ps_, 4, cs], Lb[ps_, 3, cs], ALU.mult, eng=e2)
            k.ts(t["khT"][ps_, ps_], t["ktT"][ps_, ps_], eLC[ps_, :], None, ALU.mult, eng=e1)
            k.ts(t["bhT"][ps_, ps_], t["btT"][ps_, ps_], eLC[ps_, :], None, ALU.mult, eng=e2)
            k.copy(t["vT"][ps_, ps_], buf[ps_, 1, cs], eng=e1)
        ident = c["ident"].v()
        if USTEP < 2:
            return
        p1, p2, p3 = ps(), ps(), ps()
        USUB = int(os.environ.get("USUB", "9"))
        k.tr(p1.v(), t["khT"].v(), ident)
        if USUB >= 2:
            k.tr(p2.v(), t["bhT"].v(), ident)
            k.tr(p3.v(), t["vT"].v(), ident)
        if USUB >= 3:
            k.copy(t["kh"].v(), p1.v(), eng="scalar")
        if USUB >= 4:
            k.act(t["nbh"].v(), p2.v(), AF.Identity, scale=-1.0)
            k.copy(t["V"].v(), p3.v(), eng="scalar")
        if USTEP < 3:
            return
        if d == 0:
            ms, msT, mi, nms, nmsT, nmi = c["MUs"], c["MLs"], c["MUi"], c["nMUs"], c["nMLs"], c["nMUi"]
        else:
            ms, msT, mi, nms, nmsT, nmi = c["MLs"], c["MUs"], c["MLi"], c["nMLs"], c["nMUs"], c["nMLi"]
        q1, q2, q3, q4, q5 = ps(), ps(), ps(), ps(), ps()
        k.mm(q1.v(), t["btT"].v(), t["kapT"].v())
        k.mm(q2.v(), t["kapT"].v(), t["btT"].v())
        k.mm(q3.v(), t["ktT"].v(), t["kapT"].v())
        k.mm(q4.v(), t["ktT"].v(), t["rT"].v())
        k.mm(q5.v(), t["btT"].v(), t["rT"].v())
        P, PT, R = ch.P, ch.PT, ch.R
        k.tt(P[0].v(), q1.v(), nms.v(), ALU.mult)
        k.tt(PT[0].v(), q2.v(), nmsT.v(), ALU.mult)
        k.tt(t["AkkT"].v(), q3.v(), ms.v(), ALU.mult)
        k.tt(t["ArkT"].v(), q4.v(), mi.v(), ALU.mult)
        k.tt(t["nArbT"].v(), q5.v(), nmi.v(), ALU.mult)
        k.tt(R[0].v(), P[0].v(), ident, ALU.add, eng="gpsimd")
        cur = 0
        if USTEP < 4:
            return
        for it in range(5):
            nx = 1 - cur
            a1, a2 = ps(), ps()
            k.mm(a1.v(), PT[cur].v(), P[cur].v())
            k.mm(a2.v(), P[cur].v(), PT[cur].v())
            k.copy(PT[nx].v(), a2.v(), eng="scalar")
            k.copy(P[nx].v(), a1.v(), eng="vector")
            a3 = ps()
            k.mm(a3.v(), PT[nx].v(), R[cur].v())
            k.tt(R[nx].v(), R[cur].v(), a3.v(), ALU.add)
            cur = nx
        Tt = R[cur]
        if USTEP < 5:
            return
        w_ps = ps()
        k.mm(w_ps.v(), t["kapT"].v(), ch.H.v(), start=True, stop=False)
        k.mm(w_ps.v(), t["AkkT"].v(), t["V"].v(), start=False, stop=True)
        k.copy(t["W"].v(), w_ps.v(), eng="scalar")
        u_ps = ps()
        k.mm(u_ps.v(), Tt.v(), t["W"].v())
        k.copy(t["U"].v(), u_ps.v(), eng="vector")
        o_ps = ps()
        k.mm(o_ps.v(), ch.H.v(), t["rT"].v(), start=True, stop=False)
        k.mm(o_ps.v(), t["V"].v(), t["ArkT"].v(), start=False, stop=False)
        k.mm(o_ps.v(), t["U"].v(), t["nArbT"].v(), start=False, stop=True)
        h_ps = ps()
        k.mm(h_ps.v(), t["kh"].v(), t["V"].v(), start=True, stop=False)
        k.mm(h_ps.v(), t["nbh"].v(), t["U"].v(), start=False, stop=True)
        wk = wkv[ch.g]
        for h in range(2):
            ps_ = slice(h * 64, (h + 1) * 64)
            k.tt(wk[ps_, tok0:tok0 + CH], wk[ps_, tok0:tok0 + CH], o_ps[ps_, ps_], ALU.add)
        k.stt(ch.H.v(), ch.H.v(), eLC, h_ps.v(), ALU.mult, ALU.add)

    NB = min(T // RB, NBK)
    chains = chains[:NCH]
    for ch in chains:
        load_block(ch, 0)
    for i in range(NB):
        for ch in chains:
            if i + 1 < NB:
                load_block(ch, i + 1)
        for j in range(RB // CH):
            for ch in chains:
                unit(ch, i, j)
    sc.__exit__(None, None, None)


def rwkv_finish(k, PS, c, io, g, GBd, wkv, yT, row0):
    T = T_ALL
    with k.scope() as sc:
        rp = sc.sb("rpf", [128, 16], F32)
        k.dma("sync", rp.v(), io["rp"].v()[g])
        eps = sc.sb("epsf", [128, 1], F32)
        k.memset(eps.v(), GN_EPS_RWKV)
        tb = [[sc.sb(f"tf{i}_{j}", [128, 512], F32) for j in range(2)] for i in range(5)]
        for bi, (st, N, col) in enumerate(A_BLOCKS):
            sl = slice(st, st + N)
            t = [tb[i][bi % 2] for i in range(5)]
            k.dma("sync", t[3][:, :N], GBd.v()[0:128, sl])
            k.dma("sync", t[4][:, :N], GBd.v()[128:256, sl])
            w = wkv[g]
            k.mm(PS[0][:, :N], c["bones64"].v(), w[:, sl])
            k.act(t[0][:, :N], w[:, sl], AF.Square)
            k.mm(PS[1][:, :N], c["bones64"].v(), t[0][:, :N])
            k.copy(t[1][:, :N], PS[0][:, :N], eng="scalar")
            k.tt(t[2][:, :N], t[1][:, :N], t[1][:, :N], ALU.mult)
            k.tt(t[2][:, :N], PS[1][:, :N], t[2][:, :N], ALU.subtract)
            k.act(t[2][:, :N], t[2][:, :N], AF.Sqrt, bias=eps.v())
            k.recip(t[2][:, :N], t[2][:, :N])
            k.tt(t[0][:, :N], w[:, sl], t[1][:, :N], ALU.subtract)
            k.tt(t[0][:, :N], t[0][:, :N], t[2][:, :N], ALU.mult)
            k.act(t[0][:, :N], t[0][:, :N], AF.Identity, scale=rp[:, 13:14], bias=rp[:, 14:15])
            k.tt(t[0][:, :N], t[0][:, :N], t[4][:, :N], ALU.add)
            k.tt(t[0][:, :N], t[0][:, :N], t[3][:, :N], ALU.mult)
            k.dma("sync", yT.v()[row0:row0 + 128, sl], t[0][:, :N])


def rwkv_scan2(k, PSb, c, PREs, wkv):
    T = T_ALL
    NBLK = T // RB
    UPB = RB // CH
    sc = k.scope()
    sc.__enter__()
    chains = []
    for g in range(2):
        for d in range(2):
            ch = Chain()
            ch.g, ch.d = g, d
            nm = f"s{g}{d}"
            ch.H = sc.sb(nm + "H", [128, 128], F32)
            k.memset(ch.H.v(), 0.0)
            ch.blk = [sc.sb(nm + f"blk{i}", [128, 6, RB], F32) for i in range(2)]
            ch.Lb = [sc.sb(nm + f"L{i}", [128, 4, RB], F32) for i in range(2)]
            ch.kapT = [sc.sb(nm + f"kapT{i}", [128, 128], F32) for i in range(2)]
            ch.rT = [sc.sb(nm + f"rT{i}", [128, 128], F32) for i in range(2)]
            ch.t = {n: sc.sb(nm + n, [128, 128], F32) for n in ["ktT", "btT", "khT", "nbhT", "vT", "W", "U"]}
            for tl in ch.kapT + ch.rT + [ch.t[n] for n in ["ktT", "btT", "khT", "nbhT", "vT"]]:
                k.memset(tl.v(), 0.0, eng="gpsimd")
            ch.TRS = [sc.sb(nm + f"TRS{i}", [128, 3, 128], F32) for i in range(2)]
            ch.AAA = [sc.sb(nm + f"AAA{i}", [128, 3, 128], F32) for i in range(2)]
            ch.PP = [sc.sb(nm + f"PP{i}", [128, 2, 128], F32) for i in range(2)]
            ch.R = [[sc.sb(nm + f"R{s_}{i}", [128, 128], F32) for i in range(2)] for s_ in range(2)]
            ch.order = list(range(NBLK)) if d == 0 else [0] + list(range(NBLK - 1, 0, -1))
            chains.append(ch)
    psn = [0]

    def ps():
        p = PSb[psn[0] % len(PSb)]
        psn[0] += 1
        return p

    def load_block(ch, i):
        b = ch.order[i]
        buf = ch.blk[i % 2]
        rows = [0, 1, 2, 3 + 3 * ch.d, 4 + 3 * ch.d, 5 + 3 * ch.d]
        for j, r in enumerate(rows):
            k.dma("sync", buf[:, j, :], PREs[ch.g].v()[r * 128:(r + 1) * 128, b * RB:(b + 1) * RB])
        Lb = ch.Lb[i % 2]
        lw = buf[:, 5, :]
        if ch.d == 0:
            k.op("vector", lambda e: e.tensor_tensor_scan(Lb.ap[:, 0, :], c["rmask"].ap[:, 0:RB], lw.ap, 0.0, ALU.mult, ALU.add),
                 [lw, c["rmask"].v()], [Lb.v()])
        else:
            k.op("vector", lambda e: e.tensor_tensor_scan(Lb.ap[:, 0, :][:, ::-1], c["rmask"].ap[:, 0:RB], lw.ap[:, ::-1], 0.0, ALU.mult, ALU.add),
                 [lw, c["rmask"].v()], [Lb.v()])
        k.tt(Lb[:, 1, :], Lb[:, 0, :], lw, ALU.subtract)
        k.act(Lb[:, 1, :], Lb[:, 1, :], AF.Exp)
        k.act(Lb[:, 2, :], Lb[:, 0, :], AF.Exp)
        k.act(Lb[:, 3, :], Lb[:, 0, :], AF.Exp, scale=-1.0)

    def geom(ch, n):
        i, j = n // UPB, n % UPB
        d = ch.d
        cj = j if d == 0 else (UPB - 1 - j)
        c0 = cj * CH
        last = c0 + CH - 1 if d == 0 else c0
        return i, slice(c0, c0 + CH), ch.order[i] * RB + c0, last

    ident = c["ident"].v()

    def A1(ch, n):
        i, cs, tok0, last = geom(ch, n)
        s_ = n % 2
        buf, Lb, t = ch.blk[i % 2], ch.Lb[i % 2], ch.t
        eLC = Lb[:, 2, last:last + 1]
        for h in range(2):
            p_ = slice(h * 64, (h + 1) * 64)
            e1 = "vector" if h == 0 else "gpsimd"
            e2 = "gpsimd" if h == 0 else "vector"
            k.tt(ch.kapT[s_][p_, p_], buf[p_, 2, cs], Lb[p_, 1, cs], ALU.mult, eng=e1)
            k.tt(ch.rT[s_][p_, p_], buf[p_, 0, cs], Lb[p_, 2, cs], ALU.mult, eng=e2)
            k.tt(t["ktT"][p_, p_], buf[p_, 3, cs], Lb[p_, 3, cs], ALU.mult, eng=e1)
            k.tt(t["btT"][p_, p_], buf[p_, 4, cs], Lb[p_, 3, cs], ALU.mult, eng=e2)
            k.act(t["khT"][p_, p_], t["ktT"][p_, p_], AF.Identity, scale=eLC[p_, :])
            k.act(t["nbhT"][p_, p_], t["btT"][p_, p_], AF.Identity, scale=eLC[p_, :])
            k.copy(t["vT"][p_, p_], buf[p_, 1, cs], eng=e1)

    def A2(ch, n):
        s_ = n % 2
        t = ch.t
        ch.pT3, ch.pPP, ch.pA3 = ps(), ps(), ps()
        k.tr(ch.pT3[:, 0:128], t["khT"].v(), ident)
        k.tr(ch.pT3[:, 128:256], t["nbhT"].v(), ident)
        k.tr(ch.pT3[:, 256:384], t["vT"].v(), ident)
        k.mm(ch.pPP[:, 0:128], t["btT"].v(), ch.kapT[s_].v())
        k.mm(ch.pPP[:, 128:256], ch.kapT[s_].v(), t["btT"].v())
        k.mm(ch.pA3[:, 0:128], t["ktT"].v(), ch.kapT[s_].v())
        k.mm(ch.pA3[:, 128:256], t["ktT"].v(), ch.rT[s_].v())
        k.mm(ch.pA3[:, 256:384], t["btT"].v(), ch.rT[s_].v())

    def A3(ch, n):
        s_ = n % 2
        f = "f" if ch.d == 0 else "b"
        k.copy(ch.TRS[s_].v().re("p a b -> p (a b)"), ch.pT3[:, 0:384], eng="scalar")
        k.tt(ch.PP[0].v().re("p a b -> p (a b)"), ch.pPP[:, 0:256], c["np2" + f].v(), ALU.mult)
        k.tt(ch.AAA[s_].v().re("p a b -> p (a b)"), ch.pA3[:, 0:384], c["m3" + f].v(), ALU.mult)
        k.tt(ch.R[s_][0].v(), ch.PP[0][:, 0, :], ident, ALU.add, eng="gpsimd")

    def Na(ch, n, it):
        cur = it % 2
        ch.pN = ps()
        k.mm(ch.pN[:, 0:128], ch.PP[cur][:, 1, :], ch.PP[cur][:, 0, :])
        k.mm(ch.pN[:, 128:256], ch.PP[cur][:, 0, :], ch.PP[cur][:, 1, :])

    def Nb(ch, n, it):
        nx = 1 - it % 2
        k.copy(ch.PP[nx].v().re("p a b -> p (a b)"), ch.pN[:, 0:256], eng=("scalar" if (it + ch.g) % 2 == 0 else "vector"))

    def Nc(ch, n, it):
        s_ = n % 2
        nx = 1 - it % 2
        ch.pR = ps()
        k.mm(ch.pR[:, 0:128], ch.PP[nx][:, 1, :], ch.R[s_][it % 2].v())

    def Nd(ch, n, it):
        s_ = n % 2
        k.tt(ch.R[s_][1 - it % 2].v(), ch.R[s_][it % 2].v(), ch.pR[:, 0:128], ALU.add)

    def B1(ch, n):
        s_ = n % 2
        ch.pW = ps()
        k.mm(ch.pW[:, 0:128], ch.kapT[s_].v(), ch.H.v(), start=True, stop=False)
        k.mm(ch.pW[:, 0:128], ch.AAA[s_][:, 0, :], ch.TRS[s_][:, 2, :], start=False, stop=True)

    def B2(ch, n):
        k.copy(ch.t["W"].v(), ch.pW[:, 0:128], eng="scalar")

    def B3(ch, n):
        s_ = n % 2
        ch.pU = ps()
        k.mm(ch.pU[:, 0:128], ch.R[s_][1].v(), ch.t["W"].v())

    def B4(ch, n):
        k.act(ch.t["U"].v(), ch.pU[:, 0:128], AF.Identity, scale=-1.0)

    def B5(ch, n):
        s_ = n % 2
        V_ = ch.TRS[s_][:, 2, :]
        ch.pO, ch.pH = ps(), ps()
        k.mm(ch.pO[:, 0:128], ch.H.v(), ch.rT[s_].v(), start=True, stop=False)
        k.mm(ch.pO[:, 0:128], V_, ch.AAA[s_][:, 1, :], start=False, stop=False)
        k.mm(ch.pO[:, 0:128], ch.t["U"].v(), ch.AAA[s_][:, 2, :], start=False, stop=True)
        k.mm(ch.pH[:, 0:128], ch.TRS[s_][:, 0, :], V_, start=True, stop=False)
        k.mm(ch.pH[:, 0:128], ch.TRS[s_][:, 1, :], ch.t["U"].v(), start=False, stop=True)

    def B6(ch, n):
        i, cs, tok0, last = geom(ch, n)
        eLC = ch.Lb[i % 2][:, 2, last:last + 1]
        wk = wkv[ch.g]
        for h in range(2):
            p_ = slice(h * 64, (h + 1) * 64)
            k.tt(wk[p_, tok0:tok0 + CH], wk[p_, tok0:tok0 + CH], ch.pO[p_, p_], ALU.add)
        k.stt(ch.H.v(), ch.H.v(), eLC, ch.pH[:, 0:128], ALU.mult, ALU.add)

    NU = NBLK * UPB

    def partA(n):
        if n == 0:
            for ch in chains:
                load_block(ch, 0)
        if n % UPB == 1:
            i = n // UPB
            for ch in chains:
                if i + 1 < NBLK:
                    load_block(ch, i + 1)
        for ch in chains:
            A1(ch, n)
        for ch in chains:
            A2(ch, n)
            A3(ch, n)
        for it in range(5):
            for st in (Na, Nb, Nc, Nd):
                for ch in chains:
                    st(ch, n, it)

    def partB(n):
        for st in (B1, B2, B3, B4, B5, B6):
            for ch in chains:
                st(ch, n)

    partA(0)
    for n in range(NU):
        if n + 1 < NU:
            partA(n + 1)
        partB(n)
    sc.__exit__(None, None, None)


LN_EPS = 1e-5
CK = 128
NCHUNK = T_ALL // CK
KSCALE = 128 ** -0.5
NEG = -1.0e30
RQ, RQS, RK, RKS, RV, RG = 0, 2, 4, 6, 8, 10
MQ, MK, MV, MO, GF0, GI0, GF1, GI1 = 12, 14, 16, 18, 20, 21, 22, 23
NC_ODD = 24
ORDER = {0: list(range(NCHUNK)), 1: [1, 0] + list(range(NCHUNK - 1, 1, -1))}


def rope_pass(k, io, pT, QK):
    with k.scope() as sc:
        a = [sc.sb(f"ra{i}", [128, 512], F32) for i in range(2)]
        b = [sc.sb(f"rb{i}", [128, 512], F32) for i in range(2)]
        cc = [sc.sb(f"rcc{i}", [128, 512], F32) for i in range(2)]
        ss = [sc.sb(f"rss{i}", [128, 512], F32) for i in range(2)]
        it = 0
        for (st, N, col) in A_BLOCKS:
            sl = slice(st, st + N)
            i2 = it % 2
            it += 1
            if col == 0:
                k.dma("sync", cc[i2][:, :N], io["ropeC"].v()[:, st - T_CTX:st - T_CTX + N])
                k.dma("sync", ss[i2][:, :N], io["ropeS"].v()[:, st - T_CTX:st - T_CTX + N])
            for hh in range(2):
                for (src, swp, dst, scale) in [(RQ + hh, RQS + hh, 0 + hh, 1.0), (RK + hh, RKS + hh, 2 + hh, KSCALE)]:
                    ta, tb_ = a[(hh) % 2], b[(hh) % 2]
                    k.dma("sync", ta[:, :N], pT.v()[src * 128:(src + 1) * 128, sl])
                    if col == 0:
                        k.dma("sync", tb_[:, :N], pT.v()[swp * 128:(swp + 1) * 128, sl])
                        k.tt(ta[:, :N], ta[:, :N], cc[i2][:, :N], ALU.mult)
                        k.tt(tb_[:, :N], tb_[:, :N], ss[i2][:, :N], ALU.mult, eng="gpsimd")
                        k.tt(ta[:, :N], ta[:, :N], tb_[:, :N], ALU.add)
                    if scale != 1.0:
                        k.ts(ta[:, :N], ta[:, :N], scale, None, ALU.mult)
                    k.dma("sync", QK.v()[dst * 128:(dst + 1) * 128, sl], ta[:, :N])
                ta = a[hh % 2]
                k.dma("sync", ta[:, :N], pT.v()[(MK + hh) * 128:(MK + hh + 1) * 128, sl])
                k.ts(ta[:, :N], ta[:, :N], KSCALE, None, ALU.mult)
                k.dma("sync", QK.v()[(4 + hh) * 128:(5 + hh) * 128, sl], ta[:, :N])


def retention(k, PS, c, io, pT, QK, oacc):
    sc = k.scope()
    sc.__enter__()
    lg = sc.sb("lgc", [128, 4], F32)
    k.dma("sync", lg.v(), io["lgc"].v())
    chains = []
    for hh in range(2):
        for d in range(2):
            ch = type("C", (), {})()
            ch.hh, ch.d = hh, d
            nm = f"r{hh}{d}"
            l = lg[:, hh * 2 + d:hh * 2 + d + 1]
            ch.Dm = sc.sb(nm + "Dm", [128, 128], F32)
            ch.XI = sc.sb(nm + "XI", [128, 128], F32)
            ch.zg = sc.sb(nm + "zg", [128, 2], F32)
            k.act(ch.Dm.v(), c["ABSD"].v(), AF.Exp, scale=l)
            k.tt(ch.Dm.v(), ch.Dm.v(), (c["MUi"] if d == 0 else c["MLi"]).v(), ALU.mult)
            k.act(ch.XI.v(), (c["POS1F"] if d == 0 else c["POS1B"]).v(), AF.Exp, scale=l)
            k.act(ch.zg[:, 0:1], c["ZP"][:, (0 if d == 0 else 1):(1 if d == 0 else 2)], AF.Exp, scale=l)
            k.act(ch.zg[:, 1:2], c["ZP"][:, 2:3], AF.Exp, scale=l)
            ch.R = sc.sb(nm + "R", [128, 128], F32)
            k.memset(ch.R.v(), 0.0)
            ch.q = [sc.sb(nm + f"q{i}", [128, 128], F32) for i in range(2)]
            ch.k = [sc.sb(nm + f"k{i}", [128, 128], F32) for i in range(2)]
            ch.v = [sc.sb(nm + f"v{i}", [128, 128], F32) for i in range(2)]
            ch.Vt = [sc.sb(nm + f"Vt{i}", [128, 128], F32) for i in range(2)]
            ch.Kz = [sc.sb(nm + f"Kz{i}", [128, 128], F32) for i in range(2)]
            ch.St = [sc.sb(nm + f"St{i}", [128, 128], F32) for i in range(2)]
            ch.qx = [sc.sb(nm + f"qx{i}", [128, 128], F32) for i in range(2)]
            chains.append(ch)
    psn = [0]

    def ps():
        p = PS[psn[0] % len(PS)]
        psn[0] += 1
        return p

    def load(ch, i):
        n = ORDER[ch.d][i]
        sl = slice(n * CK, (n + 1) * CK)
        k.dma("sync", ch.q[i % 2].v(), QK.v()[(0 + ch.hh) * 128:(1 + ch.hh) * 128, sl])
        k.dma("sync", ch.k[i % 2].v(), QK.v()[(2 + ch.hh) * 128:(3 + ch.hh) * 128, sl])
        k.dma("sync", ch.v[i % 2].v(), pT.v()[(RV + ch.hh) * 128:(RV + ch.hh + 1) * 128, sl])

    ident = c["ident"].v()

    def R1(ch, i):
        s_ = i % 2
        q, kk, v = ch.q[s_], ch.k[s_], ch.v[s_]
        p1, p2, p3 = ps(), ps(), ps()
        k.tr(p1[:, 0:128], v.v(), ident)
        k.tr(p2[:, 0:128], kk.v(), ident)
        k.mm(p3[:, 0:128], kk.v(), q.v())
        k.copy(ch.Vt[s_].v(), p1[:, 0:128], eng="scalar")
        k.ts(ch.Kz[s_].v(), p2[:, 0:128], ch.zg[:, 0:1], None, ALU.mult)
        k.tt(ch.St[s_].v(), p3[:, 0:128], ch.Dm.v(), ALU.mult)
        k.tt(ch.qx[s_].v(), q.v(), ch.XI.v(), ALU.mult, eng="gpsimd")

    def R2a(ch, i):
        s_ = i % 2
        ch.po, ch.pr = ps(), ps()
        k.mm(ch.po[:, 0:128], ch.Vt[s_].v(), ch.St[s_].v(), start=True, stop=False)
        k.mm(ch.po[:, 0:128], ch.R.v(), ch.qx[s_].v(), start=False, stop=True)
        k.mm(ch.pr[:, 0:128], ch.Kz[s_].v(), ch.Vt[s_].v())

    def R2b(ch, i):
        n = ORDER[ch.d][i]
        sl = slice(n * CK, (n + 1) * CK)
        oa = oacc[ch.hh]
        k.tt(oa[:, sl], oa[:, sl], ch.po[:, 0:128], ALU.add)
        k.stt(ch.R.v(), ch.R.v(), ch.zg[:, 1:2], ch.pr[:, 0:128], ALU.mult, ALU.add)

    for ch in chains:
        load(ch, 0)
        load(ch, 1)
    for ch in chains:
        R1(ch, 0)
    for i in range(NCHUNK):
        if i + 1 < NCHUNK:
            for ch in chains:
                R1(ch, i + 1)
        for ch in chains:
            R2a(ch, i)
        for ch in chains:
            R2b(ch, i)
            if i + 2 < NCHUNK:
                load(ch, i + 2)
    sc.__exit__(None, None, None)


def gn_finish(k, PS, c, gnp, oacc_h, gate_src, gate_func, yT, row0, pre_scale=None):
    with k.scope() as sc:
        tb = [[sc.sb(f"gf{i}_{j}", [128, 512], F32) for j in range(2)] for i in range(4)]
        for bi, (st, N, col) in enumerate(A_BLOCKS):
            sl = slice(st, st + N)
            t = [tb[i][bi % 2] for i in range(4)]
            gate_src(sl, N, t[3])
            k.act(t[3][:, :N], t[3][:, :N], gate_func)
            w = oacc_h
            k.mm(PS[0][:, :N], c["ones128"].v(), w[:, sl])
            k.act(t[0][:, :N], w[:, sl], AF.Square)
            k.mm(PS[1][:, :N], c["ones128"].v(), t[0][:, :N])
            k.copy(t[1][:, :N], PS[0][:, :N], eng="scalar")
            k.tt(t[2][:, :N], t[1][:, :N], t[1][:, :N], ALU.mult)
            k.tt(t[2][:, :N], PS[1][:, :N], t[2][:, :N], ALU.subtract)
            k.act(t[2][:, :N], t[2][:, :N], AF.Sqrt, bias=c["eps"].v())
            k.recip(t[2][:, :N], t[2][:, :N])
            k.tt(t[0][:, :N], w[:, sl], t[1][:, :N], ALU.subtract)
            k.tt(t[0][:, :N], t[0][:, :N], t[2][:, :N], ALU.mult)
            k.act(t[0][:, :N], t[0][:, :N], AF.Identity, scale=gnp[:, 0:1], bias=gnp[:, 1:2])
            k.tt(t[0][:, :N], t[0][:, :N], t[3][:, :N], ALU.mult)
            k.dma("sync", yT.v()[row0:row0 + 128, sl], t[0][:, :N])


NC_ = NCHUNK


def mlstm(k, PS, c, io, pT, QK, hacc):
    T = T_ALL
    sc = k.scope()
    sc.__enter__()
    gbias = sc.sb("gbias", [128, 4], F32)
    k.dma("sync", gbias.v(), io["gbias"].v())
    one = sc.sb("m_one", [128, 1], F32)
    k.memset(one.v(), 1.0)
    D = {}
    for d in range(2):
      for hh in range(2):
        o = type("D", (), {})()
        nm = f"m{d}{hh}"
        NCK = [NC_, CK]
        x = sc.sb(nm + "x", NCK, F32)
        t1 = sc.sb(nm + "t1", NCK, F32)
        t2 = sc.sb(nm + "t2", NCK, F32)
        bb = sc.sb(nm + "b", NCK, F32)
        cm = sc.sb(nm + "cm", NCK, F32)
        o.RQ = sc.sb(nm + "RQ", [NC_, 3, CK], F32)
        o.CW = sc.sb(nm + "CW", [NC_, 2, CK], F32)
        rows = sc.sb(nm + "rows", [1, 6, NC_], F32)
        colsb = sc.sb(nm + "colsb", [NC_, 4], F32)
        o.SOB = sc.sb(nm + "SOB", [128, NC_], F32)
        gb = sc.sb(nm + "gb", [NC_, 2], F32)
        k.dma("sync", gb.v(), io["gbias"].v()[hh:hh + 1, 2 * d:2 * d + 2].bcast([NC_, 2]))
        k.dma("sync", x.v(), pT.v()[(GF0 + 2 * d) * 128 + hh, :].re("(n j) -> n j", j=CK))
        k.dma("sync", t2.v(), pT.v()[(GI0 + 2 * d) * 128 + hh, :].re("(n j) -> n j", j=CK))
        k.ts(x.v(), x.v(), gb[:, 0:1], None, ALU.add)
        k.act(t1.v(), x.v(), AF.Abs)
        k.act(t1.v(), t1.v(), AF.Exp, scale=-1.0)
        k.act(t1.v(), t1.v(), AF.Ln, bias=one[0:NC_, :])
        k.ts(x.v(), x.v(), 0.0, None, ALU.min)
        k.tt(x.v(), x.v(), t1.v(), ALU.subtract)
        k.ts(t2.v(), t2.v(), gb[:, 1:2], None, ALU.add)
        rv = (lambda ap: ap) if d == 0 else (lambda ap: ap[:, ::-1])
        k.op("vector", lambda e: e.tensor_tensor_scan(rv(bb.ap), c["ones"].ap[0:NC_, :], rv(x.ap), 0.0, ALU.mult, ALU.add),
             [x.v(), c["ones"].v()], [bb.v()])
        k.tt(o.CW[:, 0, :], t2.v(), bb.v(), ALU.subtract)
        k.op("vector", lambda e: e.tensor_tensor_scan(rv(cm.ap), rv(o.CW.ap[:, 0, :]), rv(o.CW.ap[:, 0, :]), NEG, ALU.max, ALU.max),
             [o.CW.v()], [cm.v()])
        e0 = CK - 1 if d == 0 else 0
        identN = c["ident"][0:NC_, 0:NC_]
        pa, pb_ = PS[0], PS[1]
        k.tr(pa[0:1, 0:NC_], bb[:, e0:e0 + 1], identN)
        k.tr(pb_[0:1, 0:NC_], cm[:, e0:e0 + 1], identN)
        k.copy(rows[:, 0, :], pa[0:1, 0:NC_], eng="scalar")
        k.copy(rows[:, 1, :], pb_[0:1, 0:NC_], eng="scalar")
        bE, cE, Mn, mp, mu, so = (rows.ap[:, i, :] for i in range(6))
        rw = [rows.v()]
        if d == 0:
            k.op("vector", lambda e: e.tensor_tensor_scan(Mn, cE, bE, NEG, ALU.max, ALU.add), rw, rw)
            k.memset(rows[:, 3, 0:1], NEG)
            k.copy(rows[:, 3, 1:NC_], rows[:, 2, 0:NC_ - 1])
        else:
            k.op("vector", lambda e: e.tensor_tensor_scan(Mn[:, 0:2][:, ::-1], cE[:, 0:2][:, ::-1], bE[:, 0:2][:, ::-1], NEG, ALU.max, ALU.add), rw, rw)
            k.op("vector", lambda e: e.tensor_tensor_scan(Mn[:, 2:NC_][:, ::-1], cE[:, 2:NC_][:, ::-1], bE[:, 2:NC_][:, ::-1], Mn[:, 0:1], ALU.max, ALU.add), rw, rw)
            k.memset(rows[:, 3, 1:2], NEG)
            k.copy(rows[:, 3, 0:1], rows[:, 2, 1:2])
            k.copy(rows[:, 3, NC_ - 1:NC_], rows[:, 2, 0:1])
            k.copy(rows[:, 3, 2:NC_ - 1], rows[:, 2, 3:NC_])
        k.tt(rows[:, 4, :], rows[:, 3, :], rows[:, 1, :], ALU.max)
        k.tt(rows[:, 5, :], rows[:, 3, :], rows[:, 4, :], ALU.subtract)
        k.act(rows[:, 5, :], rows[:, 5, :], AF.Exp)
        ident1 = c["ident"][0:1, 0:1]
        pc_ = PS[2]
        k.tr(pc_[0:NC_, 0:1], rows[:, 3, :], ident1)
        k.tr(pc_[0:NC_, 1:2], rows[:, 4, :], ident1)
        k.copy(colsb[:, 0:2], pc_[0:NC_, 0:2], eng="scalar")
        k.ts(colsb[:, 2:3], colsb[:, 1:2], -1.0, None, ALU.mult)
        k.mm(PS[3][:, 0:NC_], c["ones"][0:1, :], rows[:, 5, :])
        k.copy(o.SOB.v(), PS[3][:, 0:NC_], eng="scalar")
        k.ts(o.RQ[:, 0, :], cm.v(), colsb[:, 0:1], -1.0, ALU.max, ALU.mult)
        k.ts(o.RQ[:, 1, :], o.RQ[:, 0, :], colsb[:, 0:1], None, ALU.add)
        k.tt(o.RQ[:, 2, :], o.RQ[:, 0, :], bb.v(), ALU.subtract)
        k.act(o.CW[:, 1, :], o.CW[:, 0, :], AF.Exp, bias=colsb[:, 2:3])
        D[(d, hh)] = o

    chains = []
    for hh in range(2):
        for d in range(2):
            ch = type("C", (), {})()
            ch.hh, ch.d = hh, d
            nm = f"ml{hh}{d}"
            ch.Ct = sc.sb(nm + "Ct", [128, 128], F32)
            ch.nB = sc.sb(nm + "nB", [128, 128], F32)
            k.memset(ch.Ct.v(), 0.0)
            k.memset(ch.nB.v(), 0.0)
            ch.q = [sc.sb(nm + f"q{i}", [128, 128], F32) for i in range(2)]
            ch.k = [sc.sb(nm + f"k{i}", [128, 128], F32) for i in range(2)]
            ch.v = [sc.sb(nm + f"v{i}", [128, 128], F32) for i in range(2)]
            for n_ in ["Wt", "den", "hh_"]:
                setattr(ch, n_, sc.sb(nm + n_, [128, 128], F32))
            for n_ in ["Vt", "Kw", "Sw", "qs"]:
                setattr(ch, n_, [sc.sb(nm + n_ + str(i_), [128, 128], F32) for i_ in range(2)])
            ch.SE = [sc.sb(nm + f"SE{i_}", [128, 256], F32) for i_ in range(2)]
            chains.append(ch)
    for ch in chains:
        ch.cq = [sc.sb(f"cq{ch.hh}{ch.d}_{i}", [128, 2], F32) for i in range(2)]
    psn = [0]

    def ps():
        p = PS[psn[0] % len(PS)]
        psn[0] += 1
        return p

    def load(ch, i):
        n = ORDER[ch.d][i]
        sl = slice(n * CK, (n + 1) * CK)
        k.dma("sync", ch.q[i % 2].v(), pT.v()[(MQ + ch.hh) * 128:(MQ + ch.hh + 1) * 128, sl])
        k.dma("sync", ch.k[i % 2].v(), QK.v()[(4 + ch.hh) * 128:(5 + ch.hh) * 128, sl])
        k.dma("sync", ch.v[i % 2].v(), pT.v()[(MV + ch.hh) * 128:(MV + ch.hh + 1) * 128, sl])

    ident = c["ident"].v()

    def M1(ch, i):
        d, hh = ch.d, ch.hh
        s_ = i % 2
        n = ORDER[d][i]
        q, kk, v = ch.q[s_], ch.k[s_], ch.v[s_]
        cq = ch.cq[s_]
        Dd = D[(d, hh)]
        oh = c["ident"][0:NC_, n:n + 1]
        p0 = ps()
        k.mm(p0[:, 0:1], Dd.CW[:, 0, :], oh)
        k.mm(p0[:, 1:2], Dd.CW[:, 1, :], oh)
        k.copy(cq.v(), p0[:, 0:2], eng="scalar")
        p1, p2, p3, p4 = ps(), ps(), ps(), ps()
        k.tr(p1[:, 0:128], v.v(), ident)
        k.tr(p2[:, 0:128], kk.v(), ident)
        k.mm(p3[:, 0:128], kk.v(), q.v())
        k.mm(p4[:, 0:384], oh.bcast([NC_, 128]), Dd.RQ.v().re("p a b -> p (a b)"))
        k.copy(ch.Vt[s_].v(), p1[:, 0:128], eng="scalar")
        k.ts(ch.Kw[s_].v(), p2[:, 0:128], cq[:, 1:2], None, ALU.mult)
        k.tt(ch.Wt.v(), p4[:, 0:128], (c["NEGF"] if d == 0 else c["NEGB"]).v(), ALU.add)
        k.act(ch.SE[s_].v(), p4[:, 128:384], AF.Exp)
        k.act(ch.Wt.v(), ch.Wt.v(), AF.Exp, bias=cq[:, 0:1])
        k.tt(ch.Sw[s_].v(), p3[:, 0:128], ch.Wt.v(), ALU.mult)
        k.tt(ch.qs[s_].v(), q.v(), ch.SE[s_][:, 0:128], ALU.mult, eng="gpsimd")

    def M2a(ch, i):
        s_ = i % 2
        ch.pn, ch.pd = ps(), ps()
        k.mm(ch.pn[:, 0:128], ch.Vt[s_].v(), ch.Sw[s_].v(), start=True, stop=False)
        k.mm(ch.pn[:, 0:128], ch.Ct.v(), ch.qs[s_].v(), start=False, stop=True)
        k.mm(ch.pd[:, 0:128], c["ones"].v(), ch.Sw[s_].v(), start=True, stop=False)
        k.mm(ch.pd[:, 0:128], ch.nB.v(), ch.qs[s_].v(), start=False, stop=True)

    def M2b(ch, i):
        s_ = i % 2
        n = ORDER[ch.d][i]
        sl = slice(n * CK, (n + 1) * CK)
        k.act(ch.den.v(), ch.pd[:, 0:128], AF.Abs)
        k.tt(ch.den.v(), ch.den.v(), ch.SE[s_][:, 128:256], ALU.max)
        k.recip(ch.den.v(), ch.den.v())
        k.tt(ch.hh_.v(), ch.pn[:, 0:128], ch.den.v(), ALU.mult)
        ha = hacc[ch.hh]
        k.tt(ha[:, sl], ha[:, sl], ch.hh_.v(), ALU.add, eng="gpsimd")

    def M2c(ch, i):
        s_ = i % 2
        ch.pc, ch.pb = ps(), ps()
        k.mm(ch.pc[:, 0:128], ch.Kw[s_].v(), ch.Vt[s_].v())
        k.mm(ch.pb[:, 0:128], ch.Kw[s_].v(), c["ones"].v())

    def M2d(ch, i):
        n = ORDER[ch.d][i]
        so = D[(ch.d, ch.hh)].SOB[:, n:n + 1]
        k.stt(ch.Ct.v(), ch.Ct.v(), so, ch.pc[:, 0:128], ALU.mult, ALU.add)
        k.stt(ch.nB.v(), ch.nB.v(), so, ch.pb[:, 0:128], ALU.mult, ALU.add)

    for ch in chains:
        load(ch, 0)
        load(ch, 1)
    for ch in chains:
        M1(ch, 0)
    for i in range(NC_):
        if i + 1 < NC_:
            for ch in chains:
                M1(ch, i + 1)
        for stg in (M2a, M2b, M2c, M2d):
            for ch in chains:
                stg(ch, i)
        if i + 2 < NC_:
            for ch in chains:
                load(ch, i + 2)
    sc.__exit__(None, None, None)


def pm(v, nchunk):
    return np.ascontiguousarray(np.asarray(v, np.float32).reshape(nchunk, 128).T)
def c_inputs(inp, layer, b, m, xlT, xcT, ylT, ycT):
    d = {}
    if xcT is not None:
        d["xT"] = np.ascontiguousarray(np.concatenate([xcT[:, 128*m:128*m+128], xlT[:, 2048*m:2048*m+2048]], 1))
        d["yT"] = np.ascontiguousarray(np.concatenate([ycT[:, 128*m:128*m+128], ylT[:, 2048*m:2048*m+2048]], 1))
    else:
        d["xT"] = np.ascontiguousarray(xlT[:, 2048*m:2048*m+2048])
        d["yT"] = np.ascontiguousarray(ylT[:, 2048*m:2048*m+2048])
    sc = np.stack([pm(inp["c"][b], 8), pm(inp["c_ctx"], 8)], -1)
    d["sc"] = np.ascontiguousarray(sc.reshape(128, 16))
    d["w_mod"] = np.ascontiguousarray(inp["w_mod"][layer])
    d["b_mod"] = pm(inp["b_mod"][layer], 48)
    d["lnp"] = np.ascontiguousarray(np.concatenate([pm(inp["ln_g"][layer,0],8), pm(inp["ln_g"][layer,1],8), pm(inp["ln_b"][layer,0],8), pm(inp["ln_b"][layer,1],8)], 1))
    d["w_out"] = np.ascontiguousarray(inp["even_w_out" if layer % 2 == 0 else "odd_w_out"][layer//2])
    d["rw"] = np.ascontiguousarray(inp["router_w"][layer])
    d["rb"] = np.ascontiguousarray(inp["router_b"][layer].reshape(32,1))
    d["w1"] = np.ascontiguousarray(inp["exp_w1"][layer])
    d["w2"] = np.ascontiguousarray(inp["exp_w2"][layer])
    d["b1"] = np.ascontiguousarray(inp["exp_b1"][layer].reshape(32,16,128).transpose(2,0,1).reshape(128, 512))
    d["b2"] = np.ascontiguousarray(inp["exp_b2"][layer].reshape(32,8,128).transpose(2,0,1).reshape(128, 256))
    d["ident"] = np.eye(128, dtype=np.float32)
    return d

def even_cols(m):
    cols = []
    for base in (0, 512, 1024):
        cols += list(range(base + 256*m, base + 256*m + 256))
    cols += list(range(1536, 1920))
    cols += list(range(1920 + 256*m, 1920 + 256*m + 256))
    cols += list(range(1920 + 512 + 256*m, 1920 + 512 + 256*m + 256))
    return np.array(cols)

def a0_inputs(inp, b, m, xallT):
    i = 0; layer = 0
    d = {}
    d["xT"] = np.ascontiguousarray(xallT)
    sc = np.stack([pm(inp["c"][b], 8), pm(inp["c_ctx"], 8)], -1)
    d["sc"] = np.ascontiguousarray(sc.reshape(128, 16))
    d["w_mod"] = np.ascontiguousarray(inp["w_mod"][layer])
    d["b_mod"] = pm(inp["b_mod"][layer], 48)
    d["w_in"] = np.ascontiguousarray(inp["even_w_in"][i][:, even_cols(m)])
    conv = np.zeros((128, 10), np.float32); gb = np.zeros((128, 8), np.float32); lam = np.zeros((128, 4), np.float32)
    gwbd = np.zeros((2, 2, 2, 128, 128), np.float32)
    for cc in range(2):
        ch = 256*m + 128*cc + np.arange(128)
        for j in range(4): conv[:, cc*5 + j] = inp["b_conv_w"][i][j, ch]
        conv[:, cc*5 + 4] = inp["b_conv_b"][i][ch]
        for dd in range(2):
            lam[:, cc*2 + dd] = inp["b_lam"][i][dd, ch]
            for s in range(2):
                gb[:, cc*4 + dd*2 + s] = inp["b_gate_b"][i][dd, s, ch]
                for g2 in range(2):
                    gwbd[cc, dd, s, g2*64:(g2+1)*64, g2*64:(g2+1)*64] = inp["b_gate_w"][i][dd, s, 4*m + 2*cc + g2]
    d["conv"] = conv; d["gb"] = gb; d["lam"] = lam; d["gwbd"] = gwbd
    d["ident"] = np.eye(128, dtype=np.float32)
    return d


def rwkv_consts():
    i = np.arange(128)
    r, c = i[:, None], i[None, :]
    MUs = (c > r).astype(np.float32); MLs = (c < r).astype(np.float32)
    MUi = (c >= r).astype(np.float32); MLi = (c <= r).astype(np.float32)
    bones = ((r // 64) == (c // 64)).astype(np.float32)
    mats = [np.eye(128, dtype=np.float32), bones, bones / 64.0,
            -MUs, -MLs, -MLs, -MUs, MUs, MUi, MUi, MLs, MLi, MLi]
    cst = np.ascontiguousarray(np.stack(mats, 1))
    rmask = np.ones((128, 256), np.float32); rmask[:, ::64] = 0.0
    m42 = np.stack([(i % 4 == s) for s in range(4)] + [(i % 2 == s) for s in range(2)], 1).astype(np.float32)
    return cst, rmask, np.ascontiguousarray(m42)

def rwkv_inputs(inp, m):
    i = 0
    rp = np.zeros((2, 128, 16), np.float32)
    wBs = np.zeros((2, 128, 128), np.float32); aBs = np.zeros((2, 128, 128), np.float32); gBs = np.zeros((2, 128, 128), np.float32)
    p = np.arange(128)
    mu = inp["a_mu"][i]
    for g in range(2):
        ch = 256*m + 128*g + p
        rp[g, :, 0] = mu[ch]; rp[g, :, 1] = mu[512 + ch]; rp[g, :, 2] = mu[1024 + ch]
        rp[g, :, 3] = mu[1536 + p]; rp[g, :, 4] = mu[1664 + p]; rp[g, :, 5] = mu[1792 + p]
        for d in range(2):
            rp[g, :, 6 + d] = inp["a_w0"][i][d, ch]; rp[g, :, 8 + d] = inp["a_a0"][i][d, ch]
            wBs[g, d*64:(d+1)*64, :] = inp["a_wB"][i][d][:, ch]
            aBs[g, d*64:(d+1)*64, :] = inp["a_aB"][i][d][:, ch]
        rp[g, :, 10] = inp["a_kk"][i][ch]; rp[g, :, 11] = inp["a_ka"][i][ch]; rp[g, :, 12] = inp["a_rk"][i].reshape(-1)[ch]
        rp[g, :, 13] = inp["a_gn_g"][i][ch]; rp[g, :, 14] = inp["a_gn_b"][i][ch]
        gBs[g] = inp["a_gB"][i][:, ch]
    cst, rmask, m42 = rwkv_consts()
    return {"rp": rp, "wBs": wBs, "aBs": aBs, "gBs": gBs, "cst": cst, "rmask": rmask, "m42": m42}


def odd_wsel(inp, m):
    W = inp["odd_w_in"][0]
    cols = []
    def blk(base, h): return list(range(base + 128*h, base + 128*h + 128))
    def swp(base, h): return list(range(base + 128*h + 64, base + 128*h + 128)) + list(range(base + 128*h, base + 128*h + 64))
    hs = [2*m, 2*m + 1]
    chunks = []
    for f, base in [(blk, 0), (swp, 0), (blk, 512), (swp, 512), (blk, 1024), (blk, 1536),
                    (blk, 2048), (blk, 2560), (blk, 3072), (blk, 3584)]:
        for h in hs:
            chunks.append(W[:, f(base, h)])
    for d in range(2):
        for kind in (1, 0):
            g = np.zeros((1024, 128), np.float32)
            for hh, h in enumerate(hs):
                g[:, hh] = W[:, 4096 + kind*8 + d*4 + h]
            chunks.append(g)
    return np.ascontiguousarray(np.concatenate(chunks, 1))

def odd_consts():
    i = np.arange(128)
    r, c = i[:, None].astype(np.float32), i[None, :].astype(np.float32)
    ABSD = np.abs(c - r)
    MUi = (c >= r).astype(np.float32); MLi = (c <= r).astype(np.float32)
    POS1F = np.broadcast_to(c + 1.0, (128, 128)); POS1B = np.broadcast_to(128.0 - c, (128, 128))
    NEGF = np.where(c >= r, 0.0, -1.0e30); NEGB = np.where(c <= r, 0.0, -1.0e30)
    mats = [np.eye(128), ABSD, MUi, MLi, POS1F, POS1B, np.full((128,128), 1/128.0), NEGF, NEGB, np.ones((128,128))]
    cst = np.ascontiguousarray(np.stack([np.asarray(a, np.float32) for a in mats], 1))
    ZP = np.stack([127.0 - i, i.astype(np.float64), np.full(128, 128.0)], 1).astype(np.float32)
    t = np.arange(4096); row = (t // 64).astype(np.float64); col = (t % 64).astype(np.float64)
    inv = 10000.0 ** (-np.arange(32) / 32.0)
    ang = np.concatenate([row[None, :] * inv[:, None], col[None, :] * inv[:, None]], 0)
    CC = np.concatenate([np.cos(ang), np.cos(ang)], 0).astype(np.float32)
    SS = np.concatenate([-np.sin(ang), np.sin(ang)], 0).astype(np.float32)
    rm128 = np.ones((128, 512), np.float32); rm128[:, ::128] = 0.0
    ng128 = np.zeros((128, 512), np.float32); ng128[:, ::128] = -1.0e30
    return {"ocst": cst, "ZP": ZP, "ropeC": np.ascontiguousarray(CC), "ropeS": np.ascontiguousarray(SS), "rm128": rm128, "ng128": ng128}

def a1_inputs(inp, b, m, xallT):
    layer = 1
    d = {}
    d["xT"] = np.ascontiguousarray(xallT)
    sc = np.stack([pm(inp["c"][b], 8), pm(inp["c_ctx"], 8)], -1)
    d["sc"] = np.ascontiguousarray(sc.reshape(128, 16))
    d["w_mod"] = np.ascontiguousarray(inp["w_mod"][layer])
    d["b_mod"] = pm(inp["b_mod"][layer], 48)
    d["w_in"] = odd_wsel(inp, m)
    p = np.arange(128)
    lgc = np.zeros((128, 4), np.float32); gnp = np.zeros((128, 8), np.float32); gbias = np.zeros((128, 4), np.float32)
    for hh in range(2):
        h = 2*m + hh
        for dd in range(2):
            lgc[:, hh*2 + dd] = inp["c_log_gamma"][0][dd, h]
        gnp[:, hh*2 + 0] = inp["c_gn_g"][0][128*h + p]; gnp[:, hh*2 + 1] = inp["c_gn_b"][0][128*h + p]
        gnp[:, 4 + hh*2 + 0] = inp["d_gn_g"][0][128*h + p]; gnp[:, 4 + hh*2 + 1] = inp["d_gn_b"][0][128*h + p]
    for dd in range(2):
        for hh in range(2):
            gbias[hh, dd*2 + 0] = inp["d_fbias"][0][dd, 2*m + hh]
            gbias[hh, dd*2 + 1] = inp["d_ibias"][0][dd, 2*m + hh]
    d["lgc"] = lgc; d["gnp"] = gnp; d["gbias"] = gbias
    d.update(odd_consts())
    return d

T = T_ALL


def _load_rconsts(k, io):
    c = {}
    cst = k.sb("cst", [128, 13, 128], F32)
    k.dma("sync", cst.v(), io["cst"].v())
    names = ["ident", "bones", "bones64"]
    for i, n in enumerate(names):
        c[n] = Buf(cst.ap[:, i, :], "c_" + n)
    for n, a, b in (("np2f", 3, 5), ("np2b", 5, 7), ("m3f", 7, 10), ("m3b", 10, 13)):
        c[n] = Buf(cst.ap[:, a:b, :].rearrange("p a b -> p (a b)"), "c_" + n)
    for n in c:
        c[n].w = cst.w
    c["rmask"] = k.sb("rmask", [128, 256], F32)
    k.dma("sync", c["rmask"].v(), io["rmask"].v())
    return c


def _load_oconsts(k, io):
    c = {}
    cst = k.sb("ocst", [128, 10, 128], F32)
    k.dma("sync", cst.v(), io["ocst"].v())
    names = ["ident", "ABSD", "MUi", "MLi", "POS1F", "POS1B", "ones128", "NEGF", "NEGB", "ones"]
    for i, n in enumerate(names):
        c[n] = Buf(cst.ap[:, i, :], "c_" + n)
        c[n].w = cst.w
    c["ZP"] = k.sb("ZP", [128, 3], F32)
    k.dma("sync", c["ZP"].v(), io["ZP"].v())
    c["eps"] = k.sb("oeps", [128, 1], F32)
    k.memset(c["eps"].v(), 1e-5)
    return c


SH_IN = [("xT", [1024, T]), ("sc", [128, 16]), ("sel", [128, 2]),
         ("cst", [128, 13, 128]), ("rmask", [128, 256]), ("m42", [128, 6]),
         ("ocst", [128, 10, 128]), ("ZP", [128, 3]), ("ropeC", [128, 4096]), ("ropeS", [128, 4096]), ("ident", [128, 128])]
L_IN = [("w_mod", [1024, 6144]), ("b_mod", [128, 48]), ("lnp", [128, 32]), ("w_out", [1024, 1024]), ("rw", [1024, 32]), ("rb", [32, 1]),
        ("w1", [32, 1024, 2048]), ("b1", [128, 512]), ("w2", [32, 1024, 1024]), ("b2", [128, 256])]
A0M_IN = [("w_in", [1024, 13 * 128]), ("conv", [128, 10]), ("gb", [128, 8]), ("lam", [128, 4]), ("gwbd", [2, 2, 2, 128, 128]),
          ("rp", [2, 128, 16]), ("wBs", [2, 128, 128]), ("aBs", [2, 128, 128]), ("gBs", [2, 128, 128])]
A1M_IN = [("w_in", [1024, 24 * 128]), ("lgc", [128, 4]), ("gnp", [128, 8]), ("gbias", [128, 4])]


MARKS = []


def build_fused():
    k = KB(n_dma_sems=48)
    del MARKS[:]
    mark = lambda n_: MARKS.append((n_, dict(k.cnt)))
    sh = {n: k.dram(n, shp, F32, kind="ExternalInput") for n, shp in SH_IN}
    L = [{n: k.dram(f"L{l}_{n}", shp, F32, kind="ExternalInput") for n, shp in L_IN} for l in range(2)]
    A0 = [{n: k.dram(f"A0m{m}_{n}", shp, F32, kind="ExternalInput") for n, shp in A0M_IN} for m in range(2)]
    A1 = [{n: k.dram(f"A1m{m}_{n}", shp, F32, kind="ExternalInput") for n, shp in A1M_IN} for m in range(2)]
    out = k.dram("out", [1024, 2048], F32, kind="ExternalOutput")
    pT = k.dram("pT", [24 * 128, T], F32, kind="Internal")
    PRE = [k.dram(f"PRE{g}", [9 * 128, T], F32, kind="Internal") for g in range(2)]
    GBd = [k.dram(f"GBd{g}", [2 * 128, T], F32, kind="Internal") for g in range(2)]
    QK = k.dram("QK", [6 * 128, T], F32, kind="Internal")
    yS = k.dram("yS", [1024, T], F32, kind="Internal")
    x1all = k.dram("x1all", [1024, T], F32, kind="Internal")
    x1d0 = k.dram("x1d0", [1024, T], F32, kind="Internal")
    x1d1 = k.dram("x1d1", [1024, 2048], F32, kind="Internal")
    PSb = [k.ps(f"ps{i}", [128, 512], F32) for i in range(8)]
    PSq = [PSView(b) for b in PSb]
    cr = _load_rconsts(k, sh)
    co = _load_oconsts(k, sh)
    cc = load_consts(k, sh)
    sel = k.sb("sel", [128, 2], F32)
    k.dma("sync", sel.v(), sh["sel"].v())

    modvL = [k.sb(f"modvL{l}", [128, 48, 2], F32) for l in range(2)]
    io = dict(sh)
    io.update(L[0])
    compute_mod(k, PSb, io, list(range(6)), modvL[0])
    mark('mod0')
    for m in range(2):
        io = dict(sh)
        io.update(L[0])
        io.update(A0[m])
        io["modv"] = modvL[0]
        in_proj(k, PSb, io, 13, pT)
        cast_weights(k, L[m], f"L{m}")
        rglru(k, PSb, io, pT, 9, 11, yS, 512 + 256 * m)
        for g in range(2):
            rwkv_prep(k, PSb, cr, io, pT, g, PRE[g], GBd[g])
        k.barrier()
        with k.scope() as sc:
            wkv = [sc.sb(f"wkv{g}", [128, T], F32) for g in range(2)]
            for g in range(2):
                k.memset(wkv[g].v(), 0.0)
            rwkv_scan2(k, PSb, cr, PRE, wkv)
            for g in range(2):
                rwkv_finish(k, PSb, cr, io, g, GBd[g], wkv, yS, 256 * m + g * 128)
        mark(f'A0m{m}')
    io = dict(sh)
    io.update(L[0])
    io["modv"] = modvL[0]
    io["yT"] = yS
    io["out"] = x1all
    io["x1d"] = x1d0
    groups = [[(0, 256, 1), (256, 512, 0)], [(768, 512, 0), (1280, 512, 0)], [(1792, 512, 0), (2304, 512, 0)],
              [(2816, 512, 0), (3328, 512, 0)], [(3840, 512, 0)]]
    phase_C(k, cc, io, groups, 1152, PSb)
    mark('C0')
    io = dict(sh)
    io.update(L[1])
    compute_mod(k, PSb, io, list(range(6)), modvL[1])
    mark('mod1')
    for m in range(2):
        io = dict(sh)
        io.update(L[1])
        io.update(A1[m])
        io["modv"] = modvL[1]
        io["xT"] = x1all
        in_proj(k, PSb, io, 24, pT)
        rope_pass(k, io, pT, QK)
        k.barrier()
        with k.scope() as sc:
            gnp = sc.sb("gnp", [128, 8], F32)
            k.dma("sync", gnp.v(), io["gnp"].v())
            oacc = [sc.sb(f"oacc{i}", [128, T], F32) for i in range(4)]
            for o in oacc:
                k.memset(o.v(), 0.0)
            retention(k, PSb, co, io, pT, QK, oacc)
            for hh in range(2):
                def gsrc(sl, N, tile, hh=hh):
                    k.dma("sync", tile[:, :N], pT.v()[(RG + hh) * 128:(RG + hh + 1) * 128, sl])
                gn_finish(k, PSb, co, gnp[:, hh * 2:hh * 2 + 2], oacc[hh], gsrc, AF.Silu, yS, 256 * m + hh * 128)
            mlstm(k, PSb, co, io, pT, QK, oacc[2:4])
            for hh in range(2):
                def gsrc(sl, N, tile, hh=hh):
                    k.dma("sync", tile[:, :N], pT.v()[(MO + hh) * 128:(MO + hh + 1) * 128, sl])
                gn_finish(k, PSb, co, gnp[:, 4 + hh * 2:4 + hh * 2 + 2], oacc[2 + hh], gsrc, AF.Sigmoid, yS, 512 + 256 * m + hh * 128)
        mark(f'A1m{m}')
    io = dict(sh)
    io.update(L[1])
    io["modv"] = modvL[1]
    io["out"] = out
    io["x1d"] = x1d1

    def load_xy(xb, yb, st, N, ta, tb):
        for (src, dst) in ((x1all, xb), (yS, yb)):
            o0, o1 = 256 + st, 256 + 2048 + st
            k.dma("sync", ta[:, :, :N], src.v()[:, o0:o0 + N].re("(c p) t -> p c t", p=128))
            k.dma("sync", tb[:, :, :N], src.v()[:, o1:o1 + N].re("(c p) t -> p c t", p=128))
            k.ts(ta[:, :, :N], ta[:, :, :N], sel[:, 0:1], None, ALU.mult)
            k.stt(dst[:, :, :N], tb[:, :, :N], sel[:, 1:2], ta[:, :, :N], ALU.mult, ALU.add)
    io["load_xy"] = load_xy
    groups = [[(0, 512, 0), (512, 512, 0)], [(1024, 512, 0), (1536, 512, 0)]]
    phase_C(k, cc, io, groups, 1024, PSb)
    mark('C1')
    k.finish([out])
    return k


def _core_inputs(inp, b, m):
    d = {}
    xall = np.ascontiguousarray(np.concatenate([inp["ctx"][b].T, inp["x"][b].T], 1))
    d["xT"] = xall
    sc = np.stack([pm(inp["c"][b], 8), pm(inp["c_ctx"], 8)], -1)
    d["sc"] = np.ascontiguousarray(sc.reshape(128, 16))
    sel = np.zeros((128, 2), np.float32)
    sel[:, m] = 1.0
    d["sel"] = sel
    cst, rmask, m42 = rwkv_consts()
    d["cst"], d["rmask"], d["m42"] = cst, rmask, m42
    oc = odd_consts()
    for n in ("ocst", "ZP", "ropeC", "ropeS"):
        d[n] = oc[n]
    d["ident"] = np.eye(128, dtype=np.float32)
    for l in range(2):
        ci = c_inputs(inp, l, b, 0, xall[:, 256:], None, xall[:, 256:], None)
        for n, _ in L_IN:
            d[f"L{l}_{n}"] = ci[n]
    for mm in range(2):
        a0 = a0_inputs(inp, b, mm, xall)
        a0.update(rwkv_inputs(inp, mm))
        for n, _ in A0M_IN:
            d[f"A0m{mm}_{n}"] = a0[n]
        a1 = a1_inputs(inp, b, mm, xall)
        for n, _ in A1M_IN:
            d[f"A1m{mm}_{n}"] = a1[n]
    return d


def kernel(**inp):
    inp = {k_: np.asarray(v) for k_, v in inp.items()}
    B = 4
    kf = build_fused()
    maps = []
    for b in range(B):
        d0 = _core_inputs(inp, b, 0)
        d1 = dict(d0)
        sel = np.zeros((128, 2), np.float32)
        sel[:, 1] = 1.0
        d1["sel"] = sel
        maps += [d0, d1]
    res = run_bass_kernel_spmd(kf.nc, maps, core_ids=list(range(8)))
    out = np.zeros((B, 4096, 1024), np.float32)
    for b in range(B):
        for m in range(2):
            out[b, 2048 * m:2048 * m + 2048, :] = res.results[2 * b + m]["out"].T
    return out
```
